# Optimizing a Trainium2 kernel written in Bass

```python
import jax
import jax.numpy as jnp
from jax import lax
import numpy as np

D_MODEL = 1024
BATCH = 2
SEQ = 8192
DEPTH = 1

D_MIX = D_MODEL
GLA_HEADS = 4
GLA_DV = (D_MIX // 2) // GLA_HEADS
GLA_DK = GLA_DV // 2
GLA_LOWRANK = 16
GLA_TAU = 16.0
GLA_CHUNK = 16
ATT_HEADS = 8
ATT_HD = (D_MIX - GLA_HEADS * GLA_DV) // ATT_HEADS
DILATIONS = ((128, 1), (512, 4), (2048, 16))
ATT_BLOCK = 128
N_GROUPS = 4
EXPERTS_PER_GROUP = 8
N_EXPERTS = N_GROUPS * EXPERTS_PER_GROUP
TOP_K = 2
D_FF_EXPERT = 512
MOE_BLOCK = 128
EPS = 1e-6

GLA_QK = GLA_HEADS * GLA_DK
GLA_V = GLA_HEADS * GLA_DV
ATT_W = ATT_HEADS * ATT_HD
IN_SIZES = (GLA_QK, GLA_QK, GLA_V, GLA_V, GLA_LOWRANK, ATT_W, ATT_W, ATT_W)
IN_SPLITS = tuple(sum(IN_SIZES[:i + 1]) for i in range(len(IN_SIZES) - 1))
IN_COLS = sum(IN_SIZES)

kernel_name = 'hymba_gla_dilated_hmoe'


def rmsnorm(x, g):
    xf = x.astype(jnp.float32)
    y = xf * lax.rsqrt(jnp.mean(xf * xf, axis=-1, keepdims=True) + EPS)
    return (y * g.astype(jnp.float32)).astype(x.dtype)


def gla_chunked(q, k, v, log_a):
    B, T, H, DK = q.shape
    DV = v.shape[-1]
    C = GLA_CHUNK
    N = T // C

    def chunks(t):
        return t.astype(jnp.float32).reshape(B, N, C, H, t.shape[-1]).transpose(0, 3, 1, 2, 4)

    q = chunks(q) * (DK ** -0.5)
    k, v, la = chunks(k), chunks(v), chunks(log_a)
    G = jnp.cumsum(la, axis=3)
    causal = jnp.tril(jnp.ones((C, C), dtype=bool))[:, :, None]
    diff = G[:, :, :, :, None, :] - G[:, :, :, None, :, :]
    decay = jnp.where(causal, jnp.exp(jnp.where(causal, diff, 0.0)), 0.0)
    A = jnp.einsum('bhnid,bhnjd,bhnijd->bhnij', q, k, decay)
    o_intra = jnp.einsum('bhnij,bhnjv->bhniv', A, v)
    G_last = G[:, :, :, -1:, :]
    U = jnp.einsum('bhncd,bhncv->bhndv', k * jnp.exp(G_last - G), v)
    a_chunk = jnp.exp(G_last[:, :, :, 0, :])

    def step(S, inp):
        a, u = inp
        return a[..., None] * S + u, S

    S0 = jnp.zeros((B, H, DK, DV), jnp.float32)
    _, S_prev = lax.scan(step, S0, (jnp.moveaxis(a_chunk, 2, 0), jnp.moveaxis(U, 2, 0)))
    S_prev = jnp.moveaxis(S_prev, 0, 2)
    o_inter = jnp.einsum('bhncd,bhndv->bhncv', q * jnp.exp(G), S_prev)
    o = o_intra + o_inter
    return o.transpose(0, 2, 3, 1, 4).reshape(B, T, H, DV)


def dilated_branch(q, k, v, window, dilation):
    B, H, T, D = q.shape
    d = dilation
    L = T // d
    n_back = window // d
    P = ATT_BLOCK
    nb = -(-L // P)
    Lp = nb * P

    def phase(t):
        t = t.reshape(B, H, L, d, D).transpose(0, 1, 3, 2, 4)
        t = jnp.pad(t, ((0, 0), (0, 0), (0, 0), (0, Lp - L), (0, 0)))
        return t.reshape(B, H, d, nb, P, D)

    def with_prev(t):
        prev = jnp.concatenate([jnp.zeros_like(t[:, :, :, :1]), t[:, :, :, :-1]], axis=3)
        return jnp.concatenate([prev, t], axis=4)

    qb = phase(q)
    kb, vb = with_prev(phase(k)), with_prev(phase(v))
    s = jnp.einsum('bhrnqd,bhrnkd->bhrnqk', qb, kb) * (D ** -0.5)
    blk = jnp.arange(nb)[:, None, None]
    qpos = jnp.arange(P)[None, :, None] + P
    kpos = jnp.arange(2 * P)[None, None, :]
    dist = qpos - kpos
    valid = (dist >= 0) & (dist <= n_back) & (blk * P + kpos - P >= 0)
    s = jnp.where(valid, s, -jnp.inf)
    m = jnp.max(s, axis=-1)
    p = jnp.exp(s - m[..., None])
    den = jnp.sum(p, axis=-1)
    o = jnp.einsum('bhrnqk,bhrnkd->bhrnqd', p, vb) / den[..., None]

    def unphase(t):
        rest = t.shape[5:]
        t = t.reshape((B, H, d, Lp) + rest)[:, :, :, :L]
        t = jnp.moveaxis(t, 2, 3)
        return t.reshape((B, H, T) + rest)

    return unphase(o), unphase(m), unphase(den)


def hybrid_mixer(n, w_in, gla_gate_w2, gla_gate_b, gla_norm_g, w_out):
    B, T, _ = n.shape
    proj = n @ w_in
    gq, gk, gv, gr, glr, aq, ak, av = jnp.split(proj, IN_SPLITS, axis=-1)
    log_a = jax.nn.log_sigmoid((glr @ gla_gate_w2 + gla_gate_b).astype(jnp.float32)) / GLA_TAU
    heads = lambda t: t.reshape(B, T, GLA_HEADS, -1)
    o_gla = gla_chunked(heads(gq), heads(gk), heads(gv), heads(log_a))
    o_gla = o_gla * lax.rsqrt(jnp.mean(o_gla * o_gla, axis=-1, keepdims=True) + EPS) * gla_norm_g.astype(jnp.float32)
    o_gla = o_gla.reshape(B, T, GLA_V) * jax.nn.silu(gr.astype(jnp.float32))
    to_bhtd = lambda t: t.astype(jnp.float32).reshape(B, T, ATT_HEADS, ATT_HD).transpose(0, 2, 1, 3)
    q, k, v = to_bhtd(aq), to_bhtd(ak), to_bhtd(av)
    branches = [dilated_branch(q, k, v, w, d) for (w, d) in DILATIONS]
    o_all = jnp.stack([br[0] for br in branches])
    m_all = jnp.stack([br[1] for br in branches])
    den_all = jnp.stack([br[2] for br in branches])
    wts = den_all * jnp.exp(m_all - jnp.max(m_all, axis=0, keepdims=True))
    o_att = jnp.sum(wts[..., None] * o_all, axis=0) / jnp.sum(wts, axis=0)[..., None]
    o_att = o_att.transpose(0, 2, 1, 3).reshape(B, T, ATT_W)
    o = jnp.concatenate([o_gla, o_att], axis=-1).astype(n.dtype)
    return o @ w_out


def hier_moe(n, w_grp, b_grp, w_exp, b_exp, w1, w3, w2):
    B, T, D = n.shape
    M = B * T
    x = n.reshape(M, D)
    g_logits = (x @ w_grp).astype(jnp.float32) + b_grp.astype(jnp.float32)
    g_idx = jnp.argmax(g_logits, axis=-1).astype(jnp.int32)
    g_prob = jnp.take_along_axis(jax.nn.softmax(g_logits, axis=-1), g_idx[:, None], axis=-1)
    e_logits = ((x @ w_exp).astype(jnp.float32) + b_exp.astype(jnp.float32)).reshape(M, N_GROUPS, EXPERTS_PER_GROUP)
    e_logits = jnp.take_along_axis(e_logits, g_idx[:, None, None], axis=1)[:, 0]
    top_v, top_i = lax.top_k(e_logits, TOP_K)
    gate = jax.nn.softmax(top_v, axis=-1) * g_prob
    expert_id = g_idx[:, None] * EXPERTS_PER_GROUP + top_i.astype(jnp.int32)
    A = M * TOP_K
    e_flat = expert_id.reshape(A)
    tok_flat = jnp.repeat(jnp.arange(M, dtype=jnp.int32), TOP_K)
    order = jnp.argsort(e_flat)
    e_s, tok_s, w_s = e_flat[order], tok_flat[order], gate.reshape(A)[order]
    counts = jnp.bincount(e_flat, length=N_EXPERTS)
    start = jnp.cumsum(counts) - counts
    padded = (counts + MOE_BLOCK - 1) // MOE_BLOCK * MOE_BLOCK
    p_end = jnp.cumsum(padded)
    dest = (p_end - padded)[e_s] + jnp.arange(A) - start[e_s]
    R = (-(-A // MOE_BLOCK) + N_EXPERTS) * MOE_BLOCK
    n_blk = R // MOE_BLOCK
    row_tok = jnp.zeros((R,), jnp.int32).at[dest].set(tok_s)
    row_w = jnp.zeros((R,), jnp.float32).at[dest].set(w_s)
    blk_exp = jnp.minimum(jnp.searchsorted(p_end, jnp.arange(n_blk) * MOE_BLOCK, side='right'), N_EXPERTS - 1)

    def expert_block(args):
        toks, e = args
        xb = x[toks]
        hb = jax.nn.silu(xb @ w1[e]) * (xb @ w3[e])
        return hb @ w2[e]

    y = lax.map(expert_block, (row_tok.reshape(n_blk, MOE_BLOCK), blk_exp))
    y = y.reshape(R, D).astype(jnp.float32) * row_w[:, None]
    out = jnp.zeros((M, D), jnp.float32).at[row_tok].add(y)
    return out.astype(n.dtype).reshape(B, T, D)


def setup_inputs(seed: int = 0) -> dict:
    key = jax.random.key(seed)
    ks = jax.random.split(key, 17)
    nrm = jax.random.normal
    f = jnp.float32
    return {
        'x': nrm(ks[0], (BATCH, SEQ, D_MODEL), f),
        'norm1_g': 1.0 + 0.02 * nrm(ks[1], (DEPTH, D_MODEL), f),
        'w_in': nrm(ks[2], (DEPTH, D_MODEL, IN_COLS), f) * D_MODEL ** -0.5,
        'gla_gate_w2': nrm(ks[3], (DEPTH, GLA_LOWRANK, GLA_QK), f) * GLA_LOWRANK ** -0.5,
        'gla_gate_b': 0.1 * nrm(ks[4], (DEPTH, GLA_QK), f),
        'gla_norm_g': 1.0 + 0.02 * nrm(ks[5], (DEPTH, GLA_DV), f),
        'w_out': nrm(ks[6], (DEPTH, D_MIX, D_MODEL), f) * D_MIX ** -0.5,
        'norm2_g': 1.0 + 0.02 * nrm(ks[7], (DEPTH, D_MODEL), f),
        'router_group_w': nrm(ks[8], (DEPTH, D_MODEL, N_GROUPS), f) * D_MODEL ** -0.5,
        'router_group_b': 0.01 * nrm(ks[9], (DEPTH, N_GROUPS), f),
        'router_expert_w': nrm(ks[10], (DEPTH, D_MODEL, N_EXPERTS), f) * D_MODEL ** -0.5,
        'router_expert_b': 0.01 * nrm(ks[11], (DEPTH, N_EXPERTS), f),
        'expert_w1': nrm(ks[12], (DEPTH, N_EXPERTS, D_MODEL, D_FF_EXPERT), f) * D_MODEL ** -0.5,
        'expert_w3': nrm(ks[13], (DEPTH, N_EXPERTS, D_MODEL, D_FF_EXPERT), f) * D_MODEL ** -0.5,
        'expert_w2': nrm(ks[14], (DEPTH, N_EXPERTS, D_FF_EXPERT, D_MODEL), f) * D_FF_EXPERT ** -0.5,
        'final_norm_g': 1.0 + 0.02 * nrm(ks[15], (D_MODEL,), f),
    }


def reference(x, norm1_g, w_in, gla_gate_w2, gla_gate_b, gla_norm_g, w_out, norm2_g,
              router_group_w, router_group_b, router_expert_w, router_expert_b,
              expert_w1, expert_w3, expert_w2, final_norm_g):
    h = x
    for l in range(DEPTH):
        h = h + hybrid_mixer(rmsnorm(h, norm1_g[l]), w_in[l], gla_gate_w2[l], gla_gate_b[l],
                             gla_norm_g[l], w_out[l])
        h = h + hier_moe(rmsnorm(h, norm2_g[l]), router_group_w[l], router_group_b[l],
                         router_expert_w[l], router_expert_b[l],
                         expert_w1[l], expert_w3[l], expert_w2[l])
    return rmsnorm(h, final_norm_g)
```

```python
import numpy as np
import ml_dtypes
from contextlib import ExitStack
import concourse.bass as bass
import concourse.mybir as mybir
from concourse.bass_utils import run_bass_kernel_spmd

F32 = mybir.dt.float32
BF16 = mybir.dt.bfloat16
I32 = mybir.dt.int32
AF = mybir.ActivationFunctionType
ALU = mybir.AluOpType
AX = mybir.AxisListType

T = 8192
D = 1024
NSB = 16
NSUP = 4
CAP = 256
NE = 32
EPS = 1e-6
BIG = 1.0e30
RING = 6144
SEM_LIMIT = 12000
import os
ATT_MODE = int(os.environ.get('ATT_MODE', '3'))
MASK_ENG = os.environ.get('MASK_ENG', 'V')
ATT_NOTR = int(os.environ.get('ATT_NOTR', '0'))
ATT_DS = tuple(int(v) for v in os.environ.get('ATT_DS', '1,4,16').split(','))


class Buf:
    __slots__ = ("name", "w", "r", "psum")

    def __init__(self, name, psum=False):
        self.name = name
        self.w = {}
        self.r = {}
        self.psum = psum


class Tile:
    def __init__(self, t, name, buf=None):
        self.t = t
        self.b = buf if buf is not None else Buf(name)


def _split_psum(R, W):
    R2, W2 = [], list(W)
    for x in R:
        b = x.b if isinstance(x, Tile) else x
        if b.psum:
            W2.append(x)
        else:
            R2.append(x)
    return R2, W2


class SemSlot:
    def __init__(self, sem):
        self.s = sem
        self.v = 0


class Eng:
    def __init__(self, prog, name, is_pe=False):
        self.prog = prog
        self.name = name
        self.is_pe = is_pe
        self.sem = prog.new_sem()
        self.cnt = 0
        self.known = {}
        self.thunks = []

    def _wait(self, sem, val):
        if self.known.get(id(sem), 0) >= val:
            return
        self.known[id(sem)] = val
        self.thunks.append(lambda e, s=sem, v=val: e.wait_ge(s, v))

    def _deps(self, R, W):
        for x in R:
            b = x.b if isinstance(x, Tile) else x
            for (sem, val) in b.w.values():
                if sem is self.sem and self.is_pe:
                    continue
                self._wait(sem, val)
        for x in W:
            b = x.b if isinstance(x, Tile) else x
            for (sem, val) in list(b.w.values()) + list(b.r.values()):
                if sem is self.sem and self.is_pe:
                    continue
                self._wait(sem, val)

    def _record(self, R, W, sem, val):
        for x in R:
            b = x.b if isinstance(x, Tile) else x
            b.r[id(sem)] = (sem, val)
        for x in W:
            b = x.b if isinstance(x, Tile) else x
            b.w = {id(sem): (sem, val)}
            b.r = {}

    def op(self, fn, R=(), W=()):
        R, W = _split_psum(R, W)
        self._deps(R, W)
        if self.cnt >= SEM_LIMIT:
            self.sem = self.prog.new_sem()
            self.cnt = 0
        self.cnt += 1
        sem, val = self.sem, self.cnt
        self.thunks.append(lambda e, f=fn, s=sem: f(e).then_inc(s, 1))
        self._record(R, W, sem, val)

    def dma(self, fn, slot, R=(), W=()):
        self._deps(R, W)
        slot.v += 16
        sem, val = slot.s, slot.v
        self.thunks.append(lambda e, f=fn, s=sem: f(e).then_inc(s, 16))
        self._record(R, W, sem, val)

    def coll(self, fn, R=(), W=()):
        self._deps(R, W)
        sem = self.prog.new_sem()
        self.thunks.append(lambda e, f=fn, s=sem: f(e).then_inc(s))
        self._record(R, W, sem, 1)

    def wait_all(self, bufs):
        self._deps(bufs, ())


class Prog:
    def __init__(self, nc, es):
        self.nc = nc
        self.es = es
        self.es_sem = es
        self.nsem = 0
        self.V = Eng(self, "vector")
        self.A = Eng(self, "scalar")
        self.G = Eng(self, "gpsimd")
        self.P = Eng(self, "tensor", is_pe=True)
        self.S = Eng(self, "sync")

    def new_sem(self):
        self.nsem += 1
        return self.es_sem.enter_context(self.nc.semaphore("s%d" % self.nsem))

    def slot(self):
        return SemSlot(self.new_sem())

    def sb(self, name, shape, dt):
        return Tile(self.es.enter_context(self.nc.sbuf_tensor(name, shape, dt)), name)

    def ps(self, name, shape, dt):
        return Tile(self.es.enter_context(self.nc.psum_tensor(name, shape, dt)), name)

    def emit(self):
        with self.nc.Block() as block:
            @block.vector
            def _(e):
                for th in self.V.thunks:
                    th(e)

            @block.scalar
            def _(e):
                for th in self.A.thunks:
                    th(e)

            @block.gpsimd
            def _(e):
                for th in self.G.thunks:
                    th(e)

            @block.tensor
            def _(e):
                for th in self.P.thunks:
                    th(e)

            @block.sync
            def _(e):
                for th in self.S.thunks:
                    th(e)


def build(debug=False, phase2=True, do_coll=True, nsb_run=NSB, do_gla=True, do_att=True):
    nc = bass.Bass("TRN2", target_bir_lowering=False)
    dram = lambda n, s, dt, k="ExternalInput": nc.dram_tensor(n, s, dt, kind=k).ap()
    x = dram("x", [T, D], F32)
    xres = dram("xres", [2048, D], F32)
    w_in = dram("w_in", [D, 848], F32)
    g1 = dram("g1", [128, 8], F32)
    w2aug = dram("w2aug", [17, 64], F32)
    gng = dram("gng", [128, 512], F32)
    w_out = dram("w_out", [D, D], F32)
    g2r = dram("g2r", [128, D], F32)
    gfr = dram("gfr", [128, D], F32)
    wr = dram("wr", [D, 36], F32)
    brr = dram("brr", [128, 36], F32)
    NEd = NE if phase2 else 1
    w1 = dram("w1", [NEd, D, 512], F32)
    w3 = dram("w3", [NEd, D, 512], F32)
    w2 = dram("w2", [NEd, 512, D], F32)
    cf = dram("cf", [128, 544], F32)
    cb = dram("cb", [128, 640], BF16)
    out = dram("out", [2048, D], F32, "ExternalOutput")
    a_int = [dram("a_int%d" % s, [256, 2048], BF16, "Internal") for s in range(NSUP)]
    b_all = dram("b_all", [NSUP * 1024, 2048], BF16, "Internal")
    rowidx = dram("rowidx", [128, 8], I32)
    w1b = dram("w1b", [NEd, D, 512], BF16, "Internal")
    w3b = dram("w3b", [NEd, D, 512], BF16, "Internal")
    w2b = dram("w2b", [NEd, 512, D], BF16, "Internal")
    wb_buf = [Buf("wb%d" % e) for e in range(NE)]
    Xs = dram("xs_scr", [NE * CAP, D], BF16, "Internal")
    Ys = dram("ys_scr", [NE * CAP, D], F32, "Internal")
    if debug:
        dbg_o = dram("dbg_o", [NSUP * 1024, 2048], BF16, "ExternalOutput")
        dbg_h = dram("dbg_h", [2048, D], F32, "ExternalOutput")
    a_buf = [Buf("a%d" % s) for s in range(NSUP)]
    b_buf = [Buf("b%d" % s) for s in range(NSUP)]
    Xs_buf = Buf("Xs")
    Ys_buf = [Buf("Ys%d" % e) for e in range(NE)]
    out_buf = Buf("out")

    with ExitStack() as es0:
        pg = Prog(nc, es0)
        V, A, G, P, S = pg.V, pg.A, pg.G, pg.P, pg.S
        cslot = pg.slot()
        cslot_g = pg.slot()
        banks = [es0.enter_context(nc.psum_tensor("bank%d" % i, [128, 512], F32)) for i in range(8)]
        bank_buf = [Buf("bank%d" % i, psum=True) for i in range(8)]

        def carve(name, bank, c0, ncol, dt=F32, pat=None, parts=128, **kw):
            ap = banks[bank][0:parts, c0:c0 + ncol]
            if dt is not F32:
                ap = ap.bitcast(dt)
            if pat is not None:
                ap = ap.rearrange(pat, **kw)
            return Tile(ap, name, bank_buf[bank])

        cfT = pg.sb("cfT", [128, 544], F32)
        cbT = pg.sb("cbT", [128, 640], BF16)
        S.dma(lambda e: e.dma_start(out=cfT.t[:], in_=cf), cslot, W=[cfT])
        S.dma(lambda e: e.dma_start(out=cbT.t[:], in_=cb), cslot, W=[cbT])
        epsT = pg.sb("epsT", [128, 2], F32)
        V.op(lambda e: e.memset(epsT.t[:, 0:1], EPS), W=[epsT])
        V.op(lambda e: e.memset(epsT.t[:, 1:2], 64 * EPS), W=[epsT])
        identF = cfT.t[:, 0:128]
        triU = cfT.t[:, 128:256]
        strictT = cfT.t[:, 256:384]
        onesF = cfT.t[:, 384:512]
        ecol = cfT.t[:, 512:544]
        identB = cbT.t[:, 0:128]
        mask4 = cbT.t[:, 128:640]

        with ExitStack() as es1:
            pg.es = es1
            NX = 6
            Wt = pg.sb("Wt", [128, 8, 848], BF16)
            g1T = pg.sb("g1T", [128, 8], F32)
            w2a = pg.sb("w2a", [17, 64], F32)
            gngT = pg.sb("gngT", [128, 4, 128], F32)
            xt = [pg.sb("xt%d" % i, [128, D], F32) for i in range(NX)]
            xsl = [pg.slot() for _ in range(NX)]
            junk = pg.sb("junk", [128, D], BF16)
            ss1 = pg.sb("ss1", [128, 64], F32)
            rs1 = pg.sb("rs1", [128, 64], F32)
            xs = [pg.sb("xs%d" % i, [128, D], BF16) for i in range(2)]
            nT = [pg.sb("nT%d" % i, [128, 8, 512], BF16) for i in range(2)]
            qT = [pg.sb("qT%d" % i, [64, 512], BF16) for i in range(2)]
            kT = [pg.sb("kT%d" % i, [64, 512], BF16) for i in range(2)]
            QT = [[pg.sb("QT%d_%d" % (h, i), [64, 2048], BF16) for i in range(2)] for h in range(2)]
            KT = [[pg.sb("KT%d_%d" % (h, i), [64, 2048], BF16) for i in range(3)] for h in range(2)]
            VT = [[pg.sb("VT%d_%d" % (h, i), [64, 2048], BF16) for i in range(3)] for h in range(2)]
            tm = [pg.sb("tm%d" % i, [128, 4, 320], BF16) for i in range(2)]
            glrT = [pg.sb("glrT%d" % i, [32, 512], F32) for i in range(2)]
            e1 = pg.sb("e1", [128, 256], F32)
            sp = pg.sb("sp", [128, 256], F32)
            Ek = pg.sb("Ek", [128, 256], F32)
            ktl = pg.sb("ktl", [128, 4, 64], BF16)
            EqT = pg.sb("EqT", [64, 512], F32)
            EkT = pg.sb("EkT", [64, 512], F32)
            qtl = pg.sb("qtl", [64, 512], BF16)
            ktlT = pg.sb("ktlT", [64, 512], BF16)
            gg = pg.sb("gg", [128, 4, 128], F32)
            AmT = [pg.sb("AmT%d" % i, [128, 128], BF16) for i in range(2)]
            og = [pg.sb("og%d" % i, [128, 128], BF16) for i in range(2)]
            Sst = pg.sb("Sst", [64, 128], F32)
            Sbf = pg.sb("Sbf", [64, 128], BF16)
            Stmp = pg.sb("Stmp", [64, 128], F32)
            ssg = pg.sb("ssg", [128, 64], F32)
            rsg = pg.sb("rsg", [128, 64], F32)
            junk2 = pg.sb("junk2", [128, 128], BF16)
            ogT = [pg.sb("ogT%d" % i, [128, 2048], BF16) for i in range(2)]
            pTa = [pg.sb("pTa%d" % i, [128, 512], BF16) for i in range(3)]
            vp = [pg.sb("vp%d" % i, [128, 4, 128], BF16) for i in range(3)]
            Oacc = pg.sb("Oacc", [128, 2, 2048], F32)
            rden = pg.sb("rden", [64, 2048], F32)
            oTa = [pg.sb("oTa%d" % i, [128, 2048], BF16) for i in range(2)]
            stsl = [pg.slot() for _ in range(2)]
            ps_pT = carve("ps_pT", 0, 0, 512, BF16, "p (a b) -> p a b", a=8)
            ps_pr = [carve("ps_pr%d" % i, 1 + i, 0, 512) for i in range(2)]
            ps_GT = carve("ps_GT", 3, 0, 512, parts=64)
            ps_s = carve("ps_s", 4, 0, 512)
            ps_z = carve("ps_z", 5, 0, 256)
            ps_G = Tile(banks[5][:, 256:512], "ps_G", bank_buf[5])
            ps_o = carve("ps_o", 7, 0, 256, F32, "p (a b) -> p a b", a=2)
            ps_v = Tile(banks[7][:, 256:384].bitcast(BF16).rearrange("p (a b) -> p a b", a=4), "ps_v", bank_buf[7])
            ps_A = Tile(banks[6][:, 0:128], "ps_A", bank_buf[6])
            ps_og = Tile(banks[6][:, 128:256], "ps_og", bank_buf[6])
            ps_U = Tile(banks[6][0:64, 256:384], "ps_U", bank_buf[6])
            ps_ogT = Tile(banks[6][:, 384:448].bitcast(BF16), "ps_ogT", bank_buf[6])

            for kc in range(8):
                G.dma(lambda e, kc=kc: e.dma_start(out=Wt.t[:, kc, :], in_=w_in[kc * 128:(kc + 1) * 128, :]),
                      cslot_g, W=[Wt])
            S.dma(lambda e: e.dma_start(out=g1T.t[:], in_=g1), cslot, W=[g1T])
            S.dma(lambda e: e.dma_start(out=w2a.t[:], in_=w2aug), cslot, W=[w2a])
            S.dma(lambda e: e.dma_start(out=gngT.t[:].rearrange("p a b -> p (a b)"), in_=gng), cslot, W=[gngT])
            for kc in range(8):
                V.op(lambda e, kc=kc: e.tensor_scalar(out=Wt.t[:, kc, :], in0=Wt.t[:, kc, :], scalar1=g1T.t[:, kc:kc + 1],
                                                      scalar2=None, op0=ALU.mult), R=[g1T, Wt], W=[Wt])
            V.op(lambda e: e.memset(ss1.t[:], 0.0), W=[ss1])
            V.op(lambda e: e.memset(ssg.t[:], 0.0), W=[ssg])
            V.op(lambda e: e.memset(Sst.t[:], 0.0), W=[Sst])
            V.op(lambda e: e.memset(Sbf.t[:], 0.0), W=[Sbf])
            for i in range(2):
                G.op(lambda e, i=i: e.memset(glrT[i].t[:], 1.0), W=[glrT[i]])
            for i in range(3):
                G.op(lambda e, i=i: e.memset(vp[i].t[:], 1.0), W=[vp[i]])

            pr_i = [0]

            def next_pr():
                pr_i[0] ^= 1
                return ps_pr[pr_i[0]]

            def stage1(sb):
                par = sb % 2
                sup = sb // 4
                for i in range(4):
                    ti = sb * 4 + i
                    sl = ti % NX
                    S.dma(lambda e, ti=ti, sl=sl: e.dma_start(out=xt[sl].t[:], in_=x[ti * 128:(ti + 1) * 128, :]),
                          xsl[sl], W=[xt[sl]])
                    A.op(lambda e, ti=ti, sl=sl: e.activation(out=junk.t[:], in_=xt[sl].t[:], func=AF.Square,
                                                              accum_out=ss1.t[:, ti:ti + 1]),
                         R=[xt[sl]], W=[junk, ss1])
                    A.op(lambda e, ti=ti: e.activation(out=rs1.t[:, ti:ti + 1], in_=ss1.t[:, ti:ti + 1], func=AF.Sqrt, scale=1.0 / D, bias=epsT.t[:, 0:1]),
                         R=[ss1, epsT], W=[rs1])
                    V.op(lambda e, ti=ti: e.reciprocal(out=rs1.t[:, ti:ti + 1], in_=rs1.t[:, ti:ti + 1]), R=[rs1], W=[rs1])
                    xp = ti % 2
                    V.op(lambda e, ti=ti, sl=sl, xp=xp: e.tensor_scalar(out=xs[xp].t[:], in0=xt[sl].t[:],
                                                                        scalar1=rs1.t[:, ti:ti + 1], scalar2=None, op0=ALU.mult),
                         R=[xt[sl], rs1], W=[xs[xp]])
                    for kc in range(8):
                        P.op(lambda e, kc=kc, xp=xp: e.transpose(out=ps_pT.t[:, kc, :], in_=xs[xp].t[:, kc * 128:(kc + 1) * 128],
                                                                 identity=identB), R=[xs[xp], cbT], W=[ps_pT])
                    A.op(lambda e, i=i, par=par: e.activation(out=nT[par].t[:, :, i * 128:(i + 1) * 128], in_=ps_pT.t[:, :, :],
                                                              func=AF.Copy), R=[ps_pT], W=[nT[par]])
                    yield
                col = (sb % 4) * 512
                kslot = sup % 3
                fm = [
                    (0, 64, qT[par], qT[par].t[0:64, :]),
                    (64, 64, kT[par], kT[par].t[0:64, :]),
                    (128, 64, QT[0][sup % 2], QT[0][sup % 2].t[:, col:col + 512]),
                    (192, 64, QT[1][sup % 2], QT[1][sup % 2].t[:, col:col + 512]),
                    (256, 64, KT[0][kslot], KT[0][kslot].t[:, col:col + 512]),
                    (320, 64, KT[1][kslot], KT[1][kslot].t[:, col:col + 512]),
                    (384, 64, VT[0][kslot], VT[0][kslot].t[:, col:col + 512]),
                    (448, 64, VT[1][kslot], VT[1][kslot].t[:, col:col + 512]),
                    (512, 16, glrT[par], glrT[par].t[0:16, :]),
                ]
                for gi, (c0, m, dtile, dap) in enumerate(fm):
                    pp = next_pr()
                    for kc in range(8):
                        P.op(lambda e, kc=kc, c0=c0, m=m, pp=pp, par=par: e.matmul(
                            out=pp.t[0:m, :], lhsT=Wt.t[:, kc, c0:c0 + m], rhs=nT[par].t[:, kc, :],
                            start=(kc == 0), stop=(kc == 7)), R=[Wt, nT[par]], W=[pp])
                    if gi % 2 == 0:
                        V.op(lambda e, pp=pp, m=m, dap=dap: e.tensor_copy(out=dap, in_=pp.t[0:m, :]), R=[pp], W=[dtile])
                    else:
                        A.op(lambda e, pp=pp, m=m, dap=dap: e.activation(out=dap, in_=pp.t[0:m, :], func=AF.Copy),
                             R=[pp], W=[dtile])
                    yield
                for i in range(4):
                    pp = next_pr()
                    for kc in range(8):
                        P.op(lambda e, kc=kc, i=i, pp=pp, par=par: e.matmul(
                            out=pp.t[:, 0:320], lhsT=nT[par].t[:, kc, i * 128:(i + 1) * 128], rhs=Wt.t[:, kc, 528:848],
                            start=(kc == 0), stop=(kc == 7)), R=[Wt, nT[par]], W=[pp])
                    V.op(lambda e, pp=pp, i=i, par=par: e.tensor_copy(out=tm[par].t[:, i, 0:192], in_=pp.t[:, 0:192]),
                         R=[pp], W=[tm[par]])
                    A.op(lambda e, pp=pp, i=i, par=par: e.activation(out=tm[par].t[:, i, 192:320], in_=pp.t[:, 192:320],
                                                                     func=AF.Silu), R=[pp], W=[tm[par]])
                    yield

            def gla(sb):
                par = sb % 2
                sup = sb // 4
                for i in range(4):
                    P.op(lambda e, i=i, par=par: e.matmul(out=ps_z.t[:, i * 64:(i + 1) * 64], lhsT=glrT[par].t[0:17, i * 128:(i + 1) * 128],
                                                          rhs=w2a.t[0:17, :], start=True, stop=True),
                         R=[glrT[par], w2a], W=[ps_z])
                A.op(lambda e: e.activation(out=e1.t[:], in_=ps_z.t[:], func=AF.Exp, scale=-1.0), R=[ps_z], W=[e1])
                A.op(lambda e: e.activation(out=sp.t[:], in_=e1.t[:], func=AF.Ln, bias=1.0), R=[e1], W=[sp])
                yield
                P.op(lambda e: e.matmul(out=ps_G.t[:], lhsT=triU, rhs=sp.t[:], start=True, stop=True), R=[sp, cfT], W=[ps_G])
                for i in range(4):
                    P.op(lambda e, i=i: e.matmul(out=ps_GT.t[:, i * 128:(i + 1) * 128], lhsT=sp.t[:, i * 64:(i + 1) * 64], rhs=triU,
                                                 start=True, stop=True), R=[sp, cfT], W=[ps_GT])
                A.op(lambda e: e.activation(out=Ek.t[:], in_=ps_G.t[:], func=AF.Exp, scale=1.0 / 16), R=[ps_G], W=[Ek])
                A.op(lambda e: e.activation(out=EqT.t[:], in_=ps_GT.t[:], func=AF.Exp, scale=-1.0 / 16), R=[ps_GT], W=[EqT])
                A.op(lambda e: e.activation(out=EkT.t[:], in_=ps_GT.t[:], func=AF.Exp, scale=1.0 / 16), R=[ps_GT], W=[EkT])
                yield
                V.op(lambda e, par=par: e.tensor_tensor(out=ktl.t[:, :, :], in0=tm[par].t[:, :, 0:64],
                                                        in1=Ek.t[:].rearrange("p (a b) -> p a b", a=4), op=ALU.mult),
                     R=[tm[par], Ek], W=[ktl])
                V.op(lambda e, par=par: e.tensor_tensor(out=qtl.t[:], in0=qT[par].t[:], in1=EqT.t[:], op=ALU.mult),
                     R=[qT[par], EqT], W=[qtl])
                V.op(lambda e, par=par: e.tensor_tensor(out=ktlT.t[:], in0=kT[par].t[:], in1=EkT.t[:], op=ALU.mult),
                     R=[kT[par], EkT], W=[ktlT])
                G.op(lambda e, par=par: e.tensor_tensor(out=gg.t[:], in0=tm[par].t[:, :, 192:320], in1=gngT.t[:], op=ALU.mult),
                     R=[tm[par], gngT], W=[gg])
                yield
                for i in range(4):
                    ch = sb * 4 + i
                    cp = ch % 2
                    cs = slice(i * 128, (i + 1) * 128)
                    vap = tm[par].t[:, i, 64:192]
                    P.op(lambda e, cs=cs: e.matmul(out=ps_A.t[:], lhsT=ktlT.t[:, cs], rhs=qtl.t[:, cs], start=True, stop=True),
                         R=[ktlT, qtl], W=[ps_A])
                    V.op(lambda e, cp=cp: e.tensor_tensor(out=AmT[cp].t[:], in0=ps_A.t[:], in1=triU, op=ALU.mult),
                         R=[ps_A, cfT], W=[AmT[cp]])
                    P.op(lambda e, cp=cp, vap=vap: e.matmul(out=ps_og.t[:], lhsT=AmT[cp].t[:], rhs=vap, start=True, stop=False),
                         R=[AmT[cp], tm[par]], W=[ps_og])
                    P.op(lambda e, cs=cs: e.matmul(out=ps_og.t[:], lhsT=qtl.t[:, cs], rhs=Sbf.t[:], start=False, stop=True),
                         R=[qtl, Sbf], W=[ps_og])
                    P.op(lambda e, i=i, vap=vap: e.matmul(out=ps_U.t[:], lhsT=ktl.t[:, i, :], rhs=vap, start=True, stop=True),
                         R=[ktl, tm[par]], W=[ps_U])
                    yield
                    acol = EqT.t[:, i * 128 + 127:i * 128 + 128]
                    V.op(lambda e: e.tensor_tensor(out=Stmp.t[:], in0=ps_U.t[:], in1=Sst.t[:], op=ALU.add),
                         R=[ps_U, Sst], W=[Stmp])
                    V.op(lambda e, acol=acol: e.tensor_scalar(out=Sst.t[:], in0=Stmp.t[:], scalar1=acol, scalar2=None, op0=ALU.mult),
                         R=[Stmp, EqT], W=[Sst])
                    A.op(lambda e, acol=acol: e.activation(out=Sbf.t[:], in_=Stmp.t[:], func=AF.Copy, scale=acol),
                         R=[Stmp, EqT], W=[Sbf])
                    yield
                    A.op(lambda e, ch=ch: e.activation(out=junk2.t[:], in_=ps_og.t[:], func=AF.Square, accum_out=ssg.t[:, ch:ch + 1]),
                         R=[ps_og], W=[junk2, ssg])
                    A.op(lambda e, ch=ch: e.activation(out=rsg.t[:, ch:ch + 1], in_=ssg.t[:, ch:ch + 1], func=AF.Sqrt, scale=1.0 / 128, bias=epsT.t[:, 1:2]),
                         R=[ssg, epsT], W=[rsg])
                    V.op(lambda e, ch=ch: e.reciprocal(out=rsg.t[:, ch:ch + 1], in_=rsg.t[:, ch:ch + 1]), R=[rsg], W=[rsg])
                    V.op(lambda e, ch=ch, cp=cp, i=i: e.scalar_tensor_tensor(out=og[cp].t[:], in0=ps_og.t[:], scalar=rsg.t[:, ch:ch + 1],
                                                                             in1=gg.t[:, i, :], op0=ALU.mult, op1=ALU.mult),
                         R=[ps_og, rsg, gg], W=[og[cp]])
                    P.op(lambda e, cp=cp: e.transpose(out=ps_ogT.t[:], in_=og[cp].t[:], identity=identB), R=[og[cp], cbT], W=[ps_ogT])
                    cc = (ch % 16) * 128
                    A.op(lambda e, cc=cc, sup=sup: e.activation(out=ogT[sup % 2].t[:, cc:cc + 128], in_=ps_ogT.t[:], func=AF.Copy),
                         R=[ps_ogT], W=[ogT[sup % 2]])
                    yield

            def attention(sup):
                qp = sup % 2
                units = []
                for d in ATT_DS:
                    for r in range(d):
                        for n in range(16 // d):
                            units.append((d, r, n))
                ui = [0]

                def kcols(t0, d):
                    slot = (t0 // 2048) % 3
                    c0 = t0 % 2048
                    return slot, slice(c0, c0 + 127 * d + 1, d)

                def scores(u):
                    d, r, n = units[u]
                    sl3 = u % 3
                    qb = r + d * 128 * n
                    qsl = slice(qb, qb + 127 * d + 1, d)
                    t0 = sup * 2048 + qb
                    has_prev = (t0 - 128 * d) >= 0
                    cur = kcols(t0, d)
                    prev = kcols(t0 - 128 * d, d) if has_prev else cur
                    for h in range(2):
                        hs = slice(h * 64, (h + 1) * 64)
                        for blk, (ks, kc) in enumerate((prev, cur)):
                            P.op(lambda e, h=h, hs=hs, blk=blk, ks=ks, kc=kc, qsl=qsl: e.matmul(
                                out=ps_s.t[:, (h * 2 + blk) * 128:(h * 2 + blk + 1) * 128], lhsT=KT[h][ks].t[:, kc],
                                rhs=QT[h][qp].t[:, qsl], start=True, stop=True), R=[KT[h][ks], QT[h][qp]], W=[ps_s])
                            if not ATT_NOTR:
                              P.op(lambda e, h=h, hs=hs, blk=blk, ks=ks, kc=kc: e.transpose(
                                out=ps_v.t[:, h * 2 + blk, :], in_=VT[h][ks].t[:, kc], identity=cbT.t[0:64, 0:64]),
                                R=[VT[h][ks], cbT], W=[ps_v])
                    A.op(lambda e, sl3=sl3: e.activation(out=pTa[sl3].t[:], in_=ps_s.t[:], func=AF.Exp, scale=0.125),
                         R=[ps_s], W=[pTa[sl3]])
                    (G if MASK_ENG == 'G' else V).op(lambda e, sl3=sl3: e.tensor_tensor(out=pTa[sl3].t[:], in0=pTa[sl3].t[:], in1=mask4, op=ALU.mult),
                         R=[pTa[sl3], cbT], W=[pTa[sl3]])
                    V.op(lambda e, sl3=sl3: e.tensor_copy(out=vp[sl3].t[:, :, 0:64], in_=ps_v.t[:, :, :]), R=[ps_v], W=[vp[sl3]])
                    return has_prev

                def pv(u, has_prev):
                    d, r, n = units[u]
                    sl3 = u % 3
                    qb = r + d * 128 * n
                    qsl = slice(qb, qb + 127 * d + 1, d)
                    blks = (0, 1) if has_prev else (1,)
                    for h in range(2):
                        for bi, blk in enumerate(blks):
                            P.op(lambda e, h=h, blk=blk, bi=bi, sl3=sl3, nb=len(blks): e.matmul(
                                out=ps_o.t[:, h, :], lhsT=vp[sl3].t[:, h * 2 + blk, :],
                                rhs=pTa[sl3].t[:, (h * 2 + blk) * 128:(h * 2 + blk + 1) * 128],
                                start=(bi == 0), stop=(bi == nb - 1)), R=[vp[sl3], pTa[sl3]], W=[ps_o])
                    if d == 1:
                        V.op(lambda e, qsl=qsl: e.tensor_copy(out=Oacc.t[:, :, qsl], in_=ps_o.t[:, :, :]), R=[ps_o], W=[Oacc])
                    else:
                        V.op(lambda e, qsl=qsl: e.tensor_tensor(out=Oacc.t[:, :, qsl], in0=ps_o.t[:, :, :], in1=Oacc.t[:, :, qsl],
                                                                op=ALU.add), R=[ps_o, Oacc], W=[Oacc])

                hp = {}
                for u in range(len(units) + 1):
                    if u < len(units):
                        hp[u] = scores(u)
                    if u >= 1 and ATT_MODE >= 2:
                        pv(u - 1, hp[u - 1])
                    yield
                for h in range(2 if ATT_MODE >= 3 else 0):
                    V.op(lambda e, h=h: e.reciprocal(out=rden.t[:], in_=Oacc.t[64:128, h, :]), R=[Oacc], W=[rden])
                    V.op(lambda e, h=h: e.tensor_tensor(out=oTa[qp].t[h * 64:(h + 1) * 64, :], in0=Oacc.t[0:64, h, :], in1=rden.t[:],
                                                        op=ALU.mult), R=[Oacc, rden], W=[oTa[qp]])
                    yield

            def exchange(sup):
                qp = sup % 2
                S.dma(lambda e: e.dma_start(out=a_int[sup][0:128, :], in_=ogT[qp].t[:]), stsl[qp], R=[ogT[qp]], W=[a_buf[sup]])
                S.dma(lambda e: e.dma_start(out=a_int[sup][128:256, :], in_=oTa[qp].t[:]), stsl[qp], R=[oTa[qp]], W=[a_buf[sup]])
                if do_coll:
                  G.coll(lambda e: e.collective_compute("AllGather", ALU.bypass, replica_groups=[[0, 1, 2, 3], [4, 5, 6, 7]],
                                                      ins=[a_int[sup]], outs=[b_all[sup * 1024:(sup + 1) * 1024, :]]), R=[a_buf[sup]], W=[b_buf[sup]])

            wcs = [pg.slot() for _ in range(4)]

            def precast(ex):
                for (src, dst) in ((w1, w1b), (w3, w3b), (w2, w2b)):
                    G.dma(lambda e, src=src, dst=dst, ex=ex: e.dma_start(
                        out=dst[ex].rearrange("(p r) f -> p (r f)", p=128), in_=src[ex].rearrange("(p r) f -> p (r f)", p=128)),
                        wcs[ex % 4], W=[wb_buf[ex]])

            DONE = object()
            att = [None, None]

            def att_step():
                if att[0] is not None and next(att[0], DONE) is DONE:
                    exchange(att[1])
                    att[0] = None

            for sb in range(nsb_run + 1):
                if phase2 and sb < NSB:
                    precast(2 * sb)
                    precast(2 * sb + 1)
                active = []
                if sb < nsb_run:
                    active.append(stage1(sb))
                if sb >= 1 and do_gla:
                    active.append(gla(sb - 1))
                while active:
                    for g in list(active):
                        if next(g, DONE) is DONE:
                            active.remove(g)
                    att_step()
                if sb >= 1 and (sb - 1) % 4 == 3 and do_att:
                    while att[0] is not None:
                        att_step()
                    att[0] = attention((sb - 1) // 4)
                    att[1] = (sb - 1) // 4
            while att[0] is not None:
                att_step()
            if debug and not (do_gla and do_att):
                dbg1 = Buf("dbg1")
                S.dma(lambda e: e.dma_start(out=dbg_o[0:64, :], in_=QT[0][0].t[:]), stsl[0], R=[QT[0][0]], W=[dbg1])
                S.dma(lambda e: e.dma_start(out=dbg_o[128:192, :], in_=KT[0][0].t[:]), stsl[0], R=[KT[0][0]], W=[dbg1])
                S.dma(lambda e: e.dma_start(out=dbg_o[256:384, :], in_=ogT[0].t[:]), stsl[0], R=[ogT[0]], W=[dbg1])
                S.dma(lambda e: e.dma_start(out=dbg_o[384:512, :], in_=oTa[0].t[:]), stsl[0], R=[oTa[0]], W=[dbg1])
                S.wait_all([dbg1])
            pg.es = es0
        dslot = pg.slot()
        if debug and do_coll:
            S.dma(lambda e: e.dma_start(out=dbg_o, in_=b_all), dslot, R=b_buf, W=[out_buf])
        if debug and not do_coll and do_gla and do_att:
            for sup in range(nsb_run // 4):
                S.dma(lambda e, sup=sup: e.dma_start(out=dbg_o[sup * 256:(sup + 1) * 256, :], in_=a_int[sup]), dslot, R=[a_buf[sup]], W=[out_buf])
        if phase2:
          with ExitStack() as es2:
            pg.es = es2
            hh = pg.sb("hh", [128, 16, D], F32)
            gfT = pg.sb("gfT", [128, D], F32)
            ss3 = pg.sb("ss3", [128, 16], F32)
            rs3 = pg.sb("rs3", [128, 16], F32)
            gates = pg.sb("gates", [128, 16, 2], F32)
            dstf = pg.sb("dstf", [128, 16, 2], F32)
            dsti = pg.sb("dsti", [128, 16, 2], I32)

            es2a = ExitStack()
            pg.es = es2a
            ridx = pg.sb("ridx", [128, 8], I32)
            oT = pg.sb("oT", [128, 8, 2048], BF16)
            Wo = pg.sb("Wo", [128, 8, D], BF16)
            g2T = pg.sb("g2T", [128, D], F32)
            wrT = pg.sb("wrT", [128, 8, 36], F32)
            brT = pg.sb("brT", [128, 36], F32)
            xr = [pg.sb("xr%d" % i, [128, D], F32) for i in range(2)]
            xrs = [pg.slot() for _ in range(2)]
            junk3 = pg.sb("junk3", [128, D], BF16)
            ss2 = pg.sb("ss2", [128, 16], F32)
            rs2 = pg.sb("rs2", [128, 16], F32)
            n2f = [pg.sb("n2f%d" % i, [128, D], F32) for i in range(2)]
            n2b = [pg.sb("n2b%d" % i, [128, D], BF16) for i in range(2)]
            n2s = [pg.slot() for _ in range(2)]
            n2T = pg.sb("n2T", [128, 8, 128], F32)
            lg = pg.sb("lg", [128, 36], F32)
            sm = pg.sb("sm", [128, 16], F32)
            gmask = pg.sb("gmask", [128, 4], F32)
            pen = pg.sb("pen", [128, 4], F32)
            gex = pg.sb("gex", [128, 4], F32)
            elm = pg.sb("elm", [128, 32], F32)
            elm2 = pg.sb("elm2", [128, 32], F32)
            mk1 = pg.sb("mk1", [128, 32], F32)
            mk2 = pg.sb("mk2", [128, 32], F32)
            cnt = pg.sb("cnt", [128, 32], F32)
            tot = pg.sb("tot", [128, 32], F32)
            pos = pg.sb("pos", [128, 32], F32)
            tmp32 = pg.sb("tmp32", [128, 32], F32)
            ps_h = [carve("ps_h%d" % i, i, 0, 512) for i in range(2)]
            ps_t = [carve("ps_t%d" % i, 2 + i, 0, 512, F32, "p (a b) -> p a b", a=4) for i in range(2)]
            ps_y = [carve("ps_y%d" % i, 4 + i, 0, 512) for i in range(2)]
            ps_a2 = [Tile(banks[0][:, 0:256], "ps_a0", bank_buf[0]), Tile(banks[2][:, 0:256], "ps_a1", bank_buf[2])]
            ps_b2 = [Tile(banks[1][:, 0:256], "ps_b0", bank_buf[1]), Tile(banks[3][:, 0:256], "ps_b1", bank_buf[3])]
            ps_x = Tile(banks[7][:, 0:256].bitcast(BF16).rearrange("p (a b) -> p a b", a=2), "ps_x", bank_buf[7])
            ps_l = Tile(banks[7][:, 256:292], "ps_l", bank_buf[7])
            ps_r = Tile(banks[7][:, 320:352], "ps_r", bank_buf[7])
            ps_c = Tile(banks[7][:, 352:384], "ps_c", bank_buf[7])

            S.dma(lambda e: e.dma_start(out=ridx.t[:], in_=rowidx), cslot, W=[ridx])
            S.dma(lambda e: e.dma_start(out=g2T.t[:], in_=g2r), cslot, W=[g2T])
            S.dma(lambda e: e.dma_start(out=gfT.t[:], in_=gfr), cslot, W=[gfT])
            S.dma(lambda e: e.dma_start(out=brT.t[:], in_=brr), cslot, W=[brT])
            S.dma(lambda e: e.dma_start(out=wrT.t[:], in_=wr.rearrange("(c p) n -> p c n", p=128)), cslot, W=[wrT])
            for kc in range(8):
                G.dma(lambda e, kc=kc: e.dma_start(out=Wo.t[:, kc, :], in_=w_out[kc * 128:(kc + 1) * 128, :]), cslot_g, W=[Wo])
            V.op(lambda e: e.memset(ss2.t[:], 0.0), W=[ss2])
            V.op(lambda e: e.memset(ss3.t[:], 0.0), W=[ss3])
            V.op(lambda e: e.memset(tot.t[:], 0.0), W=[tot])
            V.op(lambda e: e.memset(sm.t[:], 0.0), W=[sm])
            oslot = pg.slot()
            for c in range(8):
                G.dma(lambda e, c=c: e.indirect_dma_start(out=oT.t[:, c, :], out_offset=None, in_=b_all,
                                                          in_offset=bass.IndirectOffsetOnAxis(ap=ridx.t[:, c:c + 1], axis=0)),
                      oslot, R=b_buf + [ridx], W=[oT])
            wch = [(c // 2) + 4 * (c % 2) for c in range(8)]

            def rstd_ops(ssT, rsT, ti, n, eps):
                A.op(lambda e: e.activation(out=rsT.t[:, ti:ti + 1], in_=ssT.t[:, ti:ti + 1], func=AF.Sqrt, scale=1.0 / n, bias=epsT.t[:, 0:1]),
                     R=[ssT, epsT], W=[rsT])
                V.op(lambda e: e.reciprocal(out=rsT.t[:, ti:ti + 1], in_=rsT.t[:, ti:ti + 1]), R=[rsT], W=[rsT])

            for ti in range(16):
                p2 = ti % 2
                tsl = slice(ti * 128, (ti + 1) * 128)
                S.dma(lambda e, tsl=tsl, p2=p2: e.dma_start(out=xr[p2].t[:], in_=xres[tsl, :]), xrs[p2], W=[xr[p2]])
                for half in range(2):
                    for c in range(8):
                        P.op(lambda e, c=c, half=half, tsl=tsl: e.matmul(
                            out=ps_h[half].t[:], lhsT=oT.t[:, c, tsl], rhs=Wo.t[:, wch[c], half * 512:(half + 1) * 512],
                            start=(c == 0), stop=(c == 7)), R=[oT, Wo], W=[ps_h[half]])
                    V.op(lambda e, half=half, ti=ti, p2=p2: e.tensor_tensor(
                        out=hh.t[:, ti, half * 512:(half + 1) * 512], in0=ps_h[half].t[:], in1=xr[p2].t[:, half * 512:(half + 1) * 512],
                        op=ALU.add), R=[ps_h[half], xr[p2]], W=[hh])
                A.op(lambda e, ti=ti: e.activation(out=junk3.t[:], in_=hh.t[:, ti, :], func=AF.Square, accum_out=ss2.t[:, ti:ti + 1]),
                     R=[hh], W=[junk3, ss2])
                rstd_ops(ss2, rs2, ti, D, EPS)
                V.op(lambda e, ti=ti, p2=p2: e.scalar_tensor_tensor(out=n2f[p2].t[:], in0=hh.t[:, ti, :], scalar=rs2.t[:, ti:ti + 1],
                                                                    in1=g2T.t[:], op0=ALU.mult, op1=ALU.mult),
                     R=[hh, rs2, g2T], W=[n2f[p2]])
                A.op(lambda e, p2=p2: e.activation(out=n2b[p2].t[:], in_=n2f[p2].t[:], func=AF.Copy), R=[n2f[p2]], W=[n2b[p2]])
                for q in range(2):
                    for c4 in range(4):
                        c = q * 4 + c4
                        P.op(lambda e, c=c, c4=c4, q=q, p2=p2: e.transpose(out=ps_t[q].t[:, c4, :], in_=n2f[p2].t[:, c * 128:(c + 1) * 128],
                                                                           identity=identF), R=[n2f[p2], cfT], W=[ps_t[q]])
                    if q == 0:
                        A.op(lambda e: e.activation(out=n2T.t[:, 0:4, :], in_=ps_t[0].t[:], func=AF.Copy), R=[ps_t[0]], W=[n2T])
                    else:
                        V.op(lambda e: e.tensor_copy(out=n2T.t[:, 4:8, :], in_=ps_t[1].t[:]), R=[ps_t[1]], W=[n2T])
                for c in range(8):
                    P.op(lambda e, c=c: e.matmul(out=ps_l.t[:], lhsT=n2T.t[:, c, :], rhs=wrT.t[:, c, :], start=(c == 0), stop=(c == 7)),
                         R=[n2T, wrT], W=[ps_l])
                V.op(lambda e: e.tensor_tensor(out=lg.t[:], in0=ps_l.t[:], in1=brT.t[:], op=ALU.add), R=[ps_l, brT], W=[lg])
                gl = lg.t[:, 0:4]
                el = lg.t[:, 4:36]
                c_ = lambda k: sm.t[:, k:k + 1]
                V.op(lambda e: e.reduce_max(out=c_(0), in_=gl, axis=AX.X), R=[lg], W=[sm])
                V.op(lambda e: e.tensor_scalar(out=gmask.t[:], in0=gl, scalar1=c_(0), scalar2=None, op0=ALU.is_equal), R=[lg, sm], W=[gmask])
                V.op(lambda e: e.tensor_single_scalar(out=c_(1), in_=c_(0), scalar=-1.0, op=ALU.mult), R=[sm], W=[sm])
                V.op(lambda e: e.memset(c_(2), 0.0), W=[sm])
                A.op(lambda e: e.activation(out=gex.t[:], in_=gl, func=AF.Exp, bias=c_(1), accum_out=c_(2)), R=[lg, sm], W=[gex, sm])
                V.op(lambda e: e.reciprocal(out=c_(3), in_=c_(2)), R=[sm], W=[sm])
                V.op(lambda e: e.tensor_scalar(out=pen.t[:], in0=gmask.t[:], scalar1=-1.0, scalar2=BIG, op0=ALU.add, op1=ALU.mult),
                     R=[gmask], W=[pen])
                for g in range(4):
                    V.op(lambda e, g=g: e.tensor_scalar(out=elm.t[:, g * 8:(g + 1) * 8], in0=lg.t[:, 4 + g * 8:12 + g * 8],
                                                        scalar1=pen.t[:, g:g + 1], scalar2=None, op0=ALU.add), R=[lg, pen], W=[elm])
                V.op(lambda e: e.reduce_max(out=c_(4), in_=elm.t[:], axis=AX.X), R=[elm], W=[sm])
                V.op(lambda e: e.tensor_scalar(out=mk1.t[:], in0=elm.t[:], scalar1=c_(4), scalar2=None, op0=ALU.is_equal), R=[elm, sm], W=[mk1])
                V.op(lambda e: e.scalar_tensor_tensor(out=elm2.t[:], in0=mk1.t[:], scalar=-BIG, in1=elm.t[:], op0=ALU.mult, op1=ALU.add),
                     R=[mk1, elm], W=[elm2])
                V.op(lambda e: e.reduce_max(out=c_(5), in_=elm2.t[:], axis=AX.X), R=[elm2], W=[sm])
                V.op(lambda e: e.tensor_scalar(out=mk2.t[:], in0=elm2.t[:], scalar1=c_(5), scalar2=None, op0=ALU.is_equal), R=[elm2, sm], W=[mk2])
                V.op(lambda e: e.tensor_tensor(out=c_(6), in0=c_(5), in1=c_(4), op=ALU.subtract), R=[sm], W=[sm])
                A.op(lambda e: e.activation(out=c_(7), in_=c_(6), func=AF.Exp), R=[sm], W=[sm])
                V.op(lambda e: e.tensor_single_scalar(out=c_(8), in_=c_(7), scalar=1.0, op=ALU.add), R=[sm], W=[sm])
                V.op(lambda e: e.reciprocal(out=c_(9), in_=c_(8)), R=[sm], W=[sm])
                V.op(lambda e, ti=ti: e.tensor_tensor(out=gates.t[:, ti, 0:1], in0=c_(9), in1=c_(3), op=ALU.mult), R=[sm], W=[gates])
                V.op(lambda e, ti=ti: e.tensor_tensor(out=gates.t[:, ti, 1:2], in0=gates.t[:, ti, 0:1], in1=c_(7), op=ALU.mult),
                     R=[sm, gates], W=[gates])
                V.op(lambda e: e.tensor_tensor(out=cnt.t[:], in0=mk1.t[:], in1=mk2.t[:], op=ALU.add), R=[mk1, mk2], W=[cnt])
                P.op(lambda e: e.matmul(out=ps_r.t[:], lhsT=strictT, rhs=cnt.t[:], start=True, stop=True), R=[cnt, cfT], W=[ps_r])
                P.op(lambda e: e.matmul(out=ps_c.t[:], lhsT=onesF, rhs=cnt.t[:], start=True, stop=True), R=[cnt, cfT], W=[ps_c])
                V.op(lambda e: e.tensor_tensor(out=pos.t[:], in0=ps_r.t[:], in1=tot.t[:], op=ALU.add), R=[ps_r, tot], W=[pos])
                V.op(lambda e: e.tensor_tensor(out=tot.t[:], in0=ps_c.t[:], in1=tot.t[:], op=ALU.add), R=[ps_c, tot], W=[tot])
                V.op(lambda e: e.tensor_single_scalar(out=pos.t[:], in_=pos.t[:], scalar=float(CAP - 1), op=ALU.min), R=[pos], W=[pos])
                V.op(lambda e: e.tensor_tensor(out=pos.t[:], in0=pos.t[:], in1=ecol, op=ALU.add), R=[pos, cfT], W=[pos])
                for k, mk in enumerate((mk1, mk2)):
                    V.op(lambda e, mk=mk: e.tensor_tensor(out=tmp32.t[:], in0=pos.t[:], in1=mk.t[:], op=ALU.mult), R=[pos, mk], W=[tmp32])
                    V.op(lambda e, k=k, ti=ti: e.reduce_sum(out=dstf.t[:, ti, k:k + 1], in_=tmp32.t[:], axis=AX.X), R=[tmp32], W=[dstf])
                V.op(lambda e, ti=ti: e.tensor_copy(out=dsti.t[:, ti, :], in_=dstf.t[:, ti, :]), R=[dstf], W=[dsti])
                for k in range(2):
                    G.dma(lambda e, k=k, ti=ti, p2=p2: e.indirect_dma_start(
                        out=Xs, out_offset=bass.IndirectOffsetOnAxis(ap=dsti.t[:, ti, k:k + 1], axis=0), in_=n2b[p2].t[:, :], in_offset=None),
                        n2s[p2], R=[n2b[p2], dsti], W=[Xs_buf])
            if debug:
                S.dma(lambda e: e.dma_start(out=dbg_h.rearrange("(t p) d -> p t d", p=128), in_=hh.t[:]), dslot, R=[hh], W=[out_buf])

            es2a.close()
            es2b = ExitStack()
            pg.es = es2b
            junk4 = pg.sb("junk3b", [128, D], BF16)
            xg = [pg.sb("xg%d" % i, [128, 2, D], BF16) for i in range(2)]
            xgs = [pg.slot() for _ in range(2)]
            xT = [pg.sb("xT%d" % i, [128, 8, 256], BF16) for i in range(2)]
            w1s = [pg.sb("w1s%d" % i, [128, 8, 512], BF16) for i in range(2)]
            w3s = [pg.sb("w3s%d" % i, [128, 8, 512], BF16) for i in range(2)]
            w2s = [pg.sb("w2s%d" % i, [128, 4, D], BF16) for i in range(2)]
            wsl = [pg.slot() for _ in range(2)]
            sa = [pg.sb("sa%d" % i, [128, 256], F32) for i in range(2)]
            hT = [pg.sb("hT%d" % i, [128, 4, 256], BF16) for i in range(2)]
            ysb = [pg.sb("ysb%d" % i, [128, 2, D], F32) for i in range(1)]
            yss = [pg.slot() for _ in range(2)]
            ya = [pg.sb("ya%d" % i, [128, D], F32) for i in range(1)]
            yb = [pg.sb("yb%d" % i, [128, D], F32) for i in range(1)]
            ygs = [pg.slot() for _ in range(2)]
            ot = [pg.sb("ot%d" % i, [128, D], F32) for i in range(2)]
            ots = [pg.slot() for _ in range(2)]
            Xs_all = Buf("Xs_all")
            Xs_all.w = dict(Xs_buf.w)
            for p2 in range(2):
                Xs_all.w[id(n2s[p2].s)] = (n2s[p2].s, n2s[p2].v)
            def eloads(ex):
                p2 = ex % 2
                S.dma(lambda e, ex=ex, p2=p2: e.dma_start(out=xg[p2].t[:], in_=Xs[ex * CAP:(ex + 1) * CAP, :].rearrange("(s p) f -> p s f", p=128)),
                      xgs[p2], R=[Xs_all], W=[xg[p2]])
                S.dma(lambda e, ex=ex, p2=p2: e.dma_start(out=w1s[p2].t[:], in_=w1b[ex].rearrange("(c p) f -> p c f", p=128)), wsl[p2], R=[wb_buf[ex]], W=[w1s[p2]])
                S.dma(lambda e, ex=ex, p2=p2: e.dma_start(out=w3s[p2].t[:], in_=w3b[ex].rearrange("(c p) f -> p c f", p=128)), wsl[p2], R=[wb_buf[ex]], W=[w3s[p2]])
                S.dma(lambda e, ex=ex, p2=p2: e.dma_start(out=w2s[p2].t[:], in_=w2b[ex].rearrange("(c p) f -> p c f", p=128)), wsl[p2], R=[wb_buf[ex]], W=[w2s[p2]])

            eloads(0)
            for ex in range(NE):
                p2 = ex % 2
                if ex + 1 < NE:
                    eloads(ex + 1)
                for cq in range(4):
                    for cc in range(2):
                        c = cq * 2 + cc
                        for s in range(2):
                            P.op(lambda e, c=c, cc=cc, s=s, p2=p2: e.transpose(out=ps_x.t[:, cc, s * 128:(s + 1) * 128],
                                                                               in_=xg[p2].t[:, s, c * 128:(c + 1) * 128], identity=identB),
                                 R=[xg[p2], cbT], W=[ps_x])
                    if cq % 2 == 0:
                        V.op(lambda e, cq=cq, p2=p2: e.tensor_copy(out=xT[p2].t[:, cq * 2:cq * 2 + 2, :], in_=ps_x.t[:]), R=[ps_x], W=[xT[p2]])
                    else:
                        A.op(lambda e, cq=cq, p2=p2: e.activation(out=xT[p2].t[:, cq * 2:cq * 2 + 2, :], in_=ps_x.t[:], func=AF.Copy),
                             R=[ps_x], W=[xT[p2]])
                for fc in range(4):
                    f2 = fc % 2
                    ps_a = ps_a2[f2]
                    ps_b = ps_b2[f2]
                    for c in range(8):
                        P.op(lambda e, c=c, fc=fc, p2=p2, ps_a=ps_a: e.matmul(out=ps_a.t[:], lhsT=w1s[p2].t[:, c, fc * 128:(fc + 1) * 128], rhs=xT[p2].t[:, c, :],
                                                                   start=(c == 0), stop=(c == 7)), R=[w1s[p2], xT[p2]], W=[ps_a])
                    for c in range(8):
                        P.op(lambda e, c=c, fc=fc, p2=p2, ps_b=ps_b: e.matmul(out=ps_b.t[:], lhsT=w3s[p2].t[:, c, fc * 128:(fc + 1) * 128], rhs=xT[p2].t[:, c, :],
                                                                   start=(c == 0), stop=(c == 7)), R=[w3s[p2], xT[p2]], W=[ps_b])
                    A.op(lambda e, f2=f2, ps_a=ps_a: e.activation(out=sa[f2].t[:], in_=ps_a.t[:], func=AF.Silu), R=[ps_a], W=[sa[f2]])
                    V.op(lambda e, f2=f2, fc=fc, p2=p2, ps_b=ps_b: e.tensor_tensor(out=hT[p2].t[:, fc, :], in0=ps_b.t[:], in1=sa[f2].t[:], op=ALU.mult),
                         R=[ps_b, sa[f2]], W=[hT[p2]])
                for s in range(2):
                    for half in range(2):
                        yy = ps_y[half]
                        for fc in range(4):
                            P.op(lambda e, fc=fc, s=s, half=half, p2=p2, yy=yy: e.matmul(
                                out=yy.t[:], lhsT=hT[p2].t[:, fc, s * 128:(s + 1) * 128], rhs=w2s[p2].t[:, fc, half * 512:(half + 1) * 512],
                                start=(fc == 0), stop=(fc == 3)), R=[hT[p2], w2s[p2]], W=[yy])
                        if half == 0:
                            V.op(lambda e, s=s, p2=p2, yy=yy: e.tensor_copy(out=ysb[0].t[:, s, 0:512], in_=yy.t[:]), R=[yy], W=[ysb[0]])
                        else:
                            A.op(lambda e, s=s, p2=p2, yy=yy: e.activation(out=ysb[0].t[:, s, 512:1024], in_=yy.t[:], func=AF.Copy), R=[yy], W=[ysb[0]])
                G.dma(lambda e, ex=ex, p2=p2: e.dma_start(out=Ys[ex * CAP:(ex + 1) * CAP, :].rearrange("(s p) f -> p s f", p=128), in_=ysb[0].t[:]),
                      yss[0], R=[ysb[0]], W=[Ys_buf[ex]])

            for ti in range(16):
                p2 = ti % 2
                G.dma(lambda e, ti=ti, p2=p2: e.indirect_dma_start(out=ya[0].t[:, :], out_offset=None, in_=Ys,
                                                                   in_offset=bass.IndirectOffsetOnAxis(ap=dsti.t[:, ti, 0:1], axis=0)),
                      ygs[0], R=Ys_buf + [dsti], W=[ya[0]])
                G.dma(lambda e, ti=ti, p2=p2: e.indirect_dma_start(out=yb[0].t[:, :], out_offset=None, in_=Ys,
                                                                   in_offset=bass.IndirectOffsetOnAxis(ap=dsti.t[:, ti, 1:2], axis=0)),
                      ygs[0], R=Ys_buf + [dsti], W=[yb[0]])
                V.op(lambda e, ti=ti, p2=p2: e.scalar_tensor_tensor(out=hh.t[:, ti, :], in0=ya[0].t[:], scalar=gates.t[:, ti, 0:1], in1=hh.t[:, ti, :],
                                                                    op0=ALU.mult, op1=ALU.add), R=[ya[0], gates, hh], W=[hh])
                V.op(lambda e, ti=ti, p2=p2: e.scalar_tensor_tensor(out=hh.t[:, ti, :], in0=yb[0].t[:], scalar=gates.t[:, ti, 1:2], in1=hh.t[:, ti, :],
                                                                    op0=ALU.mult, op1=ALU.add), R=[yb[0], gates, hh], W=[hh])
                A.op(lambda e, ti=ti: e.activation(out=junk4.t[:], in_=hh.t[:, ti, :], func=AF.Square, accum_out=ss3.t[:, ti:ti + 1]),
                     R=[hh], W=[junk4, ss3])
                rstd_ops(ss3, rs3, ti, D, EPS)
                V.op(lambda e, ti=ti, p2=p2: e.scalar_tensor_tensor(out=ot[p2].t[:], in0=hh.t[:, ti, :], scalar=rs3.t[:, ti:ti + 1], in1=gfT.t[:],
                                                                    op0=ALU.mult, op1=ALU.mult), R=[hh, rs3, gfT], W=[ot[p2]])
                S.dma(lambda e, ti=ti, p2=p2: e.dma_start(out=out[ti * 128:(ti + 1) * 128, :], in_=ot[p2].t[:]), ots[p2], R=[ot[p2]], W=[out_buf])
            fin = Buf("fin")
            for sl in ots + [dslot]:
                if sl.v:
                    fin.w[id(sl.s)] = (sl.s, sl.v)
            S.wait_all([fin])
            es2b.close()
            pg.es = es0
        else:
            fin = Buf("fin")
            if dslot.v:
                fin.w[id(dslot.s)] = (dslot.s, dslot.v)
            S.wait_all([fin])
        pg.emit()
    return nc


def _consts():
    a = np.arange(128)
    ident = np.eye(128, dtype=np.float32)
    triu = (a[:, None] <= a[None, :]).astype(np.float32)
    strict = (a[:, None] < a[None, :]).astype(np.float32)
    ones = np.ones((128, 128), np.float32)
    ecol = np.tile((np.arange(NE) * CAP).astype(np.float32)[None, :], (128, 1))
    cf = np.concatenate([ident, triu, strict, ones, ecol], axis=1)
    L = (a[:, None] >= a[None, :]).astype(np.float32)
    U = triu
    cb = np.concatenate([ident, L, U, L, U], axis=1).astype(ml_dtypes.bfloat16)
    return np.ascontiguousarray(cf), np.ascontiguousarray(cb)


def make_in_maps(x, norm1_g, w_in, gla_gate_w2, gla_gate_b, gla_norm_g, w_out, norm2_g,
                 router_group_w, router_group_b, router_expert_w, router_expert_b,
                 expert_w1, expert_w3, expert_w2, final_norm_g):
    f = lambda a: np.ascontiguousarray(np.asarray(a, dtype=np.float32))
    x = f(x)
    win = f(w_in)[0]
    cf, cb = _consts()
    gq0, gk0, gv0, gr0, glr0, aq0, ak0, av0 = 0, 256, 512, 1024, 1536, 1552, 2064, 2576
    w1 = f(expert_w1)[0]
    w3 = f(expert_w3)[0]
    w2 = f(expert_w2)[0]
    wo = f(w_out)[0]
    g2r = f(np.tile(np.asarray(norm2_g)[0][None, :], (128, 1)))
    gfr = f(np.tile(np.asarray(final_norm_g)[None, :], (128, 1)))
    wr = f(np.concatenate([np.asarray(router_group_w)[0], np.asarray(router_expert_w)[0]], axis=1))
    br = np.concatenate([np.asarray(router_group_b)[0], np.asarray(router_expert_b)[0]])
    brr = f(np.tile(br[None, :], (128, 1)))
    g1 = f(np.asarray(norm1_g)[0].reshape(8, 128).T)
    gng = f(np.tile(np.asarray(gla_norm_g)[0][None, :], (128, 4)))
    maps = []
    for c in range(8):
        b, j = c // 4, c % 4
        cols = np.concatenate([
            np.arange(gq0 + 64 * j, gq0 + 64 * j + 64), np.arange(gk0 + 64 * j, gk0 + 64 * j + 64),
            np.arange(aq0 + 128 * j, aq0 + 128 * j + 128), np.arange(ak0 + 128 * j, ak0 + 128 * j + 128),
            np.arange(av0 + 128 * j, av0 + 128 * j + 128), np.arange(glr0, glr0 + 16),
            np.arange(gk0 + 64 * j, gk0 + 64 * j + 64), np.arange(gv0 + 128 * j, gv0 + 128 * j + 128),
            np.arange(gr0 + 128 * j, gr0 + 128 * j + 128)])
        w2aug = np.concatenate([np.asarray(gla_gate_w2)[0][:, 64 * j:64 * j + 64],
                                np.asarray(gla_gate_b)[0][None, 64 * j:64 * j + 64]], axis=0)
        rowidx = (j * 1024 + np.arange(8)[None, :] * 128 + np.arange(128)[:, None]).astype(np.int32)
        maps.append({
            "x": x[b], "xres": np.ascontiguousarray(x[b, 2048 * j:2048 * (j + 1)]),
            "w_in": np.ascontiguousarray(win[:, cols]), "g1": g1, "w2aug": f(w2aug), "gng": gng,
            "w_out": wo, "g2r": g2r, "gfr": gfr, "wr": wr, "brr": brr, "w1": w1, "w3": w3, "w2": w2,
            "cf": cf, "cb": cb, "rowidx": np.ascontiguousarray(rowidx),
        })
    return maps


_NC_CACHE = {}


def kernel(**inputs):
    if "nc" not in _NC_CACHE:
        _NC_CACHE["nc"] = build()
    nc = _NC_CACHE["nc"]
    maps = make_in_maps(**inputs)
    res = run_bass_kernel_spmd(nc, maps, core_ids=list(range(8)))
    outs = [np.asarray(res.results[c]["out"], dtype=np.float32) for c in range(8)]
    y = np.stack([np.concatenate(outs[0:4], axis=0), np.concatenate(outs[4:8], axis=0)], axis=0)
    return y
```

```python
import numpy as np
import ml_dtypes
from contextlib import ExitStack
import concourse.bass as bass
import concourse.mybir as mybir
from concourse.bass_utils import run_bass_kernel_spmd

F32 = mybir.dt.float32
BF16 = mybir.dt.bfloat16
I32 = mybir.dt.int32
AF = mybir.ActivationFunctionType
ALU = mybir.AluOpType
AX = mybir.AxisListType

T = 8192
D = 1024
NSB = 16
NSUP = 4
CAP = 256
NE = 32
EPS = 1e-6
BIG = 1.0e30
RING = 6144
SEM_LIMIT = 12000
import os
ATT_MODE = int(os.environ.get('ATT_MODE', '3'))
MASK_ENG = os.environ.get('MASK_ENG', 'V')
ATT_NOTR = int(os.environ.get('ATT_NOTR', '0'))
ATT_DS = tuple(int(v) for v in os.environ.get('ATT_DS', '1,4,16').split(','))


class Buf:
    __slots__ = ("name", "w", "r", "psum")

    def __init__(self, name, psum=False):
        self.name = name
        self.w = {}
        self.r = {}
        self.psum = psum


class Tile:
    def __init__(self, t, name, buf=None):
        self.t = t
        self.b = buf if buf is not None else Buf(name)


def _split_psum(R, W):
    R2, W2 = [], list(W)
    for x in R:
        b = x.b if isinstance(x, Tile) else x
        if b.psum:
            W2.append(x)
        else:
            R2.append(x)
    return R2, W2


class SemSlot:
    def __init__(self, sem):
        self.s = sem
        self.v = 0


class Eng:
    def __init__(self, prog, name, is_pe=False):
        self.prog = prog
        self.name = name
        self.is_pe = is_pe
        self.sem = prog.new_sem()
        self.cnt = 0
        self.known = {}
        self.thunks = []

    def _wait(self, sem, val):
        if self.known.get(id(sem), 0) >= val:
            return
        self.known[id(sem)] = val
        self.thunks.append(lambda e, s=sem, v=val: e.wait_ge(s, v))

    def _deps(self, R, W):
        for x in R:
            b = x.b if isinstance(x, Tile) else x
            for (sem, val) in b.w.values():
                if sem is self.sem and self.is_pe:
                    continue
                self._wait(sem, val)
        for x in W:
            b = x.b if isinstance(x, Tile) else x
            for (sem, val) in list(b.w.values()) + list(b.r.values()):
                if sem is self.sem and self.is_pe:
                    continue
                self._wait(sem, val)

    def _record(self, R, W, sem, val):
        for x in R:
            b = x.b if isinstance(x, Tile) else x
            b.r[id(sem)] = (sem, val)
        for x in W:
            b = x.b if isinstance(x, Tile) else x
            b.w = {id(sem): (sem, val)}
            b.r = {}

    def op(self, fn, R=(), W=()):
        R, W = _split_psum(R, W)
        self._deps(R, W)
        if self.cnt >= SEM_LIMIT:
            self.sem = self.prog.new_sem()
            self.cnt = 0
        self.cnt += 1
        sem, val = self.sem, self.cnt
        self.thunks.append(lambda e, f=fn, s=sem: f(e).then_inc(s, 1))
        self._record(R, W, sem, val)

    def dma(self, fn, slot, R=(), W=()):
        self._deps(R, W)
        slot.v += 16
        sem, val = slot.s, slot.v
        self.thunks.append(lambda e, f=fn, s=sem: f(e).then_inc(s, 16))
        self._record(R, W, sem, val)

    def coll(self, fn, R=(), W=()):
        self._deps(R, W)
        sem = self.prog.new_sem()
        self.thunks.append(lambda e, f=fn, s=sem: f(e).then_inc(s))
        self._record(R, W, sem, 1)

    def wait_all(self, bufs):
        self._deps(bufs, ())


class Prog:
    def __init__(self, nc, es):
        self.nc = nc
        self.es = es
        self.es_sem = es
        self.nsem = 0
        self.V = Eng(self, "vector")
        self.A = Eng(self, "scalar")
        self.G = Eng(self, "gpsimd")
        self.P = Eng(self, "tensor", is_pe=True)
        self.S = Eng(self, "sync")

    def new_sem(self):
        self.nsem += 1
        return self.es_sem.enter_context(self.nc.semaphore("s%d" % self.nsem))

    def slot(self):
        return SemSlot(self.new_sem())

    def sb(self, name, shape, dt):
        return Tile(self.es.enter_context(self.nc.sbuf_tensor(name, shape, dt)), name)

    def ps(self, name, shape, dt):
        return Tile(self.es.enter_context(self.nc.psum_tensor(name, shape, dt)), name)

    def emit(self):
        with self.nc.Block() as block:
            @block.vector
            def _(e):
                for th in self.V.thunks:
                    th(e)

            @block.scalar
            def _(e):
                for th in self.A.thunks:
                    th(e)

            @block.gpsimd
            def _(e):
                for th in self.G.thunks:
                    th(e)

            @block.tensor
            def _(e):
                for th in self.P.thunks:
                    th(e)

            @block.sync
            def _(e):
                for th in self.S.thunks:
                    th(e)


def build(debug=False, phase2=True, do_coll=True, nsb_run=NSB, do_gla=True, do_att=True):
    nc = bass.Bass("TRN2", target_bir_lowering=False)
    dram = lambda n, s, dt, k="ExternalInput": nc.dram_tensor(n, s, dt, kind=k).ap()
    x = dram("x", [T, D], F32)
    xres = dram("xres", [2048, D], F32)
    w_in = dram("w_in", [D, 848], F32)
    g1 = dram("g1", [128, 8], F32)
    w2aug = dram("w2aug", [17, 64], F32)
    gng = dram("gng", [128, 512], F32)
    w_out = dram("w_out", [D, D], F32)
    g2r = dram("g2r", [128, D], F32)
    gfr = dram("gfr", [128, D], F32)
    wr = dram("wr", [D, 36], F32)
    brr = dram("brr", [128, 36], F32)
    NEd = NE if phase2 else 1
    w1 = dram("w1", [NEd, D, 512], F32)
    w3 = dram("w3", [NEd, D, 512], F32)
    w2 = dram("w2", [NEd, 512, D], F32)
    cf = dram("cf", [128, 544], F32)
    cb = dram("cb", [128, 640], BF16)
    out = dram("out", [2048, D], F32, "ExternalOutput")
    a_int = [dram("a_int%d" % s, [256, 2048], BF16, "Internal") for s in range(NSUP)]
    b_all = dram("b_all", [NSUP * 1024, 2048], BF16, "Internal")
    rowidx = dram("rowidx", [128, 8], I32)
    w1b = dram("w1b", [NEd, D, 512], BF16, "Internal")
    w3b = dram("w3b", [NEd, D, 512], BF16, "Internal")
    w2b = dram("w2b", [NEd, 512, D], BF16, "Internal")
    wb_buf = [Buf("wb%d" % e) for e in range(NE)]
    Xs = dram("xs_scr", [NE * CAP, D], BF16, "Internal")
    Ys = dram("ys_scr", [NE * CAP, D], F32, "Internal")
    if debug:
        dbg_o = dram("dbg_o", [NSUP * 1024, 2048], BF16, "ExternalOutput")
        dbg_h = dram("dbg_h", [2048, D], F32, "ExternalOutput")
    a_buf = [Buf("a%d" % s) for s in range(NSUP)]
    b_buf = [Buf("b%d" % s) for s in range(NSUP)]
    Xs_buf = Buf("Xs")
    Ys_buf = [Buf("Ys%d" % e) for e in range(NE)]
    out_buf = Buf("out")

    with ExitStack() as es0:
        pg = Prog(nc, es0)
        V, A, G, P, S = pg.V, pg.A, pg.G, pg.P, pg.S
        cslot = pg.slot()
        cslot_g = pg.slot()
        banks = [es0.enter_context(nc.psum_tensor("bank%d" % i, [128, 512], F32)) for i in range(8)]
        bank_buf = [Buf("bank%d" % i, psum=True) for i in range(8)]

        def carve(name, bank, c0, ncol, dt=F32, pat=None, parts=128, **kw):
            ap = banks[bank][0:parts, c0:c0 + ncol]
            if dt is not F32:
                ap = ap.bitcast(dt)
            if pat is not None:
                ap = ap.rearrange(pat, **kw)
            return Tile(ap, name, bank_buf[bank])

        cfT = pg.sb("cfT", [128, 544], F32)
        cbT = pg.sb("cbT", [128, 640], BF16)
        S.dma(lambda e: e.dma_start(out=cfT.t[:], in_=cf), cslot, W=[cfT])
        S.dma(lambda e: e.dma_start(out=cbT.t[:], in_=cb), cslot, W=[cbT])
        epsT = pg.sb("epsT", [128, 2], F32)
        V.op(lambda e: e.memset(epsT.t[:, 0:1], EPS), W=[epsT])
        V.op(lambda e: e.memset(epsT.t[:, 1:2], 64 * EPS), W=[epsT])
        identF = cfT.t[:, 0:128]
        triU = cfT.t[:, 128:256]
        strictT = cfT.t[:, 256:384]
        onesF = cfT.t[:, 384:512]
        ecol = cfT.t[:, 512:544]
        identB = cbT.t[:, 0:128]
        mask4 = cbT.t[:, 128:640]

        with ExitStack() as es1:
            pg.es = es1
            NX = 6
            Wt = pg.sb("Wt", [128, 8, 848], BF16)
            g1T = pg.sb("g1T", [128, 8], F32)
            w2a = pg.sb("w2a", [17, 64], F32)
            gngT = pg.sb("gngT", [128, 4, 128], F32)
            xt = [pg.sb("xt%d" % i, [128, D], F32) for i in range(NX)]
            xsl = [pg.slot() for _ in range(NX)]
            junk = pg.sb("junk", [128, D], BF16)
            ss1 = pg.sb("ss1", [128, 64], F32)
            rs1 = pg.sb("rs1", [128, 64], F32)
            xs = [pg.sb("xs%d" % i, [128, D], BF16) for i in range(2)]
            nT = [pg.sb("nT%d" % i, [128, 8, 512], BF16) for i in range(2)]
            qT = [pg.sb("qT%d" % i, [64, 512], BF16) for i in range(2)]
            kT = [pg.sb("kT%d" % i, [64, 512], BF16) for i in range(2)]
            QT = [[pg.sb("QT%d_%d" % (h, i), [64, 2048], BF16) for i in range(2)] for h in range(2)]
            KT = [[pg.sb("KT%d_%d" % (h, i), [64, 2048], BF16) for i in range(3)] for h in range(2)]
            VT = [[pg.sb("VT%d_%d" % (h, i), [64, 2048], BF16) for i in range(3)] for h in range(2)]
            tm = [pg.sb("tm%d" % i, [128, 4, 320], BF16) for i in range(2)]
            glrT = [pg.sb("glrT%d" % i, [32, 512], F32) for i in range(2)]
            e1 = pg.sb("e1", [128, 256], F32)
            sp = pg.sb("sp", [128, 256], F32)
            Ek = pg.sb("Ek", [128, 256], F32)
            ktl = pg.sb("ktl", [128, 4, 64], BF16)
            EqT = pg.sb("EqT", [64, 512], F32)
            EkT = pg.sb("EkT", [64, 512], F32)
            qtl = pg.sb("qtl", [64, 512], BF16)
            ktlT = pg.sb("ktlT", [64, 512], BF16)
            gg = pg.sb("gg", [128, 4, 128], F32)
            AmT = [pg.sb("AmT%d" % i, [128, 128], BF16) for i in range(2)]
            og = [pg.sb("og%d" % i, [128, 128], BF16) for i in range(2)]
            Sst = pg.sb("Sst", [64, 128], F32)
            Sbf = pg.sb("Sbf", [64, 128], BF16)
            Stmp = pg.sb("Stmp", [64, 128], F32)
            ssg = pg.sb("ssg", [128, 64], F32)
            rsg = pg.sb("rsg", [128, 64], F32)
            junk2 = pg.sb("junk2", [128, 128], BF16)
            ogT = [pg.sb("ogT%d" % i, [128, 2048], BF16) for i in range(2)]
            pTa = [pg.sb("pTa%d" % i, [128, 512], BF16) for i in range(3)]
            vp = [pg.sb("vp%d" % i, [128, 4, 128], BF16) for i in range(3)]
            Oacc = pg.sb("Oacc", [128, 2, 2048], F32)
            rden = pg.sb("rden", [64, 2048], F32)
            oTa = [pg.sb("oTa%d" % i, [128, 2048], BF16) for i in range(2)]
            stsl = [pg.slot() for _ in range(2)]
            ps_pT_l = [carve("ps_pT", 0, 0, 512, BF16, "p (a b) -> p a b", a=8), carve("ps_pT2", 4, 0, 512, BF16, "p (a b) -> p a b", a=8)]
            ps_pr = [carve("ps_pr%d" % i, 1 + i, 0, 512) for i in range(2)]
            ps_GT = carve("ps_GT", 3, 0, 512, parts=64)
            ps_s = carve("ps_s", 4, 0, 512)
            ps_z = carve("ps_z", 5, 0, 256)
            ps_G = Tile(banks[5][:, 256:512], "ps_G", bank_buf[5])
            ps_o = carve("ps_o", 6, 0, 256, F32, "p (a b) -> p a b", a=2)
            ps_A = Tile(banks[6][:, 256:384], "ps_A", bank_buf[6])
            ps_og = Tile(banks[6][:, 384:512], "ps_og", bank_buf[6])
            ps_U = Tile(banks[7][0:64, 0:128], "ps_U", bank_buf[7])
            ps_v = Tile(banks[7][:, 128:256].bitcast(BF16).rearrange("p (a b) -> p a b", a=4), "ps_v", bank_buf[7])
            ps_ogT = Tile(banks[7][:, 256:320].bitcast(BF16), "ps_ogT", bank_buf[7])
            ps_s_l = [ps_s, Tile(banks[5][:, :], "ps_s2", bank_buf[5]), Tile(banks[3][:, :], "ps_s3", bank_buf[3])]
            ps_o_l = [ps_o, Tile(banks[0][:, 0:256].rearrange("p (a b) -> p a b", a=2), "ps_o2", bank_buf[0])]
            ps_v_l = [ps_v, Tile(banks[1][:, 128:256].bitcast(BF16).rearrange("p (a b) -> p a b", a=4), "ps_v2", bank_buf[1])]

            for kc in range(8):
                G.dma(lambda e, kc=kc: e.dma_start(out=Wt.t[:, kc, :], in_=w_in[kc * 128:(kc + 1) * 128, :]),
                      cslot_g, W=[Wt])
            S.dma(lambda e: e.dma_start(out=g1T.t[:], in_=g1), cslot, W=[g1T])
            S.dma(lambda e: e.dma_start(out=w2a.t[:], in_=w2aug), cslot, W=[w2a])
            S.dma(lambda e: e.dma_start(out=gngT.t[:].rearrange("p a b -> p (a b)"), in_=gng), cslot, W=[gngT])
            for kc in range(8):
                V.op(lambda e, kc=kc: e.tensor_scalar(out=Wt.t[:, kc, :], in0=Wt.t[:, kc, :], scalar1=g1T.t[:, kc:kc + 1],
                                                      scalar2=None, op0=ALU.mult), R=[g1T, Wt], W=[Wt])
            V.op(lambda e: e.memset(ss1.t[:], 0.0), W=[ss1])
            V.op(lambda e: e.memset(ssg.t[:], 0.0), W=[ssg])
            V.op(lambda e: e.memset(Sst.t[:], 0.0), W=[Sst])
            V.op(lambda e: e.memset(Sbf.t[:], 0.0), W=[Sbf])
            for i in range(2):
                G.op(lambda e, i=i: e.memset(glrT[i].t[:], 1.0), W=[glrT[i]])
            for i in range(3):
                G.op(lambda e, i=i: e.memset(vp[i].t[:], 1.0), W=[vp[i]])

            pr_i = [0]

            def next_pr():
                pr_i[0] ^= 1
                return ps_pr[pr_i[0]]

            def stage1(sb):
                par = sb % 2
                sup = sb // 4
                for i in range(4):
                    ti = sb * 4 + i
                    sl = ti % NX
                    S.dma(lambda e, ti=ti, sl=sl: e.dma_start(out=xt[sl].t[:], in_=x[ti * 128:(ti + 1) * 128, :]),
                          xsl[sl], W=[xt[sl]])
                    A.op(lambda e, ti=ti, sl=sl: e.activation(out=junk.t[:], in_=xt[sl].t[:], func=AF.Square,
                                                              accum_out=ss1.t[:, ti:ti + 1]),
                         R=[xt[sl]], W=[junk, ss1])
                    A.op(lambda e, ti=ti: e.activation(out=rs1.t[:, ti:ti + 1], in_=ss1.t[:, ti:ti + 1], func=AF.Sqrt, scale=1.0 / D, bias=epsT.t[:, 0:1]),
                         R=[ss1, epsT], W=[rs1])
                    V.op(lambda e, ti=ti: e.reciprocal(out=rs1.t[:, ti:ti + 1], in_=rs1.t[:, ti:ti + 1]), R=[rs1], W=[rs1])
                    xp = ti % 2
                    V.op(lambda e, ti=ti, sl=sl, xp=xp: e.tensor_scalar(out=xs[xp].t[:], in0=xt[sl].t[:],
                                                                        scalar1=rs1.t[:, ti:ti + 1], scalar2=None, op0=ALU.mult),
                         R=[xt[sl], rs1], W=[xs[xp]])
                    ps_pT = ps_pT_l[ti % 2]
                    for kc in range(8):
                        P.op(lambda e, kc=kc, xp=xp, ps_pT=ps_pT: e.transpose(out=ps_pT.t[:, kc, :], in_=xs[xp].t[:, kc * 128:(kc + 1) * 128],
                                                                 identity=identB), R=[xs[xp], cbT], W=[ps_pT])
                    A.op(lambda e, i=i, par=par, ps_pT=ps_pT: e.activation(out=nT[par].t[:, :, i * 128:(i + 1) * 128], in_=ps_pT.t[:, :, :],
                                                              func=AF.Copy), R=[ps_pT], W=[nT[par]])
                col = (sb % 4) * 512
                kslot = sup % 3
                fm = [
                    (0, 128, [(qT[par], qT[par].t[0:64, :]), (kT[par], kT[par].t[0:64, :])]),
                    (128, 128, [(QT[0][sup % 2], QT[0][sup % 2].t[:, col:col + 512]), (QT[1][sup % 2], QT[1][sup % 2].t[:, col:col + 512])]),
                    (256, 128, [(KT[0][kslot], KT[0][kslot].t[:, col:col + 512]), (KT[1][kslot], KT[1][kslot].t[:, col:col + 512])]),
                    (384, 128, [(VT[0][kslot], VT[0][kslot].t[:, col:col + 512]), (VT[1][kslot], VT[1][kslot].t[:, col:col + 512])]),
                    (512, 16, [(glrT[par], glrT[par].t[0:16, :])]),
                ]
                for gi, (c0, m, dsts) in enumerate(fm):
                    pp = next_pr()
                    for kc in range(8):
                        P.op(lambda e, kc=kc, c0=c0, m=m, pp=pp, par=par: e.matmul(
                            out=pp.t[0:m, :], lhsT=Wt.t[:, kc, c0:c0 + m], rhs=nT[par].t[:, kc, :],
                            start=(kc == 0), stop=(kc == 7)), R=[Wt, nT[par]], W=[pp])
                    if len(dsts) == 2:
                        (t0_, d0_), (t1_, d1_) = dsts
                        A.op(lambda e, pp=pp, d0_=d0_: e.activation(out=d0_, in_=pp.t[0:64, :], func=AF.Copy), R=[pp], W=[t0_])
                        V.op(lambda e, pp=pp, d1_=d1_: e.tensor_copy(out=d1_, in_=pp.t[64:128, :]), R=[pp], W=[t1_])
                    else:
                        (t0_, d0_), = dsts
                        V.op(lambda e, pp=pp, m=m, d0_=d0_: e.tensor_copy(out=d0_, in_=pp.t[0:m, :]), R=[pp], W=[t0_])
                for i in range(4):
                    pp = next_pr()
                    for kc in range(8):
                        P.op(lambda e, kc=kc, i=i, pp=pp, par=par: e.matmul(
                            out=pp.t[:, 0:320], lhsT=nT[par].t[:, kc, i * 128:(i + 1) * 128], rhs=Wt.t[:, kc, 528:848],
                            start=(kc == 0), stop=(kc == 7)), R=[Wt, nT[par]], W=[pp])
                    V.op(lambda e, pp=pp, i=i, par=par: e.tensor_copy(out=tm[par].t[:, i, 0:192], in_=pp.t[:, 0:192]),
                         R=[pp], W=[tm[par]])
                    A.op(lambda e, pp=pp, i=i, par=par: e.activation(out=tm[par].t[:, i, 192:320], in_=pp.t[:, 192:320],
                                                                     func=AF.Silu), R=[pp], W=[tm[par]])

            def gla(sb):
                par = sb % 2
                sup = sb // 4
                for i in range(4):
                    P.op(lambda e, i=i, par=par: e.matmul(out=ps_z.t[:, i * 64:(i + 1) * 64], lhsT=glrT[par].t[0:17, i * 128:(i + 1) * 128],
                                                          rhs=w2a.t[0:17, :], start=True, stop=True),
                         R=[glrT[par], w2a], W=[ps_z])
                A.op(lambda e: e.activation(out=e1.t[:], in_=ps_z.t[:], func=AF.Exp, scale=-1.0), R=[ps_z], W=[e1])
                A.op(lambda e: e.activation(out=sp.t[:], in_=e1.t[:], func=AF.Ln, bias=1.0), R=[e1], W=[sp])
                P.op(lambda e: e.matmul(out=ps_G.t[:], lhsT=triU, rhs=sp.t[:], start=True, stop=True), R=[sp, cfT], W=[ps_G])
                for i in range(4):
                    P.op(lambda e, i=i: e.matmul(out=ps_GT.t[:, i * 128:(i + 1) * 128], lhsT=sp.t[:, i * 64:(i + 1) * 64], rhs=triU,
                                                 start=True, stop=True), R=[sp, cfT], W=[ps_GT])
                A.op(lambda e: e.activation(out=Ek.t[:], in_=ps_G.t[:], func=AF.Exp, scale=1.0 / 16), R=[ps_G], W=[Ek])
                A.op(lambda e: e.activation(out=EqT.t[:], in_=ps_GT.t[:], func=AF.Exp, scale=-1.0 / 16), R=[ps_GT], W=[EqT])
                A.op(lambda e: e.activation(out=EkT.t[:], in_=ps_GT.t[:], func=AF.Exp, scale=1.0 / 16), R=[ps_GT], W=[EkT])
                V.op(lambda e, par=par: e.tensor_tensor(out=ktl.t[:, :, :], in0=tm[par].t[:, :, 0:64],
                                                        in1=Ek.t[:].rearrange("p (a b) -> p a b", a=4), op=ALU.mult),
                     R=[tm[par], Ek], W=[ktl])
                V.op(lambda e, par=par: e.tensor_tensor(out=qtl.t[:], in0=qT[par].t[:], in1=EqT.t[:], op=ALU.mult),
                     R=[qT[par], EqT], W=[qtl])
                V.op(lambda e, par=par: e.tensor_tensor(out=ktlT.t[:], in0=kT[par].t[:], in1=EkT.t[:], op=ALU.mult),
                     R=[kT[par], EkT], W=[ktlT])
                G.op(lambda e, par=par: e.tensor_tensor(out=gg.t[:], in0=tm[par].t[:, :, 192:320], in1=gngT.t[:], op=ALU.mult),
                     R=[tm[par], gngT], W=[gg])
                for i in range(4):
                    ch = sb * 4 + i
                    cp = ch % 2
                    cs = slice(i * 128, (i + 1) * 128)
                    vap = tm[par].t[:, i, 64:192]
                    P.op(lambda e, cs=cs: e.matmul(out=ps_A.t[:], lhsT=ktlT.t[:, cs], rhs=qtl.t[:, cs], start=True, stop=True),
                         R=[ktlT, qtl], W=[ps_A])
                    V.op(lambda e, cp=cp: e.tensor_tensor(out=AmT[cp].t[:], in0=ps_A.t[:], in1=triU, op=ALU.mult),
                         R=[ps_A, cfT], W=[AmT[cp]])
                    P.op(lambda e, cp=cp, vap=vap: e.matmul(out=ps_og.t[:], lhsT=AmT[cp].t[:], rhs=vap, start=True, stop=False),
                         R=[AmT[cp], tm[par]], W=[ps_og])
                    P.op(lambda e, cs=cs: e.matmul(out=ps_og.t[:], lhsT=qtl.t[:, cs], rhs=Sbf.t[:], start=False, stop=True),
                         R=[qtl, Sbf], W=[ps_og])
                    P.op(lambda e, i=i, vap=vap: e.matmul(out=ps_U.t[:], lhsT=ktl.t[:, i, :], rhs=vap, start=True, stop=True),
                         R=[ktl, tm[par]], W=[ps_U])
                    acol = EqT.t[:, i * 128 + 127:i * 128 + 128]
                    V.op(lambda e: e.tensor_tensor(out=Stmp.t[:], in0=ps_U.t[:], in1=Sst.t[:], op=ALU.add),
                         R=[ps_U, Sst], W=[Stmp])
                    V.op(lambda e, acol=acol: e.tensor_scalar(out=Sst.t[:], in0=Stmp.t[:], scalar1=acol, scalar2=None, op0=ALU.mult),
                         R=[Stmp, EqT], W=[Sst])
                    A.op(lambda e, acol=acol: e.activation(out=Sbf.t[:], in_=Stmp.t[:], func=AF.Copy, scale=acol),
                         R=[Stmp, EqT], W=[Sbf])
                    A.op(lambda e, ch=ch: e.activation(out=junk2.t[:], in_=ps_og.t[:], func=AF.Square, accum_out=ssg.t[:, ch:ch + 1]),
                         R=[ps_og], W=[junk2, ssg])
                    A.op(lambda e, ch=ch: e.activation(out=rsg.t[:, ch:ch + 1], in_=ssg.t[:, ch:ch + 1], func=AF.Sqrt, scale=1.0 / 128, bias=epsT.t[:, 1:2]),
                         R=[ssg, epsT], W=[rsg])
                    V.op(lambda e, ch=ch: e.reciprocal(out=rsg.t[:, ch:ch + 1], in_=rsg.t[:, ch:ch + 1]), R=[rsg], W=[rsg])
                    V.op(lambda e, ch=ch, cp=cp, i=i: e.scalar_tensor_tensor(out=og[cp].t[:], in0=ps_og.t[:], scalar=rsg.t[:, ch:ch + 1],
                                                                             in1=gg.t[:, i, :], op0=ALU.mult, op1=ALU.mult),
                         R=[ps_og, rsg, gg], W=[og[cp]])
                    P.op(lambda e, cp=cp: e.transpose(out=ps_ogT.t[:], in_=og[cp].t[:], identity=identB), R=[og[cp], cbT], W=[ps_ogT])
                    cc = (ch % 16) * 128
                    A.op(lambda e, cc=cc, sup=sup: e.activation(out=ogT[sup % 2].t[:, cc:cc + 128], in_=ps_ogT.t[:], func=AF.Copy),
                         R=[ps_ogT], W=[ogT[sup % 2]])

            def attention(sup):
                qp = sup % 2
                units = []
                for d in ATT_DS:
                    for r in range(d):
                        for n in range(16 // d):
                            units.append((d, r, n))
                ui = [0]

                def kcols(t0, d):
                    slot = (t0 // 2048) % 3
                    c0 = t0 % 2048
                    return slot, slice(c0, c0 + 127 * d + 1, d)

                def scores(u):
                    d, r, n = units[u]
                    sl3 = u % 3
                    qb = r + d * 128 * n
                    qsl = slice(qb, qb + 127 * d + 1, d)
                    t0 = sup * 2048 + qb
                    has_prev = (t0 - 128 * d) >= 0
                    cur = kcols(t0, d)
                    prev = kcols(t0 - 128 * d, d) if has_prev else cur
                    pss = ps_s_l[u % 3]
                    psv = ps_v_l[u % 2]
                    for h in range(2):
                        for blk, (ks, kc) in enumerate((prev, cur)):
                            P.op(lambda e, h=h, blk=blk, ks=ks, kc=kc, qsl=qsl, pss=pss: e.matmul(
                                out=pss.t[:, (h * 2 + blk) * 128:(h * 2 + blk + 1) * 128], lhsT=KT[h][ks].t[:, kc],
                                rhs=QT[h][qp].t[:, qsl], start=True, stop=True), R=[KT[h][ks], QT[h][qp]], W=[pss])
                    for h in range(2):
                        for blk, (ks, kc) in enumerate((prev, cur)):
                            P.op(lambda e, h=h, blk=blk, ks=ks, kc=kc, psv=psv: e.transpose(
                                out=psv.t[:, h * 2 + blk, :], in_=VT[h][ks].t[:, kc], identity=cbT.t[0:64, 0:64]),
                                R=[VT[h][ks], cbT], W=[psv])
                    A.op(lambda e, sl3=sl3, pss=pss: e.activation(out=pTa[sl3].t[:], in_=pss.t[:], func=AF.Exp, scale=0.125),
                         R=[pss], W=[pTa[sl3]])
                    V.op(lambda e, sl3=sl3, psv=psv: e.tensor_copy(out=vp[sl3].t[:, :, 0:64], in_=psv.t[:, :, :]), R=[psv], W=[vp[sl3]])
                    G.op(lambda e, sl3=sl3: e.tensor_tensor(out=pTa[sl3].t[:], in0=pTa[sl3].t[:], in1=mask4, op=ALU.mult),
                         R=[pTa[sl3], cbT], W=[pTa[sl3]])
                    return has_prev

                def pv(u, has_prev):
                    d, r, n = units[u]
                    sl3 = u % 3
                    qb = r + d * 128 * n
                    qsl = slice(qb, qb + 127 * d + 1, d)
                    blks = (0, 1) if has_prev else (1,)
                    ps_o = ps_o_l[u % 2]
                    for h in range(2):
                        for bi, blk in enumerate(blks):
                            P.op(lambda e, h=h, blk=blk, bi=bi, sl3=sl3, nb=len(blks), ps_o=ps_o: e.matmul(
                                out=ps_o.t[:, h, :], lhsT=vp[sl3].t[:, h * 2 + blk, :],
                                rhs=pTa[sl3].t[:, (h * 2 + blk) * 128:(h * 2 + blk + 1) * 128],
                                start=(bi == 0), stop=(bi == nb - 1)), R=[vp[sl3], pTa[sl3]], W=[ps_o])
                    if d == 1:
                        V.op(lambda e, qsl=qsl, ps_o=ps_o: e.tensor_copy(out=Oacc.t[:, :, qsl], in_=ps_o.t[:, :, :]), R=[ps_o], W=[Oacc])
                    else:
                        V.op(lambda e, qsl=qsl, ps_o=ps_o: e.tensor_tensor(out=Oacc.t[:, :, qsl], in0=ps_o.t[:, :, :], in1=Oacc.t[:, :, qsl],
                                                                op=ALU.add), R=[ps_o, Oacc], W=[Oacc])

                hp = {}
                for u in range(len(units) + 1):
                    if u < len(units):
                        hp[u] = scores(u)
                    if u >= 1 and ATT_MODE >= 2:
                        pv(u - 1, hp[u - 1])
                for h in range(2 if ATT_MODE >= 3 else 0):
                    V.op(lambda e, h=h: e.reciprocal(out=rden.t[:], in_=Oacc.t[64:128, h, :]), R=[Oacc], W=[rden])
                    V.op(lambda e, h=h: e.tensor_tensor(out=oTa[qp].t[h * 64:(h + 1) * 64, :], in0=Oacc.t[0:64, h, :], in1=rden.t[:],
                                                        op=ALU.mult), R=[Oacc, rden], W=[oTa[qp]])

            def exchange(sup):
                qp = sup % 2
                S.dma(lambda e: e.dma_start(out=a_int[sup][0:128, :], in_=ogT[qp].t[:]), stsl[qp], R=[ogT[qp]], W=[a_buf[sup]])
                S.dma(lambda e: e.dma_start(out=a_int[sup][128:256, :], in_=oTa[qp].t[:]), stsl[qp], R=[oTa[qp]], W=[a_buf[sup]])
                if do_coll:
                  G.coll(lambda e: e.collective_compute("AllGather", ALU.bypass, replica_groups=[[0, 1, 2, 3], [4, 5, 6, 7]],
                                                      ins=[a_int[sup]], outs=[b_all[sup * 1024:(sup + 1) * 1024, :]]), R=[a_buf[sup]], W=[b_buf[sup]])

            wcs = [pg.slot() for _ in range(4)]

            def precast(ex):
                for (src, dst) in ((w1, w1b), (w3, w3b), (w2, w2b)):
                    G.dma(lambda e, src=src, dst=dst, ex=ex: e.dma_start(
                        out=dst[ex].rearrange("(p r) f -> p (r f)", p=128), in_=src[ex].rearrange("(p r) f -> p (r f)", p=128)),
                        wcs[ex % 4], W=[wb_buf[ex]])

            for sb in range(nsb_run + 1):
                if phase2 and sb < NSB:
                    precast(2 * sb)
                    precast(2 * sb + 1)
                if sb < nsb_run:
                    stage1(sb)
                if sb >= 1:
                    if do_gla:
                        gla(sb - 1)
                    if (sb - 1) % 4 == 3:
                        if do_att:
                            attention((sb - 1) // 4)
                        if do_gla and do_att:
                            exchange((sb - 1) // 4)
            if debug and not (do_gla and do_att):
                dbg1 = Buf("dbg1")
                S.dma(lambda e: e.dma_start(out=dbg_o[0:64, :], in_=QT[0][0].t[:]), stsl[0], R=[QT[0][0]], W=[dbg1])
                S.dma(lambda e: e.dma_start(out=dbg_o[128:192, :], in_=KT[0][0].t[:]), stsl[0], R=[KT[0][0]], W=[dbg1])
                S.dma(lambda e: e.dma_start(out=dbg_o[256:384, :], in_=ogT[0].t[:]), stsl[0], R=[ogT[0]], W=[dbg1])
                S.dma(lambda e: e.dma_start(out=dbg_o[384:512, :], in_=oTa[0].t[:]), stsl[0], R=[oTa[0]], W=[dbg1])
                S.wait_all([dbg1])
            pg.es = es0
        dslot = pg.slot()
        if debug and do_coll:
            S.dma(lambda e: e.dma_start(out=dbg_o, in_=b_all), dslot, R=b_buf, W=[out_buf])
        if debug and not do_coll and do_gla and do_att:
            for sup in range(nsb_run // 4):
                S.dma(lambda e, sup=sup: e.dma_start(out=dbg_o[sup * 256:(sup + 1) * 256, :], in_=a_int[sup]), dslot, R=[a_buf[sup]], W=[out_buf])
        if phase2:
          with ExitStack() as es2:
            pg.es = es2
            hh = pg.sb("hh", [128, 16, D], F32)
            gfT = pg.sb("gfT", [128, D], F32)
            ss3 = pg.sb("ss3", [128, 16], F32)
            rs3 = pg.sb("rs3", [128, 16], F32)
            gates = pg.sb("gates", [128, 16, 2], F32)
            dstf = pg.sb("dstf", [128, 16, 2], F32)
            dsti = pg.sb("dsti", [128, 16, 2], I32)

            es2a = ExitStack()
            pg.es = es2a
            ridx = pg.sb("ridx", [128, 8], I32)
            oT = pg.sb("oT", [128, 8, 2048], BF16)
            Wo = pg.sb("Wo", [128, 8, D], BF16)
            g2T = pg.sb("g2T", [128, D], F32)
            wrT = pg.sb("wrT", [128, 8, 36], F32)
            brT = pg.sb("brT", [128, 36], F32)
            xr = [pg.sb("xr%d" % i, [128, D], F32) for i in range(2)]
            xrs = [pg.slot() for _ in range(2)]
            junk3 = pg.sb("junk3", [128, D], BF16)
            ss2 = pg.sb("ss2", [128, 16], F32)
            rs2 = pg.sb("rs2", [128, 16], F32)
            n2f = [pg.sb("n2f%d" % i, [128, D], F32) for i in range(2)]
            n2b = [pg.sb("n2b%d" % i, [128, D], BF16) for i in range(2)]
            n2s = [pg.slot() for _ in range(2)]
            n2T = pg.sb("n2T", [128, 8, 128], F32)
            lg = pg.sb("lg", [128, 36], F32)
            sm = pg.sb("sm", [128, 16], F32)
            gmask = pg.sb("gmask", [128, 4], F32)
            pen = pg.sb("pen", [128, 4], F32)
            gex = pg.sb("gex", [128, 4], F32)
            elm = pg.sb("elm", [128, 32], F32)
            elm2 = pg.sb("elm2", [128, 32], F32)
            mk1 = pg.sb("mk1", [128, 32], F32)
            mk2 = pg.sb("mk2", [128, 32], F32)
            cnt = pg.sb("cnt", [128, 32], F32)
            tot = pg.sb("tot", [128, 32], F32)
            pos = pg.sb("pos", [128, 32], F32)
            tmp32 = pg.sb("tmp32", [128, 32], F32)
            ps_h = [carve("ps_h%d" % i, i, 0, 512) for i in range(2)]
            ps_t = [carve("ps_t%d" % i, 2 + i, 0, 512, F32, "p (a b) -> p a b", a=4) for i in range(2)]
            ps_y = [carve("ps_y%d" % i, 4 + i, 0, 512) for i in range(2)]
            ps_a2 = [Tile(banks[0][:, 0:256], "ps_a0", bank_buf[0]), Tile(banks[2][:, 0:256], "ps_a1", bank_buf[2])]
            ps_b2 = [Tile(banks[1][:, 0:256], "ps_b0", bank_buf[1]), Tile(banks[3][:, 0:256], "ps_b1", bank_buf[3])]
            ps_x = Tile(banks[7][:, 0:256].bitcast(BF16).rearrange("p (a b) -> p a b", a=2), "ps_x", bank_buf[7])
            ps_l = Tile(banks[7][:, 256:292], "ps_l", bank_buf[7])
            ps_r = Tile(banks[7][:, 320:352], "ps_r", bank_buf[7])
            ps_c = Tile(banks[7][:, 352:384], "ps_c", bank_buf[7])

            S.dma(lambda e: e.dma_start(out=ridx.t[:], in_=rowidx), cslot, W=[ridx])
            S.dma(lambda e: e.dma_start(out=g2T.t[:], in_=g2r), cslot, W=[g2T])
            S.dma(lambda e: e.dma_start(out=gfT.t[:], in_=gfr), cslot, W=[gfT])
            S.dma(lambda e: e.dma_start(out=brT.t[:], in_=brr), cslot, W=[brT])
            S.dma(lambda e: e.dma_start(out=wrT.t[:], in_=wr.rearrange("(c p) n -> p c n", p=128)), cslot, W=[wrT])
            for kc in range(8):
                G.dma(lambda e, kc=kc: e.dma_start(out=Wo.t[:, kc, :], in_=w_out[kc * 128:(kc + 1) * 128, :]), cslot_g, W=[Wo])
            V.op(lambda e: e.memset(ss2.t[:], 0.0), W=[ss2])
            V.op(lambda e: e.memset(ss3.t[:], 0.0), W=[ss3])
            V.op(lambda e: e.memset(tot.t[:], 0.0), W=[tot])
            V.op(lambda e: e.memset(sm.t[:], 0.0), W=[sm])
            oslot = pg.slot()
            for c in range(8):
                G.dma(lambda e, c=c: e.indirect_dma_start(out=oT.t[:, c, :], out_offset=None, in_=b_all,
                                                          in_offset=bass.IndirectOffsetOnAxis(ap=ridx.t[:, c:c + 1], axis=0)),
                      oslot, R=b_buf + [ridx], W=[oT])
            wch = [(c // 2) + 4 * (c % 2) for c in range(8)]

            def rstd_ops(ssT, rsT, ti, n, eps):
                A.op(lambda e: e.activation(out=rsT.t[:, ti:ti + 1], in_=ssT.t[:, ti:ti + 1], func=AF.Sqrt, scale=1.0 / n, bias=epsT.t[:, 0:1]),
                     R=[ssT, epsT], W=[rsT])
                V.op(lambda e: e.reciprocal(out=rsT.t[:, ti:ti + 1], in_=rsT.t[:, ti:ti + 1]), R=[rsT], W=[rsT])

            for ti in range(16):
                p2 = ti % 2
                tsl = slice(ti * 128, (ti + 1) * 128)
                S.dma(lambda e, tsl=tsl, p2=p2: e.dma_start(out=xr[p2].t[:], in_=xres[tsl, :]), xrs[p2], W=[xr[p2]])
                for half in range(2):
                    for c in range(8):
                        P.op(lambda e, c=c, half=half, tsl=tsl: e.matmul(
                            out=ps_h[half].t[:], lhsT=oT.t[:, c, tsl], rhs=Wo.t[:, wch[c], half * 512:(half + 1) * 512],
                            start=(c == 0), stop=(c == 7)), R=[oT, Wo], W=[ps_h[half]])
                    V.op(lambda e, half=half, ti=ti, p2=p2: e.tensor_tensor(
                        out=hh.t[:, ti, half * 512:(half + 1) * 512], in0=ps_h[half].t[:], in1=xr[p2].t[:, half * 512:(half + 1) * 512],
                        op=ALU.add), R=[ps_h[half], xr[p2]], W=[hh])
                A.op(lambda e, ti=ti: e.activation(out=junk3.t[:], in_=hh.t[:, ti, :], func=AF.Square, accum_out=ss2.t[:, ti:ti + 1]),
                     R=[hh], W=[junk3, ss2])
                rstd_ops(ss2, rs2, ti, D, EPS)
                V.op(lambda e, ti=ti, p2=p2: e.scalar_tensor_tensor(out=n2f[p2].t[:], in0=hh.t[:, ti, :], scalar=rs2.t[:, ti:ti + 1],
                                                                    in1=g2T.t[:], op0=ALU.mult, op1=ALU.mult),
                     R=[hh, rs2, g2T], W=[n2f[p2]])
                A.op(lambda e, p2=p2: e.activation(out=n2b[p2].t[:], in_=n2f[p2].t[:], func=AF.Copy), R=[n2f[p2]], W=[n2b[p2]])
                for q in range(2):
                    for c4 in range(4):
                        c = q * 4 + c4
                        P.op(lambda e, c=c, c4=c4, q=q, p2=p2: e.transpose(out=ps_t[q].t[:, c4, :], in_=n2f[p2].t[:, c * 128:(c + 1) * 128],
                                                                           identity=identF), R=[n2f[p2], cfT], W=[ps_t[q]])
                    if q == 0:
                        A.op(lambda e: e.activation(out=n2T.t[:, 0:4, :], in_=ps_t[0].t[:], func=AF.Copy), R=[ps_t[0]], W=[n2T])
                    else:
                        V.op(lambda e: e.tensor_copy(out=n2T.t[:, 4:8, :], in_=ps_t[1].t[:]), R=[ps_t[1]], W=[n2T])
                for c in range(8):
                    P.op(lambda e, c=c: e.matmul(out=ps_l.t[:], lhsT=n2T.t[:, c, :], rhs=wrT.t[:, c, :], start=(c == 0), stop=(c == 7)),
                         R=[n2T, wrT], W=[ps_l])
                V.op(lambda e: e.tensor_tensor(out=lg.t[:], in0=ps_l.t[:], in1=brT.t[:], op=ALU.add), R=[ps_l, brT], W=[lg])
                gl = lg.t[:, 0:4]
                el = lg.t[:, 4:36]
                c_ = lambda k: sm.t[:, k:k + 1]
                V.op(lambda e: e.reduce_max(out=c_(0), in_=gl, axis=AX.X), R=[lg], W=[sm])
                V.op(lambda e: e.tensor_scalar(out=gmask.t[:], in0=gl, scalar1=c_(0), scalar2=None, op0=ALU.is_equal), R=[lg, sm], W=[gmask])
                V.op(lambda e: e.tensor_single_scalar(out=c_(1), in_=c_(0), scalar=-1.0, op=ALU.mult), R=[sm], W=[sm])
                V.op(lambda e: e.memset(c_(2), 0.0), W=[sm])
                A.op(lambda e: e.activation(out=gex.t[:], in_=gl, func=AF.Exp, bias=c_(1), accum_out=c_(2)), R=[lg, sm], W=[gex, sm])
                V.op(lambda e: e.reciprocal(out=c_(3), in_=c_(2)), R=[sm], W=[sm])
                V.op(lambda e: e.tensor_scalar(out=pen.t[:], in0=gmask.t[:], scalar1=-1.0, scalar2=BIG, op0=ALU.add, op1=ALU.mult),
                     R=[gmask], W=[pen])
                for g in range(4):
                    V.op(lambda e, g=g: e.tensor_scalar(out=elm.t[:, g * 8:(g + 1) * 8], in0=lg.t[:, 4 + g * 8:12 + g * 8],
                                                        scalar1=pen.t[:, g:g + 1], scalar2=None, op0=ALU.add), R=[lg, pen], W=[elm])
                V.op(lambda e: e.reduce_max(out=c_(4), in_=elm.t[:], axis=AX.X), R=[elm], W=[sm])
                V.op(lambda e: e.tensor_scalar(out=mk1.t[:], in0=elm.t[:], scalar1=c_(4), scalar2=None, op0=ALU.is_equal), R=[elm, sm], W=[mk1])
                V.op(lambda e: e.scalar_tensor_tensor(out=elm2.t[:], in0=mk1.t[:], scalar=-BIG, in1=elm.t[:], op0=ALU.mult, op1=ALU.add),
                     R=[mk1, elm], W=[elm2])
                V.op(lambda e: e.reduce_max(out=c_(5), in_=elm2.t[:], axis=AX.X), R=[elm2], W=[sm])
                V.op(lambda e: e.tensor_scalar(out=mk2.t[:], in0=elm2.t[:], scalar1=c_(5), scalar2=None, op0=ALU.is_equal), R=[elm2, sm], W=[mk2])
                V.op(lambda e: e.tensor_tensor(out=c_(6), in0=c_(5), in1=c_(4), op=ALU.subtract), R=[sm], W=[sm])
                A.op(lambda e: e.activation(out=c_(7), in_=c_(6), func=AF.Exp), R=[sm], W=[sm])
                V.op(lambda e: e.tensor_single_scalar(out=c_(8), in_=c_(7), scalar=1.0, op=ALU.add), R=[sm], W=[sm])
                V.op(lambda e: e.reciprocal(out=c_(9), in_=c_(8)), R=[sm], W=[sm])
                V.op(lambda e, ti=ti: e.tensor_tensor(out=gates.t[:, ti, 0:1], in0=c_(9), in1=c_(3), op=ALU.mult), R=[sm], W=[gates])
                V.op(lambda e, ti=ti: e.tensor_tensor(out=gates.t[:, ti, 1:2], in0=gates.t[:, ti, 0:1], in1=c_(7), op=ALU.mult),
                     R=[sm, gates], W=[gates])
                V.op(lambda e: e.tensor_tensor(out=cnt.t[:], in0=mk1.t[:], in1=mk2.t[:], op=ALU.add), R=[mk1, mk2], W=[cnt])
                P.op(lambda e: e.matmul(out=ps_r.t[:], lhsT=strictT, rhs=cnt.t[:], start=True, stop=True), R=[cnt, cfT], W=[ps_r])
                P.op(lambda e: e.matmul(out=ps_c.t[:], lhsT=onesF, rhs=cnt.t[:], start=True, stop=True), R=[cnt, cfT], W=[ps_c])
                V.op(lambda e: e.tensor_tensor(out=pos.t[:], in0=ps_r.t[:], in1=tot.t[:], op=ALU.add), R=[ps_r, tot], W=[pos])
                V.op(lambda e: e.tensor_tensor(out=tot.t[:], in0=ps_c.t[:], in1=tot.t[:], op=ALU.add), R=[ps_c, tot], W=[tot])
                V.op(lambda e: e.tensor_single_scalar(out=pos.t[:], in_=pos.t[:], scalar=float(CAP - 1), op=ALU.min), R=[pos], W=[pos])
                V.op(lambda e: e.tensor_tensor(out=pos.t[:], in0=pos.t[:], in1=ecol, op=ALU.add), R=[pos, cfT], W=[pos])
                for k, mk in enumerate((mk1, mk2)):
                    V.op(lambda e, mk=mk: e.tensor_tensor(out=tmp32.t[:], in0=pos.t[:], in1=mk.t[:], op=ALU.mult), R=[pos, mk], W=[tmp32])
                    V.op(lambda e, k=k, ti=ti: e.reduce_sum(out=dstf.t[:, ti, k:k + 1], in_=tmp32.t[:], axis=AX.X), R=[tmp32], W=[dstf])
                V.op(lambda e, ti=ti: e.tensor_copy(out=dsti.t[:, ti, :], in_=dstf.t[:, ti, :]), R=[dstf], W=[dsti])
                for k in range(2):
                    G.dma(lambda e, k=k, ti=ti, p2=p2: e.indirect_dma_start(
                        out=Xs, out_offset=bass.IndirectOffsetOnAxis(ap=dsti.t[:, ti, k:k + 1], axis=0), in_=n2b[p2].t[:, :], in_offset=None),
                        n2s[p2], R=[n2b[p2], dsti], W=[Xs_buf])
            if debug:
                S.dma(lambda e: e.dma_start(out=dbg_h.rearrange("(t p) d -> p t d", p=128), in_=hh.t[:]), dslot, R=[hh], W=[out_buf])

            es2a.close()
            es2b = ExitStack()
            pg.es = es2b
            junk4 = pg.sb("junk3b", [128, D], BF16)
            xg = [pg.sb("xg%d" % i, [128, 2, D], BF16) for i in range(2)]
            xgs = [pg.slot() for _ in range(2)]
            xT = [pg.sb("xT%d" % i, [128, 8, 256], BF16) for i in range(2)]
            w1s = [pg.sb("w1s%d" % i, [128, 8, 512], BF16) for i in range(2)]
            w3s = [pg.sb("w3s%d" % i, [128, 8, 512], BF16) for i in range(2)]
            w2s = [pg.sb("w2s%d" % i, [128, 4, D], BF16) for i in range(2)]
            wsl = [pg.slot() for _ in range(2)]
            sa = [pg.sb("sa%d" % i, [128, 256], F32) for i in range(2)]
            hT = [pg.sb("hT%d" % i, [128, 4, 256], BF16) for i in range(2)]
            ysb = [pg.sb("ysb%d" % i, [128, 2, D], F32) for i in range(1)]
            yss = [pg.slot() for _ in range(2)]
            ya = [pg.sb("ya%d" % i, [128, D], F32) for i in range(1)]
            yb = [pg.sb("yb%d" % i, [128, D], F32) for i in range(1)]
            ygs = [pg.slot() for _ in range(2)]
            ot = [pg.sb("ot%d" % i, [128, D], F32) for i in range(2)]
            ots = [pg.slot() for _ in range(2)]
            Xs_all = Buf("Xs_all")
            Xs_all.w = dict(Xs_buf.w)
            for p2 in range(2):
                Xs_all.w[id(n2s[p2].s)] = (n2s[p2].s, n2s[p2].v)
            def eloads(ex):
                p2 = ex % 2
                S.dma(lambda e, ex=ex, p2=p2: e.dma_start(out=xg[p2].t[:], in_=Xs[ex * CAP:(ex + 1) * CAP, :].rearrange("(s p) f -> p s f", p=128)),
                      xgs[p2], R=[Xs_all], W=[xg[p2]])
                S.dma(lambda e, ex=ex, p2=p2: e.dma_start(out=w1s[p2].t[:], in_=w1b[ex].rearrange("(c p) f -> p c f", p=128)), wsl[p2], R=[wb_buf[ex]], W=[w1s[p2]])
                S.dma(lambda e, ex=ex, p2=p2: e.dma_start(out=w3s[p2].t[:], in_=w3b[ex].rearrange("(c p) f -> p c f", p=128)), wsl[p2], R=[wb_buf[ex]], W=[w3s[p2]])
                S.dma(lambda e, ex=ex, p2=p2: e.dma_start(out=w2s[p2].t[:], in_=w2b[ex].rearrange("(c p) f -> p c f", p=128)), wsl[p2], R=[wb_buf[ex]], W=[w2s[p2]])

            eloads(0)
            for ex in range(NE):
                p2 = ex % 2
                if ex + 1 < NE:
                    eloads(ex + 1)
                for cq in range(4):
                    for cc in range(2):
                        c = cq * 2 + cc
                        for s in range(2):
                            P.op(lambda e, c=c, cc=cc, s=s, p2=p2: e.transpose(out=ps_x.t[:, cc, s * 128:(s + 1) * 128],
                                                                               in_=xg[p2].t[:, s, c * 128:(c + 1) * 128], identity=identB),
                                 R=[xg[p2], cbT], W=[ps_x])
                    if cq % 2 == 0:
                        V.op(lambda e, cq=cq, p2=p2: e.tensor_copy(out=xT[p2].t[:, cq * 2:cq * 2 + 2, :], in_=ps_x.t[:]), R=[ps_x], W=[xT[p2]])
                    else:
                        A.op(lambda e, cq=cq, p2=p2: e.activation(out=xT[p2].t[:, cq * 2:cq * 2 + 2, :], in_=ps_x.t[:], func=AF.Copy),
                             R=[ps_x], W=[xT[p2]])
                for fc in range(4):
                    f2 = fc % 2
                    ps_a = ps_a2[f2]
                    ps_b = ps_b2[f2]
                    for c in range(8):
                        P.op(lambda e, c=c, fc=fc, p2=p2, ps_a=ps_a: e.matmul(out=ps_a.t[:], lhsT=w1s[p2].t[:, c, fc * 128:(fc + 1) * 128], rhs=xT[p2].t[:, c, :],
                                                                   start=(c == 0), stop=(c == 7)), R=[w1s[p2], xT[p2]], W=[ps_a])
                    for c in range(8):
                        P.op(lambda e, c=c, fc=fc, p2=p2, ps_b=ps_b: e.matmul(out=ps_b.t[:], lhsT=w3s[p2].t[:, c, fc * 128:(fc + 1) * 128], rhs=xT[p2].t[:, c, :],
                                                                   start=(c == 0), stop=(c == 7)), R=[w3s[p2], xT[p2]], W=[ps_b])
                    A.op(lambda e, f2=f2, ps_a=ps_a: e.activation(out=sa[f2].t[:], in_=ps_a.t[:], func=AF.Silu), R=[ps_a], W=[sa[f2]])
                    V.op(lambda e, f2=f2, fc=fc, p2=p2, ps_b=ps_b: e.tensor_tensor(out=hT[p2].t[:, fc, :], in0=ps_b.t[:], in1=sa[f2].t[:], op=ALU.mult),
                         R=[ps_b, sa[f2]], W=[hT[p2]])
                for s in range(2):
                    for half in range(2):
                        yy = ps_y[half]
                        for fc in range(4):
                            P.op(lambda e, fc=fc, s=s, half=half, p2=p2, yy=yy: e.matmul(
                                out=yy.t[:], lhsT=hT[p2].t[:, fc, s * 128:(s + 1) * 128], rhs=w2s[p2].t[:, fc, half * 512:(half + 1) * 512],
                                start=(fc == 0), stop=(fc == 3)), R=[hT[p2], w2s[p2]], W=[yy])
                        if half == 0:
                            V.op(lambda e, s=s, p2=p2, yy=yy: e.tensor_copy(out=ysb[0].t[:, s, 0:512], in_=yy.t[:]), R=[yy], W=[ysb[0]])
                        else:
                            A.op(lambda e, s=s, p2=p2, yy=yy: e.activation(out=ysb[0].t[:, s, 512:1024], in_=yy.t[:], func=AF.Copy), R=[yy], W=[ysb[0]])
                G.dma(lambda e, ex=ex, p2=p2: e.dma_start(out=Ys[ex * CAP:(ex + 1) * CAP, :].rearrange("(s p) f -> p s f", p=128), in_=ysb[0].t[:]),
                      yss[0], R=[ysb[0]], W=[Ys_buf[ex]])

            for ti in range(16):
                p2 = ti % 2
                G.dma(lambda e, ti=ti, p2=p2: e.indirect_dma_start(out=ya[0].t[:, :], out_offset=None, in_=Ys,
                                                                   in_offset=bass.IndirectOffsetOnAxis(ap=dsti.t[:, ti, 0:1], axis=0)),
                      ygs[0], R=Ys_buf + [dsti], W=[ya[0]])
                G.dma(lambda e, ti=ti, p2=p2: e.indirect_dma_start(out=yb[0].t[:, :], out_offset=None, in_=Ys,
                                                                   in_offset=bass.IndirectOffsetOnAxis(ap=dsti.t[:, ti, 1:2], axis=0)),
                      ygs[0], R=Ys_buf + [dsti], W=[yb[0]])
                V.op(lambda e, ti=ti, p2=p2: e.scalar_tensor_tensor(out=hh.t[:, ti, :], in0=ya[0].t[:], scalar=gates.t[:, ti, 0:1], in1=hh.t[:, ti, :],
                                                                    op0=ALU.mult, op1=ALU.add), R=[ya[0], gates, hh], W=[hh])
                V.op(lambda e, ti=ti, p2=p2: e.scalar_tensor_tensor(out=hh.t[:, ti, :], in0=yb[0].t[:], scalar=gates.t[:, ti, 1:2], in1=hh.t[:, ti, :],
                                                                    op0=ALU.mult, op1=ALU.add), R=[yb[0], gates, hh], W=[hh])
                A.op(lambda e, ti=ti: e.activation(out=junk4.t[:], in_=hh.t[:, ti, :], func=AF.Square, accum_out=ss3.t[:, ti:ti + 1]),
                     R=[hh], W=[junk4, ss3])
                rstd_ops(ss3, rs3, ti, D, EPS)
                V.op(lambda e, ti=ti, p2=p2: e.scalar_tensor_tensor(out=ot[p2].t[:], in0=hh.t[:, ti, :], scalar=rs3.t[:, ti:ti + 1], in1=gfT.t[:],
                                                                    op0=ALU.mult, op1=ALU.mult), R=[hh, rs3, gfT], W=[ot[p2]])
                S.dma(lambda e, ti=ti, p2=p2: e.dma_start(out=out[ti * 128:(ti + 1) * 128, :], in_=ot[p2].t[:]), ots[p2], R=[ot[p2]], W=[out_buf])
            fin = Buf("fin")
            for sl in ots + [dslot]:
                if sl.v:
                    fin.w[id(sl.s)] = (sl.s, sl.v)
            S.wait_all([fin])
            es2b.close()
            pg.es = es0
        else:
            fin = Buf("fin")
            if dslot.v:
                fin.w[id(dslot.s)] = (dslot.s, dslot.v)
            S.wait_all([fin])
        pg.emit()
    return nc


def _consts():
    a = np.arange(128)
    ident = np.eye(128, dtype=np.float32)
    triu = (a[:, None] <= a[None, :]).astype(np.float32)
    strict = (a[:, None] < a[None, :]).astype(np.float32)
    ones = np.ones((128, 128), np.float32)
    ecol = np.tile((np.arange(NE) * CAP).astype(np.float32)[None, :], (128, 1))
    cf = np.concatenate([ident, triu, strict, ones, ecol], axis=1)
    L = (a[:, None] >= a[None, :]).astype(np.float32)
    U = triu
    cb = np.concatenate([ident, L, U, L, U], axis=1).astype(ml_dtypes.bfloat16)
    return np.ascontiguousarray(cf), np.ascontiguousarray(cb)


def make_in_maps(x, norm1_g, w_in, gla_gate_w2, gla_gate_b, gla_norm_g, w_out, norm2_g,
                 router_group_w, router_group_b, router_expert_w, router_expert_b,
                 expert_w1, expert_w3, expert_w2, final_norm_g):
    f = lambda a: np.ascontiguousarray(np.asarray(a, dtype=np.float32))
    x = f(x)
    win = f(w_in)[0]
    cf, cb = _consts()
    gq0, gk0, gv0, gr0, glr0, aq0, ak0, av0 = 0, 256, 512, 1024, 1536, 1552, 2064, 2576
    w1 = f(expert_w1)[0]
    w3 = f(expert_w3)[0]
    w2 = f(expert_w2)[0]
    wo = f(w_out)[0]
    g2r = f(np.tile(np.asarray(norm2_g)[0][None, :], (128, 1)))
    gfr = f(np.tile(np.asarray(final_norm_g)[None, :], (128, 1)))
    wr = f(np.concatenate([np.asarray(router_group_w)[0], np.asarray(router_expert_w)[0]], axis=1))
    br = np.concatenate([np.asarray(router_group_b)[0], np.asarray(router_expert_b)[0]])
    brr = f(np.tile(br[None, :], (128, 1)))
    g1 = f(np.asarray(norm1_g)[0].reshape(8, 128).T)
    gng = f(np.tile(np.asarray(gla_norm_g)[0][None, :], (128, 4)))
    maps = []
    for c in range(8):
        b, j = c // 4, c % 4
        cols = np.concatenate([
            np.arange(gq0 + 64 * j, gq0 + 64 * j + 64), np.arange(gk0 + 64 * j, gk0 + 64 * j + 64),
            np.arange(aq0 + 128 * j, aq0 + 128 * j + 128), np.arange(ak0 + 128 * j, ak0 + 128 * j + 128),
            np.arange(av0 + 128 * j, av0 + 128 * j + 128), np.arange(glr0, glr0 + 16),
            np.arange(gk0 + 64 * j, gk0 + 64 * j + 64), np.arange(gv0 + 128 * j, gv0 + 128 * j + 128),
            np.arange(gr0 + 128 * j, gr0 + 128 * j + 128)])
        w2aug = np.concatenate([np.asarray(gla_gate_w2)[0][:, 64 * j:64 * j + 64],
                                np.asarray(gla_gate_b)[0][None, 64 * j:64 * j + 64]], axis=0)
        rowidx = (j * 1024 + np.arange(8)[None, :] * 128 + np.arange(128)[:, None]).astype(np.int32)
        maps.append({
            "x": x[b], "xres": np.ascontiguousarray(x[b, 2048 * j:2048 * (j + 1)]),
            "w_in": np.ascontiguousarray(win[:, cols]), "g1": g1, "w2aug": f(w2aug), "gng": gng,
            "w_out": wo, "g2r": g2r, "gfr": gfr, "wr": wr, "brr": brr, "w1": w1, "w3": w3, "w2": w2,
            "cf": cf, "cb": cb, "rowidx": np.ascontiguousarray(rowidx),
        })
    return maps


_NC_CACHE = {}


def kernel(**inputs):
    if "nc" not in _NC_CACHE:
        _NC_CACHE["nc"] = build()
    nc = _NC_CACHE["nc"]
    maps = make_in_maps(**inputs)
    res = run_bass_kernel_spmd(nc, maps, core_ids=list(range(8)))
    outs = [np.asarray(res.results[c]["out"], dtype=np.float32) for c in range(8)]
    y = np.stack([np.concatenate(outs[0:4], axis=0), np.concatenate(outs[4:8], axis=0)], axis=0)
    return y
```

```python
import numpy as np
import ml_dtypes
from contextlib import ExitStack
import concourse.bass as bass
import concourse.mybir as mybir
from concourse.bass_utils import run_bass_kernel_spmd

F32 = mybir.dt.float32
BF16 = mybir.dt.bfloat16
I32 = mybir.dt.int32
AF = mybir.ActivationFunctionType
ALU = mybir.AluOpType
AX = mybir.AxisListType

T = 8192
D = 1024
NSB = 16
NSUP = 4
CAP = 256
NE = 32
EPS = 1e-6
BIG = 1.0e30
RING = 6144
SEM_LIMIT = 12000
import os
ATT_MODE = int(os.environ.get('ATT_MODE', '3'))
MASK_ENG = os.environ.get('MASK_ENG', 'V')
ATT_NOTR = int(os.environ.get('ATT_NOTR', '0'))
ATT_DS = tuple(int(v) for v in os.environ.get('ATT_DS', '1,4,16').split(','))


class Buf:
    __slots__ = ("name", "w", "r", "psum")

    def __init__(self, name, psum=False):
        self.name = name
        self.w = {}
        self.r = {}
        self.psum = psum


class Tile:
    def __init__(self, t, name, buf=None):
        self.t = t
        self.b = buf if buf is not None else Buf(name)


def _split_psum(R, W):
    R2, W2 = [], list(W)
    for x in R:
        b = x.b if isinstance(x, Tile) else x
        if b.psum:
            W2.append(x)
        else:
            R2.append(x)
    return R2, W2


class SemSlot:
    def __init__(self, sem):
        self.s = sem
        self.v = 0


class Eng:
    def __init__(self, prog, name, is_pe=False):
        self.prog = prog
        self.name = name
        self.is_pe = is_pe
        self.sem = prog.new_sem()
        self.cnt = 0
        self.known = {}
        self.thunks = []

    def _wait(self, sem, val):
        if self.known.get(id(sem), 0) >= val:
            return
        self.known[id(sem)] = val
        self.thunks.append(lambda e, s=sem, v=val: e.wait_ge(s, v))

    def _deps(self, R, W):
        for x in R:
            b = x.b if isinstance(x, Tile) else x
            for (sem, val) in b.w.values():
                if sem is self.sem and self.is_pe:
                    continue
                self._wait(sem, val)
        for x in W:
            b = x.b if isinstance(x, Tile) else x
            for (sem, val) in list(b.w.values()) + list(b.r.values()):
                if sem is self.sem and self.is_pe:
                    continue
                self._wait(sem, val)

    def _record(self, R, W, sem, val):
        for x in R:
            b = x.b if isinstance(x, Tile) else x
            b.r[id(sem)] = (sem, val)
        for x in W:
            b = x.b if isinstance(x, Tile) else x
            b.w = {id(sem): (sem, val)}
            b.r = {}

    def op(self, fn, R=(), W=()):
        R, W = _split_psum(R, W)
        self._deps(R, W)
        if self.cnt >= SEM_LIMIT:
            self.sem = self.prog.new_sem()
            self.cnt = 0
        self.cnt += 1
        sem, val = self.sem, self.cnt
        self.thunks.append(lambda e, f=fn, s=sem: f(e).then_inc(s, 1))
        self._record(R, W, sem, val)

    def dma(self, fn, slot, R=(), W=()):
        self._deps(R, W)
        slot.v += 16
        sem, val = slot.s, slot.v
        self.thunks.append(lambda e, f=fn, s=sem: f(e).then_inc(s, 16))
        self._record(R, W, sem, val)

    def coll(self, fn, R=(), W=()):
        self._deps(R, W)
        sem = self.prog.new_sem()
        self.thunks.append(lambda e, f=fn, s=sem: f(e).then_inc(s))
        self._record(R, W, sem, 1)

    def wait_all(self, bufs):
        self._deps(bufs, ())


class Prog:
    def __init__(self, nc, es):
        self.nc = nc
        self.es = es
        self.es_sem = es
        self.nsem = 0
        self.V = Eng(self, "vector")
        self.A = Eng(self, "scalar")
        self.G = Eng(self, "gpsimd")
        self.P = Eng(self, "tensor", is_pe=True)
        self.S = Eng(self, "sync")

    def new_sem(self):
        self.nsem += 1
        return self.es_sem.enter_context(self.nc.semaphore("s%d" % self.nsem))

    def slot(self):
        return SemSlot(self.new_sem())

    def sb(self, name, shape, dt):
        return Tile(self.es.enter_context(self.nc.sbuf_tensor(name, shape, dt)), name)

    def ps(self, name, shape, dt):
        return Tile(self.es.enter_context(self.nc.psum_tensor(name, shape, dt)), name)

    def emit(self):
        with self.nc.Block() as block:
            @block.vector
            def _(e):
                for th in self.V.thunks:
                    th(e)

            @block.scalar
            def _(e):
                for th in self.A.thunks:
                    th(e)

            @block.gpsimd
            def _(e):
                for th in self.G.thunks:
                    th(e)

            @block.tensor
            def _(e):
                for th in self.P.thunks:
                    th(e)

            @block.sync
            def _(e):
                for th in self.S.thunks:
                    th(e)


def build(debug=False, phase2=True, do_coll=True, nsb_run=NSB, do_gla=True, do_att=True):
    nc = bass.Bass("TRN2", target_bir_lowering=False)
    dram = lambda n, s, dt, k="ExternalInput": nc.dram_tensor(n, s, dt, kind=k).ap()
    x = dram("x", [T, D], F32)
    xres = dram("xres", [2048, D], F32)
    w_in = dram("w_in", [D, 848], F32)
    g1 = dram("g1", [128, 8], F32)
    w2aug = dram("w2aug", [17, 64], F32)
    gng = dram("gng", [128, 512], F32)
    w_out = dram("w_out", [D, D], F32)
    g2r = dram("g2r", [128, D], F32)
    gfr = dram("gfr", [128, D], F32)
    wr = dram("wr", [D, 36], F32)
    brr = dram("brr", [128, 36], F32)
    NEd = NE if phase2 else 1
    w1 = dram("w1", [NEd, D, 512], F32)
    w3 = dram("w3", [NEd, D, 512], F32)
    w2 = dram("w2", [NEd, 512, D], F32)
    cf = dram("cf", [128, 544], F32)
    cb = dram("cb", [128, 640], BF16)
    out = dram("out", [2048, D], F32, "ExternalOutput")
    a_int = [dram("a_int%d" % s, [256, 2048], BF16, "Internal") for s in range(NSUP)]
    b_all = dram("b_all", [NSUP * 1024, 2048], BF16, "Internal")
    rowidx = dram("rowidx", [128, 8], I32)
    w1b = dram("w1b", [NEd, D, 512], BF16, "Internal")
    w3b = dram("w3b", [NEd, D, 512], BF16, "Internal")
    w2b = dram("w2b", [NEd, 512, D], BF16, "Internal")
    wb_buf = [Buf("wb%d" % e) for e in range(NE)]
    Xs = dram("xs_scr", [NE * CAP, D], BF16, "Internal")
    Ys = dram("ys_scr", [NE * CAP, D], F32, "Internal")
    if debug:
        dbg_o = dram("dbg_o", [NSUP * 1024, 2048], BF16, "ExternalOutput")
        dbg_h = dram("dbg_h", [2048, D], F32, "ExternalOutput")
    a_buf = [Buf("a%d" % s) for s in range(NSUP)]
    b_buf = [Buf("b%d" % s) for s in range(NSUP)]
    Xs_buf = Buf("Xs")
    Ys_buf = [Buf("Ys%d" % e) for e in range(NE)]
    out_buf = Buf("out")

    with ExitStack() as es0:
        pg = Prog(nc, es0)
        V, A, G, P, S = pg.V, pg.A, pg.G, pg.P, pg.S
        cslot = pg.slot()
        cslot_g = pg.slot()
        banks = [es0.enter_context(nc.psum_tensor("bank%d" % i, [128, 512], F32)) for i in range(8)]
        bank_buf = [Buf("bank%d" % i, psum=True) for i in range(8)]

        def carve(name, bank, c0, ncol, dt=F32, pat=None, parts=128, **kw):
            ap = banks[bank][0:parts, c0:c0 + ncol]
            if dt is not F32:
                ap = ap.bitcast(dt)
            if pat is not None:
                ap = ap.rearrange(pat, **kw)
            return Tile(ap, name, bank_buf[bank])

        cfT = pg.sb("cfT", [128, 544], F32)
        cbT = pg.sb("cbT", [128, 640], BF16)
        S.dma(lambda e: e.dma_start(out=cfT.t[:], in_=cf), cslot, W=[cfT])
        S.dma(lambda e: e.dma_start(out=cbT.t[:], in_=cb), cslot, W=[cbT])
        epsT = pg.sb("epsT", [128, 2], F32)
        V.op(lambda e: e.memset(epsT.t[:, 0:1], EPS), W=[epsT])
        V.op(lambda e: e.memset(epsT.t[:, 1:2], 64 * EPS), W=[epsT])
        identF = cfT.t[:, 0:128]
        triU = cfT.t[:, 128:256]
        strictT = cfT.t[:, 256:384]
        onesF = cfT.t[:, 384:512]
        ecol = cfT.t[:, 512:544]
        identB = cbT.t[:, 0:128]
        mask4 = cbT.t[:, 128:640]

        with ExitStack() as es1:
            pg.es = es1
            NX = 6
            Wt = pg.sb("Wt", [128, 8, 848], BF16)
            g1T = pg.sb("g1T", [128, 8], F32)
            w2a = pg.sb("w2a", [17, 64], F32)
            gngT = pg.sb("gngT", [128, 4, 128], F32)
            xt = [pg.sb("xt%d" % i, [128, D], F32) for i in range(NX)]
            xsl = [pg.slot() for _ in range(NX)]
            junk = pg.sb("junk", [128, D], BF16)
            ss1 = pg.sb("ss1", [128, 64], F32)
            rs1 = pg.sb("rs1", [128, 64], F32)
            xs = [pg.sb("xs%d" % i, [128, D], BF16) for i in range(4)]
            nT = [pg.sb("nT%d" % i, [128, 8, 512], BF16) for i in range(2)]
            qT = [pg.sb("qT%d" % i, [64, 512], BF16) for i in range(2)]
            kT = [pg.sb("kT%d" % i, [64, 512], BF16) for i in range(2)]
            QT = [[pg.sb("QT%d_%d" % (h, i), [64, 2048], BF16) for i in range(2)] for h in range(2)]
            KT = [[pg.sb("KT%d_%d" % (h, i), [64, 2048], BF16) for i in range(3)] for h in range(2)]
            VT = [[pg.sb("VT%d_%d" % (h, i), [64, 2048], BF16) for i in range(3)] for h in range(2)]
            tm = [pg.sb("tm%d" % i, [128, 4, 320], BF16) for i in range(2)]
            glrT = [pg.sb("glrT%d" % i, [32, 512], F32) for i in range(2)]
            e1 = pg.sb("e1", [128, 256], F32)
            sp = pg.sb("sp", [128, 256], F32)
            Ek = pg.sb("Ek", [128, 256], F32)
            ktl = pg.sb("ktl", [128, 4, 64], BF16)
            EqT = pg.sb("EqT", [64, 512], F32)
            EkT = pg.sb("EkT", [64, 512], F32)
            qtl = pg.sb("qtl", [64, 512], BF16)
            ktlT = pg.sb("ktlT", [64, 512], BF16)
            gg = pg.sb("gg", [128, 4, 128], F32)
            AmT = [pg.sb("AmT%d" % i, [128, 128], BF16) for i in range(4)]
            og = [pg.sb("og%d" % i, [128, 128], BF16) for i in range(4)]
            Sst = pg.sb("Sst", [64, 128], F32)
            Sbf = pg.sb("Sbf", [64, 128], BF16)
            Stmp = pg.sb("Stmp", [64, 128], F32)
            ssg = pg.sb("ssg", [128, 64], F32)
            rsg = pg.sb("rsg", [128, 64], F32)
            junk2 = pg.sb("junk2", [128, 128], BF16)
            ogT = [pg.sb("ogT%d" % i, [128, 2048], BF16) for i in range(2)]
            pTa = [pg.sb("pTa%d" % i, [128, 512], BF16) for i in range(3)]
            vp = [pg.sb("vp%d" % i, [128, 4, 128], BF16) for i in range(3)]
            Oacc = pg.sb("Oacc", [128, 2, 2048], F32)
            rden = pg.sb("rden", [64, 1024], F32)
            oTa = [pg.sb("oTa%d" % i, [128, 2048], BF16) for i in range(2)]
            stsl = [pg.slot() for _ in range(2)]
            ps_pT_l = [carve("ps_pT", 0, 0, 512, BF16, "p (a b) -> p a b", a=8), carve("ps_pT2", 4, 0, 512, BF16, "p (a b) -> p a b", a=8)]
            ps_pr = [carve("ps_pr%d" % i, 1 + i, 0, 512) for i in range(2)]
            ps_GT = carve("ps_GT", 3, 0, 512, parts=64)
            ps_s = carve("ps_s", 4, 0, 512)
            ps_z = carve("ps_z", 5, 0, 256)
            ps_G = ps_z
            ps_U = Tile(banks[5][0:64, 256:384], "ps_U", bank_buf[5])
            ps_ogT_l = [Tile(banks[5][:, 384:448].bitcast(BF16), "ps_ogT0", bank_buf[5]),
                        Tile(banks[5][:, 448:512].bitcast(BF16), "ps_ogT1", bank_buf[5])]
            ps_A4 = carve("ps_A4", 6, 0, 512, F32, "p (a b) -> p a b", a=4)
            ps_og4 = carve("ps_og4", 7, 0, 512, F32, "p (a b) -> p a b", a=4)
            ps_o = Tile(banks[6][:, 0:256].rearrange("p (a b) -> p a b", a=2), "ps_o", bank_buf[6])
            ps_v = Tile(banks[7][:, 128:256].bitcast(BF16).rearrange("p (a b) -> p a b", a=4), "ps_v", bank_buf[7])
            ps_s_l = [ps_s, Tile(banks[5][:, :], "ps_s2", bank_buf[5]), Tile(banks[3][:, :], "ps_s3", bank_buf[3])]
            ps_o_l = [ps_o, Tile(banks[0][:, 0:256].rearrange("p (a b) -> p a b", a=2), "ps_o2", bank_buf[0])]
            ps_v_l = [ps_v, Tile(banks[1][:, 128:256].bitcast(BF16).rearrange("p (a b) -> p a b", a=4), "ps_v2", bank_buf[1])]

            for kc in range(8):
                G.dma(lambda e, kc=kc: e.dma_start(out=Wt.t[:, kc, :], in_=w_in[kc * 128:(kc + 1) * 128, :]),
                      cslot_g, W=[Wt])
            S.dma(lambda e: e.dma_start(out=g1T.t[:], in_=g1), cslot, W=[g1T])
            S.dma(lambda e: e.dma_start(out=w2a.t[:], in_=w2aug), cslot, W=[w2a])
            S.dma(lambda e: e.dma_start(out=gngT.t[:].rearrange("p a b -> p (a b)"), in_=gng), cslot, W=[gngT])
            for kc in range(8):
                V.op(lambda e, kc=kc: e.tensor_scalar(out=Wt.t[:, kc, :], in0=Wt.t[:, kc, :], scalar1=g1T.t[:, kc:kc + 1],
                                                      scalar2=None, op0=ALU.mult), R=[g1T, Wt], W=[Wt])
            V.op(lambda e: e.memset(ss1.t[:], 0.0), W=[ss1])
            V.op(lambda e: e.memset(ssg.t[:], 0.0), W=[ssg])
            V.op(lambda e: e.memset(Sst.t[:], 0.0), W=[Sst])
            V.op(lambda e: e.memset(Sbf.t[:], 0.0), W=[Sbf])
            for i in range(2):
                G.op(lambda e, i=i: e.memset(glrT[i].t[:], 1.0), W=[glrT[i]])
            for i in range(3):
                G.op(lambda e, i=i: e.memset(vp[i].t[:], 1.0), W=[vp[i]])

            pr_i = [0]

            def next_pr():
                pr_i[0] ^= 1
                return ps_pr[pr_i[0]]

            def stage1(sb):
                par = sb % 2
                sup = sb // 4
                for i in range(4):
                    ti = sb * 4 + i
                    sl = ti % NX
                    S.dma(lambda e, ti=ti, sl=sl: e.dma_start(out=xt[sl].t[:], in_=x[ti * 128:(ti + 1) * 128, :]),
                          xsl[sl], W=[xt[sl]])
                    A.op(lambda e, ti=ti, sl=sl: e.activation(out=junk.t[:], in_=xt[sl].t[:], func=AF.Square,
                                                              accum_out=ss1.t[:, ti:ti + 1]),
                         R=[xt[sl]], W=[junk, ss1])
                t0i = sb * 4
                A.op(lambda e: e.activation(out=rs1.t[:, t0i:t0i + 4], in_=ss1.t[:, t0i:t0i + 4], func=AF.Sqrt, scale=1.0 / D, bias=epsT.t[:, 0:1]),
                     R=[ss1, epsT], W=[rs1])
                V.op(lambda e: e.reciprocal(out=rs1.t[:, t0i:t0i + 4], in_=rs1.t[:, t0i:t0i + 4]), R=[rs1], W=[rs1])
                for i in range(4):
                    ti = sb * 4 + i
                    sl = ti % NX
                    xp = ti % 4
                    V.op(lambda e, ti=ti, sl=sl, xp=xp: e.tensor_scalar(out=xs[xp].t[:], in0=xt[sl].t[:],
                                                                        scalar1=rs1.t[:, ti:ti + 1], scalar2=None, op0=ALU.mult),
                         R=[xt[sl], rs1], W=[xs[xp]])
                yield "front_a"
                for i in range(4):
                    ti = sb * 4 + i
                    xp = ti % 4
                    ps_pT = ps_pT_l[ti % 2]
                    for kc in range(8):
                        P.op(lambda e, kc=kc, xp=xp, ps_pT=ps_pT: e.transpose(out=ps_pT.t[:, kc, :], in_=xs[xp].t[:, kc * 128:(kc + 1) * 128],
                                                                 identity=identB), R=[xs[xp], cbT], W=[ps_pT])
                    A.op(lambda e, i=i, par=par, ps_pT=ps_pT: e.activation(out=nT[par].t[:, :, i * 128:(i + 1) * 128], in_=ps_pT.t[:, :, :],
                                                              func=AF.Copy), R=[ps_pT], W=[nT[par]])
                yield "front_b"
                col = (sb % 4) * 512
                kslot = sup % 3
                fm = [
                    (0, 128, [(qT[par], qT[par].t[0:64, :]), (kT[par], kT[par].t[0:64, :])]),
                    (128, 128, [(QT[0][sup % 2], QT[0][sup % 2].t[:, col:col + 512]), (QT[1][sup % 2], QT[1][sup % 2].t[:, col:col + 512])]),
                    (256, 128, [(KT[0][kslot], KT[0][kslot].t[:, col:col + 512]), (KT[1][kslot], KT[1][kslot].t[:, col:col + 512])]),
                    (384, 128, [(VT[0][kslot], VT[0][kslot].t[:, col:col + 512]), (VT[1][kslot], VT[1][kslot].t[:, col:col + 512])]),
                    (512, 16, [(glrT[par], glrT[par].t[0:16, :])]),
                ]
                for gi, (c0, m, dsts) in enumerate(fm):
                    pp = next_pr()
                    for kc in range(8):
                        P.op(lambda e, kc=kc, c0=c0, m=m, pp=pp, par=par: e.matmul(
                            out=pp.t[0:m, :], lhsT=Wt.t[:, kc, c0:c0 + m], rhs=nT[par].t[:, kc, :],
                            start=(kc == 0), stop=(kc == 7)), R=[Wt, nT[par]], W=[pp])
                    if len(dsts) == 2:
                        (t0_, d0_), (t1_, d1_) = dsts
                        A.op(lambda e, pp=pp, d0_=d0_: e.activation(out=d0_, in_=pp.t[0:64, :], func=AF.Copy), R=[pp], W=[t0_])
                        V.op(lambda e, pp=pp, d1_=d1_: e.tensor_copy(out=d1_, in_=pp.t[64:128, :]), R=[pp], W=[t1_])
                    else:
                        (t0_, d0_), = dsts
                        V.op(lambda e, pp=pp, m=m, d0_=d0_: e.tensor_copy(out=d0_, in_=pp.t[0:m, :]), R=[pp], W=[t0_])
                    if gi == 1 or gi == 4:
                        yield "fm"
                for i in range(4):
                    pp = next_pr()
                    for kc in range(8):
                        P.op(lambda e, kc=kc, i=i, pp=pp, par=par: e.matmul(
                            out=pp.t[:, 0:320], lhsT=nT[par].t[:, kc, i * 128:(i + 1) * 128], rhs=Wt.t[:, kc, 528:848],
                            start=(kc == 0), stop=(kc == 7)), R=[Wt, nT[par]], W=[pp])
                    V.op(lambda e, pp=pp, i=i, par=par: e.tensor_copy(out=tm[par].t[:, i, 0:192], in_=pp.t[:, 0:192]),
                         R=[pp], W=[tm[par]])
                    A.op(lambda e, pp=pp, i=i, par=par: e.activation(out=tm[par].t[:, i, 192:320], in_=pp.t[:, 192:320],
                                                                     func=AF.Silu), R=[pp], W=[tm[par]])
                    if i == 1 or i == 3:
                        yield "tm"

            def gla(sb):
                par = sb % 2
                sup = sb // 4
                for i in range(4):
                    P.op(lambda e, i=i, par=par: e.matmul(out=ps_z.t[:, i * 64:(i + 1) * 64], lhsT=glrT[par].t[0:17, i * 128:(i + 1) * 128],
                                                          rhs=w2a.t[0:17, :], start=True, stop=True),
                         R=[glrT[par], w2a], W=[ps_z])
                A.op(lambda e: e.activation(out=e1.t[:], in_=ps_z.t[:], func=AF.Exp, scale=-1.0), R=[ps_z], W=[e1])
                A.op(lambda e: e.activation(out=sp.t[:], in_=e1.t[:], func=AF.Ln, bias=1.0), R=[e1], W=[sp])
                yield "p1"
                P.op(lambda e: e.matmul(out=ps_G.t[:], lhsT=triU, rhs=sp.t[:], start=True, stop=True), R=[sp, cfT], W=[ps_G])
                for i in range(4):
                    P.op(lambda e, i=i: e.matmul(out=ps_GT.t[:, i * 128:(i + 1) * 128], lhsT=sp.t[:, i * 64:(i + 1) * 64], rhs=triU,
                                                 start=True, stop=True), R=[sp, cfT], W=[ps_GT])
                A.op(lambda e: e.activation(out=Ek.t[:], in_=ps_G.t[:], func=AF.Exp, scale=1.0 / 16), R=[ps_G], W=[Ek])
                A.op(lambda e: e.activation(out=EqT.t[:], in_=ps_GT.t[:], func=AF.Exp, scale=-1.0 / 16), R=[ps_GT], W=[EqT])
                A.op(lambda e: e.activation(out=EkT.t[:], in_=ps_GT.t[:], func=AF.Exp, scale=1.0 / 16), R=[ps_GT], W=[EkT])
                V.op(lambda e, par=par: e.tensor_tensor(out=ktl.t[:, :, :], in0=tm[par].t[:, :, 0:64],
                                                        in1=Ek.t[:].rearrange("p (a b) -> p a b", a=4), op=ALU.mult),
                     R=[tm[par], Ek], W=[ktl])
                V.op(lambda e, par=par: e.tensor_tensor(out=qtl.t[:], in0=qT[par].t[:], in1=EqT.t[:], op=ALU.mult),
                     R=[qT[par], EqT], W=[qtl])
                V.op(lambda e, par=par: e.tensor_tensor(out=ktlT.t[:], in0=kT[par].t[:], in1=EkT.t[:], op=ALU.mult),
                     R=[kT[par], EkT], W=[ktlT])
                G.op(lambda e, par=par: e.tensor_tensor(out=gg.t[:], in0=tm[par].t[:, :, 192:320], in1=gngT.t[:], op=ALU.mult),
                     R=[tm[par], gngT], W=[gg])
                yield "p2"
                ch0 = sb * 4
                css = [slice(i * 128, (i + 1) * 128) for i in range(4)]
                vaps = [tm[par].t[:, i, 64:192] for i in range(4)]
                for i in range(4):
                    P.op(lambda e, i=i: e.matmul(out=ps_A4.t[:, i, :], lhsT=ktlT.t[:, css[i]], rhs=qtl.t[:, css[i]], start=True, stop=True),
                         R=[ktlT, qtl], W=[ps_A4])
                for i in range(4):
                    V.op(lambda e, i=i: e.tensor_tensor(out=AmT[i].t[:], in0=ps_A4.t[:, i, :], in1=triU, op=ALU.mult),
                         R=[ps_A4, cfT], W=[AmT[i]])
                yield "a"
                for i in range(4):
                    P.op(lambda e, i=i: e.matmul(out=ps_U.t[:], lhsT=ktl.t[:, i, :], rhs=vaps[i], start=True, stop=True),
                         R=[ktl, tm[par]], W=[ps_U])
                    P.op(lambda e, i=i: e.matmul(out=ps_og4.t[:, i, :], lhsT=AmT[i].t[:], rhs=vaps[i], start=True, stop=False),
                         R=[AmT[i], tm[par]], W=[ps_og4])
                    P.op(lambda e, i=i: e.matmul(out=ps_og4.t[:, i, :], lhsT=qtl.t[:, css[i]], rhs=Sbf.t[:], start=False, stop=True),
                         R=[qtl, Sbf], W=[ps_og4])
                    acol = EqT.t[:, i * 128 + 127:i * 128 + 128]
                    V.op(lambda e: e.tensor_tensor(out=Stmp.t[:], in0=ps_U.t[:], in1=Sst.t[:], op=ALU.add),
                         R=[ps_U, Sst], W=[Stmp])
                    A.op(lambda e, acol=acol: e.activation(out=Sbf.t[:], in_=Stmp.t[:], func=AF.Copy, scale=acol),
                         R=[Stmp, EqT], W=[Sbf])
                    V.op(lambda e, acol=acol: e.tensor_scalar(out=Sst.t[:], in0=Stmp.t[:], scalar1=acol, scalar2=None, op0=ALU.mult),
                         R=[Stmp, EqT], W=[Sst])
                yield "b"
                for i in range(4):
                    A.op(lambda e, i=i: e.activation(out=junk2.t[:], in_=ps_og4.t[:, i, :], func=AF.Square, accum_out=ssg.t[:, ch0 + i:ch0 + i + 1]),
                         R=[ps_og4], W=[junk2, ssg])
                A.op(lambda e: e.activation(out=rsg.t[:, ch0:ch0 + 4], in_=ssg.t[:, ch0:ch0 + 4], func=AF.Sqrt, scale=1.0 / 128, bias=epsT.t[:, 1:2]),
                     R=[ssg, epsT], W=[rsg])
                V.op(lambda e: e.reciprocal(out=rsg.t[:, ch0:ch0 + 4], in_=rsg.t[:, ch0:ch0 + 4]), R=[rsg], W=[rsg])
                for i in range(4):
                    V.op(lambda e, i=i: e.scalar_tensor_tensor(out=og[i].t[:], in0=ps_og4.t[:, i, :], scalar=rsg.t[:, ch0 + i:ch0 + i + 1],
                                                               in1=gg.t[:, i, :], op0=ALU.mult, op1=ALU.mult),
                         R=[ps_og4, rsg, gg], W=[og[i]])
                yield "c"
                for i in range(4):
                    pt = ps_ogT_l[i % 2]
                    P.op(lambda e, i=i, pt=pt: e.transpose(out=pt.t[:], in_=og[i].t[:], identity=identB), R=[og[i], cbT], W=[pt])
                    cc = ((ch0 + i) % 16) * 128
                    A.op(lambda e, cc=cc, sup=sup, pt=pt: e.activation(out=ogT[sup % 2].t[:, cc:cc + 128], in_=pt.t[:], func=AF.Copy),
                         R=[pt], W=[ogT[sup % 2]])

            def attention(sup):
                qp = sup % 2
                units = []
                for d in ATT_DS:
                    for r in range(d):
                        for n in range(16 // d):
                            units.append((d, r, n))
                ui = [0]

                def kcols(t0, d):
                    slot = (t0 // 2048) % 3
                    c0 = t0 % 2048
                    return slot, slice(c0, c0 + 127 * d + 1, d)

                def scores(u):
                    d, r, n = units[u]
                    sl3 = u % 3
                    qb = r + d * 128 * n
                    qsl = slice(qb, qb + 127 * d + 1, d)
                    t0 = sup * 2048 + qb
                    has_prev = (t0 - 128 * d) >= 0
                    cur = kcols(t0, d)
                    prev = kcols(t0 - 128 * d, d) if has_prev else cur
                    pss = ps_s_l[u % 3]
                    psv = ps_v_l[u % 2]
                    for h in range(2):
                        for blk, (ks, kc) in enumerate((prev, cur)):
                            P.op(lambda e, h=h, blk=blk, ks=ks, kc=kc, qsl=qsl, pss=pss: e.matmul(
                                out=pss.t[:, (h * 2 + blk) * 128:(h * 2 + blk + 1) * 128], lhsT=KT[h][ks].t[:, kc],
                                rhs=QT[h][qp].t[:, qsl], start=True, stop=True), R=[KT[h][ks], QT[h][qp]], W=[pss])
                    for h in range(2):
                        for blk, (ks, kc) in enumerate((prev, cur)):
                            P.op(lambda e, h=h, blk=blk, ks=ks, kc=kc, psv=psv: e.transpose(
                                out=psv.t[:, h * 2 + blk, :], in_=VT[h][ks].t[:, kc], identity=cbT.t[0:64, 0:64]),
                                R=[VT[h][ks], cbT], W=[psv])
                    A.op(lambda e, sl3=sl3, pss=pss: e.activation(out=pTa[sl3].t[:], in_=pss.t[:], func=AF.Exp, scale=0.125),
                         R=[pss], W=[pTa[sl3]])
                    V.op(lambda e, sl3=sl3, psv=psv: e.tensor_copy(out=vp[sl3].t[:, :, 0:64], in_=psv.t[:, :, :]), R=[psv], W=[vp[sl3]])
                    G.op(lambda e, sl3=sl3: e.tensor_tensor(out=pTa[sl3].t[:], in0=pTa[sl3].t[:], in1=mask4, op=ALU.mult),
                         R=[pTa[sl3], cbT], W=[pTa[sl3]])
                    return has_prev

                def pv(u, has_prev):
                    d, r, n = units[u]
                    sl3 = u % 3
                    qb = r + d * 128 * n
                    qsl = slice(qb, qb + 127 * d + 1, d)
                    blks = (0, 1) if has_prev else (1,)
                    ps_o = ps_o_l[u % 2]
                    for h in range(2):
                        for bi, blk in enumerate(blks):
                            P.op(lambda e, h=h, blk=blk, bi=bi, sl3=sl3, nb=len(blks), ps_o=ps_o: e.matmul(
                                out=ps_o.t[:, h, :], lhsT=vp[sl3].t[:, h * 2 + blk, :],
                                rhs=pTa[sl3].t[:, (h * 2 + blk) * 128:(h * 2 + blk + 1) * 128],
                                start=(bi == 0), stop=(bi == nb - 1)), R=[vp[sl3], pTa[sl3]], W=[ps_o])
                    if d == 1:
                        V.op(lambda e, qsl=qsl, ps_o=ps_o: e.tensor_copy(out=Oacc.t[:, :, qsl], in_=ps_o.t[:, :, :]), R=[ps_o], W=[Oacc])
                    else:
                        V.op(lambda e, qsl=qsl, ps_o=ps_o: e.tensor_tensor(out=Oacc.t[:, :, qsl], in0=ps_o.t[:, :, :], in1=Oacc.t[:, :, qsl],
                                                                op=ALU.add), R=[ps_o, Oacc], W=[Oacc])

                hp = {}
                for u in range(len(units) + 1):
                    if u < len(units):
                        hp[u] = scores(u)
                    if u >= 1 and ATT_MODE >= 2:
                        pv(u - 1, hp[u - 1])
                for h in range(2 if ATT_MODE >= 3 else 0):
                    for hf in range(2):
                        cs_ = slice(hf * 1024, (hf + 1) * 1024)
                        V.op(lambda e, h=h, cs_=cs_: e.reciprocal(out=rden.t[:], in_=Oacc.t[64:128, h, cs_]), R=[Oacc], W=[rden])
                        V.op(lambda e, h=h, cs_=cs_: e.tensor_tensor(out=oTa[qp].t[h * 64:(h + 1) * 64, cs_], in0=Oacc.t[0:64, h, cs_], in1=rden.t[:],
                                                                     op=ALU.mult), R=[Oacc, rden], W=[oTa[qp]])

            def exchange(sup):
                qp = sup % 2
                S.dma(lambda e: e.dma_start(out=a_int[sup][0:128, :], in_=ogT[qp].t[:]), stsl[qp], R=[ogT[qp]], W=[a_buf[sup]])
                S.dma(lambda e: e.dma_start(out=a_int[sup][128:256, :], in_=oTa[qp].t[:]), stsl[qp], R=[oTa[qp]], W=[a_buf[sup]])
                if do_coll:
                  G.coll(lambda e: e.collective_compute("AllGather", ALU.bypass, replica_groups=[[0, 1, 2, 3], [4, 5, 6, 7]],
                                                      ins=[a_int[sup]], outs=[b_all[sup * 1024:(sup + 1) * 1024, :]]), R=[a_buf[sup]], W=[b_buf[sup]])

            wcs = [pg.slot() for _ in range(4)]

            def precast(ex):
                for (src, dst) in ((w1, w1b), (w3, w3b), (w2, w2b)):
                    G.dma(lambda e, src=src, dst=dst, ex=ex: e.dma_start(
                        out=dst[ex].rearrange("(p r) f -> p (r f)", p=128), in_=src[ex].rearrange("(p r) f -> p (r f)", p=128)),
                        wcs[ex % 4], W=[wb_buf[ex]])

            gen1 = {}
            gen2 = {}

            def adv(gd, k):
                if k in gd:
                    if next(gd[k], None) is None:
                        del gd[k]

            for it in range(nsb_run + 2):
                if phase2 and it < NSB:
                    precast(2 * it)
                    precast(2 * it + 1)
                if it < nsb_run:
                    gen1[it] = stage1(it)
                if it >= 2 and do_gla:
                    gen2[it - 2] = gla(it - 2)
                adv(gen1, it)
                adv(gen2, it - 2)
                adv(gen1, it - 1)
                adv(gen2, it - 2)
                adv(gen1, it - 1)
                adv(gen2, it - 2)
                adv(gen1, it - 1)
                adv(gen2, it - 2)
                adv(gen1, it - 1)
                adv(gen2, it - 2)
                adv(gen1, it)
                adv(gen2, it - 2)
                adv(gen1, it - 1)
                adv(gen2, it - 2)
                sbd = it - 2
                if sbd >= 0 and sbd % 4 == 3:
                    if do_att:
                        attention(sbd // 4)
                    if do_gla and do_att:
                        exchange(sbd // 4)
            assert not gen1 and not gen2, (list(gen1), list(gen2))
            if debug and not (do_gla and do_att):
                dbg1 = Buf("dbg1")
                S.dma(lambda e: e.dma_start(out=dbg_o[0:64, :], in_=QT[0][0].t[:]), stsl[0], R=[QT[0][0]], W=[dbg1])
                S.dma(lambda e: e.dma_start(out=dbg_o[128:192, :], in_=KT[0][0].t[:]), stsl[0], R=[KT[0][0]], W=[dbg1])
                S.dma(lambda e: e.dma_start(out=dbg_o[256:384, :], in_=ogT[0].t[:]), stsl[0], R=[ogT[0]], W=[dbg1])
                S.dma(lambda e: e.dma_start(out=dbg_o[384:512, :], in_=oTa[0].t[:]), stsl[0], R=[oTa[0]], W=[dbg1])
                S.wait_all([dbg1])
            pg.es = es0
        dslot = pg.slot()
        if debug and do_coll:
            S.dma(lambda e: e.dma_start(out=dbg_o, in_=b_all), dslot, R=b_buf, W=[out_buf])
        if debug and not do_coll and do_gla and do_att:
            for sup in range(nsb_run // 4):
                S.dma(lambda e, sup=sup: e.dma_start(out=dbg_o[sup * 256:(sup + 1) * 256, :], in_=a_int[sup]), dslot, R=[a_buf[sup]], W=[out_buf])
        if phase2:
          with ExitStack() as es2:
            pg.es = es2
            hh = pg.sb("hh", [128, 16, D], F32)
            gfT = pg.sb("gfT", [128, D], F32)
            ss3 = pg.sb("ss3", [128, 16], F32)
            rs3 = pg.sb("rs3", [128, 16], F32)
            gates = pg.sb("gates", [128, 16, 2], F32)
            dstf = pg.sb("dstf", [128, 16, 2], F32)
            dsti = pg.sb("dsti", [128, 16, 2], I32)

            es2a = ExitStack()
            pg.es = es2a
            ridx = pg.sb("ridx", [128, 8], I32)
            oT = pg.sb("oT", [128, 8, 2048], BF16)
            Wo = pg.sb("Wo", [128, 8, D], BF16)
            g2T = pg.sb("g2T", [128, D], F32)
            wrT = pg.sb("wrT", [128, 8, 36], F32)
            brT = pg.sb("brT", [128, 36], F32)
            xr = [pg.sb("xr%d" % i, [128, D], F32) for i in range(2)]
            xrs = [pg.slot() for _ in range(2)]
            junk3 = pg.sb("junk3", [128, D], BF16)
            ss2 = pg.sb("ss2", [128, 16], F32)
            rs2 = pg.sb("rs2", [128, 16], F32)
            n2f = [pg.sb("n2f%d" % i, [128, D], F32) for i in range(2)]
            n2b = [pg.sb("n2b%d" % i, [128, D], BF16) for i in range(2)]
            n2s = [pg.slot() for _ in range(2)]
            n2T = pg.sb("n2T", [128, 8, 128], F32)
            lg = pg.sb("lg", [128, 36], F32)
            sm = pg.sb("sm", [128, 16], F32)
            gmask = pg.sb("gmask", [128, 4], F32)
            pen = pg.sb("pen", [128, 4], F32)
            gex = pg.sb("gex", [128, 4], F32)
            elm = pg.sb("elm", [128, 32], F32)
            elm2 = pg.sb("elm2", [128, 32], F32)
            mk1 = pg.sb("mk1", [128, 32], F32)
            mk2 = pg.sb("mk2", [128, 32], F32)
            cnt = pg.sb("cnt", [128, 32], F32)
            tot = pg.sb("tot", [128, 32], F32)
            pos = pg.sb("pos", [128, 32], F32)
            tmp32 = pg.sb("tmp32", [128, 32], F32)
            ps_h = [carve("ps_h%d" % i, i, 0, 512) for i in range(2)]
            ps_t = [carve("ps_t%d" % i, 2 + i, 0, 512, F32, "p (a b) -> p a b", a=4) for i in range(2)]
            ps_y = [carve("ps_y%d" % i, 4 + i, 0, 512) for i in range(2)]
            ps_a2 = [Tile(banks[0][:, 0:256], "ps_a0", bank_buf[0]), Tile(banks[2][:, 0:256], "ps_a1", bank_buf[2])]
            ps_b2 = [Tile(banks[1][:, 0:256], "ps_b0", bank_buf[1]), Tile(banks[3][:, 0:256], "ps_b1", bank_buf[3])]
            ps_x = Tile(banks[7][:, 0:256].bitcast(BF16).rearrange("p (a b) -> p a b", a=2), "ps_x", bank_buf[7])
            ps_l = Tile(banks[7][:, 256:292], "ps_l", bank_buf[7])
            ps_r = Tile(banks[7][:, 320:352], "ps_r", bank_buf[7])
            ps_c = Tile(banks[7][:, 352:384], "ps_c", bank_buf[7])

            S.dma(lambda e: e.dma_start(out=ridx.t[:], in_=rowidx), cslot, W=[ridx])
            S.dma(lambda e: e.dma_start(out=g2T.t[:], in_=g2r), cslot, W=[g2T])
            S.dma(lambda e: e.dma_start(out=gfT.t[:], in_=gfr), cslot, W=[gfT])
            S.dma(lambda e: e.dma_start(out=brT.t[:], in_=brr), cslot, W=[brT])
            S.dma(lambda e: e.dma_start(out=wrT.t[:], in_=wr.rearrange("(c p) n -> p c n", p=128)), cslot, W=[wrT])
            for kc in range(8):
                G.dma(lambda e, kc=kc: e.dma_start(out=Wo.t[:, kc, :], in_=w_out[kc * 128:(kc + 1) * 128, :]), cslot_g, W=[Wo])
            V.op(lambda e: e.memset(ss2.t[:], 0.0), W=[ss2])
            V.op(lambda e: e.memset(ss3.t[:], 0.0), W=[ss3])
            V.op(lambda e: e.memset(tot.t[:], 0.0), W=[tot])
            V.op(lambda e: e.memset(sm.t[:], 0.0), W=[sm])
            oslot = pg.slot()
            for c in range(8):
                G.dma(lambda e, c=c: e.indirect_dma_start(out=oT.t[:, c, :], out_offset=None, in_=b_all,
                                                          in_offset=bass.IndirectOffsetOnAxis(ap=ridx.t[:, c:c + 1], axis=0)),
                      oslot, R=b_buf + [ridx], W=[oT])
            wch = [(c // 2) + 4 * (c % 2) for c in range(8)]

            def rstd_ops(ssT, rsT, ti, n, eps):
                A.op(lambda e: e.activation(out=rsT.t[:, ti:ti + 1], in_=ssT.t[:, ti:ti + 1], func=AF.Sqrt, scale=1.0 / n, bias=epsT.t[:, 0:1]),
                     R=[ssT, epsT], W=[rsT])
                V.op(lambda e: e.reciprocal(out=rsT.t[:, ti:ti + 1], in_=rsT.t[:, ti:ti + 1]), R=[rsT], W=[rsT])

            for ti in range(16):
                p2 = ti % 2
                tsl = slice(ti * 128, (ti + 1) * 128)
                S.dma(lambda e, tsl=tsl, p2=p2: e.dma_start(out=xr[p2].t[:], in_=xres[tsl, :]), xrs[p2], W=[xr[p2]])
                for half in range(2):
                    for c in range(8):
                        P.op(lambda e, c=c, half=half, tsl=tsl: e.matmul(
                            out=ps_h[half].t[:], lhsT=oT.t[:, c, tsl], rhs=Wo.t[:, wch[c], half * 512:(half + 1) * 512],
                            start=(c == 0), stop=(c == 7)), R=[oT, Wo], W=[ps_h[half]])
                    V.op(lambda e, half=half, ti=ti, p2=p2: e.tensor_tensor(
                        out=hh.t[:, ti, half * 512:(half + 1) * 512], in0=ps_h[half].t[:], in1=xr[p2].t[:, half * 512:(half + 1) * 512],
                        op=ALU.add), R=[ps_h[half], xr[p2]], W=[hh])
                A.op(lambda e, ti=ti: e.activation(out=junk3.t[:], in_=hh.t[:, ti, :], func=AF.Square, accum_out=ss2.t[:, ti:ti + 1]),
                     R=[hh], W=[junk3, ss2])
                rstd_ops(ss2, rs2, ti, D, EPS)
                V.op(lambda e, ti=ti, p2=p2: e.scalar_tensor_tensor(out=n2f[p2].t[:], in0=hh.t[:, ti, :], scalar=rs2.t[:, ti:ti + 1],
                                                                    in1=g2T.t[:], op0=ALU.mult, op1=ALU.mult),
                     R=[hh, rs2, g2T], W=[n2f[p2]])
                A.op(lambda e, p2=p2: e.activation(out=n2b[p2].t[:], in_=n2f[p2].t[:], func=AF.Copy), R=[n2f[p2]], W=[n2b[p2]])
                for q in range(2):
                    for c4 in range(4):
                        c = q * 4 + c4
                        P.op(lambda e, c=c, c4=c4, q=q, p2=p2: e.transpose(out=ps_t[q].t[:, c4, :], in_=n2f[p2].t[:, c * 128:(c + 1) * 128],
                                                                           identity=identF), R=[n2f[p2], cfT], W=[ps_t[q]])
                    if q == 0:
                        A.op(lambda e: e.activation(out=n2T.t[:, 0:4, :], in_=ps_t[0].t[:], func=AF.Copy), R=[ps_t[0]], W=[n2T])
                    else:
                        V.op(lambda e: e.tensor_copy(out=n2T.t[:, 4:8, :], in_=ps_t[1].t[:]), R=[ps_t[1]], W=[n2T])
                for c in range(8):
                    P.op(lambda e, c=c: e.matmul(out=ps_l.t[:], lhsT=n2T.t[:, c, :], rhs=wrT.t[:, c, :], start=(c == 0), stop=(c == 7)),
                         R=[n2T, wrT], W=[ps_l])
                V.op(lambda e: e.tensor_tensor(out=lg.t[:], in0=ps_l.t[:], in1=brT.t[:], op=ALU.add), R=[ps_l, brT], W=[lg])
                gl = lg.t[:, 0:4]
                el = lg.t[:, 4:36]
                c_ = lambda k: sm.t[:, k:k + 1]
                V.op(lambda e: e.reduce_max(out=c_(0), in_=gl, axis=AX.X), R=[lg], W=[sm])
                V.op(lambda e: e.tensor_scalar(out=gmask.t[:], in0=gl, scalar1=c_(0), scalar2=None, op0=ALU.is_equal), R=[lg, sm], W=[gmask])
                V.op(lambda e: e.tensor_single_scalar(out=c_(1), in_=c_(0), scalar=-1.0, op=ALU.mult), R=[sm], W=[sm])
                V.op(lambda e: e.memset(c_(2), 0.0), W=[sm])
                A.op(lambda e: e.activation(out=gex.t[:], in_=gl, func=AF.Exp, bias=c_(1), accum_out=c_(2)), R=[lg, sm], W=[gex, sm])
                V.op(lambda e: e.reciprocal(out=c_(3), in_=c_(2)), R=[sm], W=[sm])
                V.op(lambda e: e.tensor_scalar(out=pen.t[:], in0=gmask.t[:], scalar1=-1.0, scalar2=BIG, op0=ALU.add, op1=ALU.mult),
                     R=[gmask], W=[pen])
                for g in range(4):
                    V.op(lambda e, g=g: e.tensor_scalar(out=elm.t[:, g * 8:(g + 1) * 8], in0=lg.t[:, 4 + g * 8:12 + g * 8],
                                                        scalar1=pen.t[:, g:g + 1], scalar2=None, op0=ALU.add), R=[lg, pen], W=[elm])
                V.op(lambda e: e.reduce_max(out=c_(4), in_=elm.t[:], axis=AX.X), R=[elm], W=[sm])
                V.op(lambda e: e.tensor_scalar(out=mk1.t[:], in0=elm.t[:], scalar1=c_(4), scalar2=None, op0=ALU.is_equal), R=[elm, sm], W=[mk1])
                V.op(lambda e: e.scalar_tensor_tensor(out=elm2.t[:], in0=mk1.t[:], scalar=-BIG, in1=elm.t[:], op0=ALU.mult, op1=ALU.add),
                     R=[mk1, elm], W=[elm2])
                V.op(lambda e: e.reduce_max(out=c_(5), in_=elm2.t[:], axis=AX.X), R=[elm2], W=[sm])
                V.op(lambda e: e.tensor_scalar(out=mk2.t[:], in0=elm2.t[:], scalar1=c_(5), scalar2=None, op0=ALU.is_equal), R=[elm2, sm], W=[mk2])
                V.op(lambda e: e.tensor_tensor(out=c_(6), in0=c_(5), in1=c_(4), op=ALU.subtract), R=[sm], W=[sm])
                A.op(lambda e: e.activation(out=c_(7), in_=c_(6), func=AF.Exp), R=[sm], W=[sm])
                V.op(lambda e: e.tensor_single_scalar(out=c_(8), in_=c_(7), scalar=1.0, op=ALU.add), R=[sm], W=[sm])
                V.op(lambda e: e.reciprocal(out=c_(9), in_=c_(8)), R=[sm], W=[sm])
                V.op(lambda e, ti=ti: e.tensor_tensor(out=gates.t[:, ti, 0:1], in0=c_(9), in1=c_(3), op=ALU.mult), R=[sm], W=[gates])
                V.op(lambda e, ti=ti: e.tensor_tensor(out=gates.t[:, ti, 1:2], in0=gates.t[:, ti, 0:1], in1=c_(7), op=ALU.mult),
                     R=[sm, gates], W=[gates])
                V.op(lambda e: e.tensor_tensor(out=cnt.t[:], in0=mk1.t[:], in1=mk2.t[:], op=ALU.add), R=[mk1, mk2], W=[cnt])
                P.op(lambda e: e.matmul(out=ps_r.t[:], lhsT=strictT, rhs=cnt.t[:], start=True, stop=True), R=[cnt, cfT], W=[ps_r])
                P.op(lambda e: e.matmul(out=ps_c.t[:], lhsT=onesF, rhs=cnt.t[:], start=True, stop=True), R=[cnt, cfT], W=[ps_c])
                V.op(lambda e: e.tensor_tensor(out=pos.t[:], in0=ps_r.t[:], in1=tot.t[:], op=ALU.add), R=[ps_r, tot], W=[pos])
                V.op(lambda e: e.tensor_tensor(out=tot.t[:], in0=ps_c.t[:], in1=tot.t[:], op=ALU.add), R=[ps_c, tot], W=[tot])
                V.op(lambda e: e.tensor_single_scalar(out=pos.t[:], in_=pos.t[:], scalar=float(CAP - 1), op=ALU.min), R=[pos], W=[pos])
                V.op(lambda e: e.tensor_tensor(out=pos.t[:], in0=pos.t[:], in1=ecol, op=ALU.add), R=[pos, cfT], W=[pos])
                for k, mk in enumerate((mk1, mk2)):
                    V.op(lambda e, mk=mk: e.tensor_tensor(out=tmp32.t[:], in0=pos.t[:], in1=mk.t[:], op=ALU.mult), R=[pos, mk], W=[tmp32])
                    V.op(lambda e, k=k, ti=ti: e.reduce_sum(out=dstf.t[:, ti, k:k + 1], in_=tmp32.t[:], axis=AX.X), R=[tmp32], W=[dstf])
                V.op(lambda e, ti=ti: e.tensor_copy(out=dsti.t[:, ti, :], in_=dstf.t[:, ti, :]), R=[dstf], W=[dsti])
                for k in range(2):
                    G.dma(lambda e, k=k, ti=ti, p2=p2: e.indirect_dma_start(
                        out=Xs, out_offset=bass.IndirectOffsetOnAxis(ap=dsti.t[:, ti, k:k + 1], axis=0), in_=n2b[p2].t[:, :], in_offset=None),
                        n2s[p2], R=[n2b[p2], dsti], W=[Xs_buf])
            if debug:
                S.dma(lambda e: e.dma_start(out=dbg_h.rearrange("(t p) d -> p t d", p=128), in_=hh.t[:]), dslot, R=[hh], W=[out_buf])

            es2a.close()
            es2b = ExitStack()
            pg.es = es2b
            junk4 = pg.sb("junk3b", [128, D], BF16)
            xg = [pg.sb("xg%d" % i, [128, 2, D], BF16) for i in range(2)]
            xgs = [pg.slot() for _ in range(2)]
            xT = [pg.sb("xT%d" % i, [128, 8, 256], BF16) for i in range(2)]
            w1s = [pg.sb("w1s%d" % i, [128, 8, 512], BF16) for i in range(2)]
            w3s = [pg.sb("w3s%d" % i, [128, 8, 512], BF16) for i in range(2)]
            w2s = [pg.sb("w2s%d" % i, [128, 4, D], BF16) for i in range(2)]
            wsl = [pg.slot() for _ in range(2)]
            sa = [pg.sb("sa%d" % i, [128, 256], F32) for i in range(2)]
            hT = [pg.sb("hT%d" % i, [128, 4, 256], BF16) for i in range(2)]
            ysb = [pg.sb("ysb%d" % i, [128, 2, D], F32) for i in range(1)]
            yss = [pg.slot() for _ in range(2)]
            ya = [pg.sb("ya%d" % i, [128, D], F32) for i in range(1)]
            yb = [pg.sb("yb%d" % i, [128, D], F32) for i in range(1)]
            ygs = [pg.slot() for _ in range(2)]
            ot = [pg.sb("ot%d" % i, [128, D], F32) for i in range(2)]
            ots = [pg.slot() for _ in range(2)]
            Xs_all = Buf("Xs_all")
            Xs_all.w = dict(Xs_buf.w)
            for p2 in range(2):
                Xs_all.w[id(n2s[p2].s)] = (n2s[p2].s, n2s[p2].v)
            def eloads(ex):
                p2 = ex % 2
                S.dma(lambda e, ex=ex, p2=p2: e.dma_start(out=xg[p2].t[:], in_=Xs[ex * CAP:(ex + 1) * CAP, :].rearrange("(s p) f -> p s f", p=128)),
                      xgs[p2], R=[Xs_all], W=[xg[p2]])
                S.dma(lambda e, ex=ex, p2=p2: e.dma_start(out=w1s[p2].t[:], in_=w1b[ex].rearrange("(c p) f -> p c f", p=128)), wsl[p2], R=[wb_buf[ex]], W=[w1s[p2]])
                S.dma(lambda e, ex=ex, p2=p2: e.dma_start(out=w3s[p2].t[:], in_=w3b[ex].rearrange("(c p) f -> p c f", p=128)), wsl[p2], R=[wb_buf[ex]], W=[w3s[p2]])
                S.dma(lambda e, ex=ex, p2=p2: e.dma_start(out=w2s[p2].t[:], in_=w2b[ex].rearrange("(c p) f -> p c f", p=128)), wsl[p2], R=[wb_buf[ex]], W=[w2s[p2]])

            eloads(0)
            for ex in range(NE):
                p2 = ex % 2
                if ex + 1 < NE:
                    eloads(ex + 1)
                for cq in range(4):
                    for cc in range(2):
                        c = cq * 2 + cc
                        for s in range(2):
                            P.op(lambda e, c=c, cc=cc, s=s, p2=p2: e.transpose(out=ps_x.t[:, cc, s * 128:(s + 1) * 128],
                                                                               in_=xg[p2].t[:, s, c * 128:(c + 1) * 128], identity=identB),
                                 R=[xg[p2], cbT], W=[ps_x])
                    if cq % 2 == 0:
                        V.op(lambda e, cq=cq, p2=p2: e.tensor_copy(out=xT[p2].t[:, cq * 2:cq * 2 + 2, :], in_=ps_x.t[:]), R=[ps_x], W=[xT[p2]])
                    else:
                        A.op(lambda e, cq=cq, p2=p2: e.activation(out=xT[p2].t[:, cq * 2:cq * 2 + 2, :], in_=ps_x.t[:], func=AF.Copy),
                             R=[ps_x], W=[xT[p2]])
                for fc in range(4):
                    f2 = fc % 2
                    ps_a = ps_a2[f2]
                    ps_b = ps_b2[f2]
                    for c in range(8):
                        P.op(lambda e, c=c, fc=fc, p2=p2, ps_a=ps_a: e.matmul(out=ps_a.t[:], lhsT=w1s[p2].t[:, c, fc * 128:(fc + 1) * 128], rhs=xT[p2].t[:, c, :],
                                                                   start=(c == 0), stop=(c == 7)), R=[w1s[p2], xT[p2]], W=[ps_a])
                    for c in range(8):
                        P.op(lambda e, c=c, fc=fc, p2=p2, ps_b=ps_b: e.matmul(out=ps_b.t[:], lhsT=w3s[p2].t[:, c, fc * 128:(fc + 1) * 128], rhs=xT[p2].t[:, c, :],
                                                                   start=(c == 0), stop=(c == 7)), R=[w3s[p2], xT[p2]], W=[ps_b])
                    A.op(lambda e, f2=f2, ps_a=ps_a: e.activation(out=sa[f2].t[:], in_=ps_a.t[:], func=AF.Silu), R=[ps_a], W=[sa[f2]])
                    V.op(lambda e, f2=f2, fc=fc, p2=p2, ps_b=ps_b: e.tensor_tensor(out=hT[p2].t[:, fc, :], in0=ps_b.t[:], in1=sa[f2].t[:], op=ALU.mult),
                         R=[ps_b, sa[f2]], W=[hT[p2]])
                for s in range(2):
                    for half in range(2):
                        yy = ps_y[half]
                        for fc in range(4):
                            P.op(lambda e, fc=fc, s=s, half=half, p2=p2, yy=yy: e.matmul(
                                out=yy.t[:], lhsT=hT[p2].t[:, fc, s * 128:(s + 1) * 128], rhs=w2s[p2].t[:, fc, half * 512:(half + 1) * 512],
                                start=(fc == 0), stop=(fc == 3)), R=[hT[p2], w2s[p2]], W=[yy])
                        if half == 0:
                            V.op(lambda e, s=s, p2=p2, yy=yy: e.tensor_copy(out=ysb[0].t[:, s, 0:512], in_=yy.t[:]), R=[yy], W=[ysb[0]])
                        else:
                            A.op(lambda e, s=s, p2=p2, yy=yy: e.activation(out=ysb[0].t[:, s, 512:1024], in_=yy.t[:], func=AF.Copy), R=[yy], W=[ysb[0]])
                G.dma(lambda e, ex=ex, p2=p2: e.dma_start(out=Ys[ex * CAP:(ex + 1) * CAP, :].rearrange("(s p) f -> p s f", p=128), in_=ysb[0].t[:]),
                      yss[0], R=[ysb[0]], W=[Ys_buf[ex]])

            for ti in range(16):
                p2 = ti % 2
                G.dma(lambda e, ti=ti, p2=p2: e.indirect_dma_start(out=ya[0].t[:, :], out_offset=None, in_=Ys,
                                                                   in_offset=bass.IndirectOffsetOnAxis(ap=dsti.t[:, ti, 0:1], axis=0)),
                      ygs[0], R=Ys_buf + [dsti], W=[ya[0]])
                G.dma(lambda e, ti=ti, p2=p2: e.indirect_dma_start(out=yb[0].t[:, :], out_offset=None, in_=Ys,
                                                                   in_offset=bass.IndirectOffsetOnAxis(ap=dsti.t[:, ti, 1:2], axis=0)),
                      ygs[0], R=Ys_buf + [dsti], W=[yb[0]])
                V.op(lambda e, ti=ti, p2=p2: e.scalar_tensor_tensor(out=hh.t[:, ti, :], in0=ya[0].t[:], scalar=gates.t[:, ti, 0:1], in1=hh.t[:, ti, :],
                                                                    op0=ALU.mult, op1=ALU.add), R=[ya[0], gates, hh], W=[hh])
                V.op(lambda e, ti=ti, p2=p2: e.scalar_tensor_tensor(out=hh.t[:, ti, :], in0=yb[0].t[:], scalar=gates.t[:, ti, 1:2], in1=hh.t[:, ti, :],
                                                                    op0=ALU.mult, op1=ALU.add), R=[yb[0], gates, hh], W=[hh])
                A.op(lambda e, ti=ti: e.activation(out=junk4.t[:], in_=hh.t[:, ti, :], func=AF.Square, accum_out=ss3.t[:, ti:ti + 1]),
                     R=[hh], W=[junk4, ss3])
                rstd_ops(ss3, rs3, ti, D, EPS)
                V.op(lambda e, ti=ti, p2=p2: e.scalar_tensor_tensor(out=ot[p2].t[:], in0=hh.t[:, ti, :], scalar=rs3.t[:, ti:ti + 1], in1=gfT.t[:],
                                                                    op0=ALU.mult, op1=ALU.mult), R=[hh, rs3, gfT], W=[ot[p2]])
                S.dma(lambda e, ti=ti, p2=p2: e.dma_start(out=out[ti * 128:(ti + 1) * 128, :], in_=ot[p2].t[:]), ots[p2], R=[ot[p2]], W=[out_buf])
            fin = Buf("fin")
            for sl in ots + [dslot]:
                if sl.v:
                    fin.w[id(sl.s)] = (sl.s, sl.v)
            S.wait_all([fin])
            es2b.close()
            pg.es = es0
        else:
            fin = Buf("fin")
            if dslot.v:
                fin.w[id(dslot.s)] = (dslot.s, dslot.v)
            S.wait_all([fin])
        pg.emit()
    return nc


def _consts():
    a = np.arange(128)
    ident = np.eye(128, dtype=np.float32)
    triu = (a[:, None] <= a[None, :]).astype(np.float32)
    strict = (a[:, None] < a[None, :]).astype(np.float32)
    ones = np.ones((128, 128), np.float32)
    ecol = np.tile((np.arange(NE) * CAP).astype(np.float32)[None, :], (128, 1))
    cf = np.concatenate([ident, triu, strict, ones, ecol], axis=1)
    L = (a[:, None] >= a[None, :]).astype(np.float32)
    U = triu
    cb = np.concatenate([ident, L, U, L, U], axis=1).astype(ml_dtypes.bfloat16)
    return np.ascontiguousarray(cf), np.ascontiguousarray(cb)


def make_in_maps(x, norm1_g, w_in, gla_gate_w2, gla_gate_b, gla_norm_g, w_out, norm2_g,
                 router_group_w, router_group_b, router_expert_w, router_expert_b,
                 expert_w1, expert_w3, expert_w2, final_norm_g):
    f = lambda a: np.ascontiguousarray(np.asarray(a, dtype=np.float32))
    x = f(x)
    win = f(w_in)[0]
    cf, cb = _consts()
    gq0, gk0, gv0, gr0, glr0, aq0, ak0, av0 = 0, 256, 512, 1024, 1536, 1552, 2064, 2576
    w1 = f(expert_w1)[0]
    w3 = f(expert_w3)[0]
    w2 = f(expert_w2)[0]
    wo = f(w_out)[0]
    g2r = f(np.tile(np.asarray(norm2_g)[0][None, :], (128, 1)))
    gfr = f(np.tile(np.asarray(final_norm_g)[None, :], (128, 1)))
    wr = f(np.concatenate([np.asarray(router_group_w)[0], np.asarray(router_expert_w)[0]], axis=1))
    br = np.concatenate([np.asarray(router_group_b)[0], np.asarray(router_expert_b)[0]])
    brr = f(np.tile(br[None, :], (128, 1)))
    g1 = f(np.asarray(norm1_g)[0].reshape(8, 128).T)
    gng = f(np.tile(np.asarray(gla_norm_g)[0][None, :], (128, 4)))
    maps = []
    for c in range(8):
        b, j = c // 4, c % 4
        cols = np.concatenate([
            np.arange(gq0 + 64 * j, gq0 + 64 * j + 64), np.arange(gk0 + 64 * j, gk0 + 64 * j + 64),
            np.arange(aq0 + 128 * j, aq0 + 128 * j + 128), np.arange(ak0 + 128 * j, ak0 + 128 * j + 128),
            np.arange(av0 + 128 * j, av0 + 128 * j + 128), np.arange(glr0, glr0 + 16),
            np.arange(gk0 + 64 * j, gk0 + 64 * j + 64), np.arange(gv0 + 128 * j, gv0 + 128 * j + 128),
            np.arange(gr0 + 128 * j, gr0 + 128 * j + 128)])
        w2aug = np.concatenate([np.asarray(gla_gate_w2)[0][:, 64 * j:64 * j + 64],
                                np.asarray(gla_gate_b)[0][None, 64 * j:64 * j + 64]], axis=0)
        rowidx = (j * 1024 + np.arange(8)[None, :] * 128 + np.arange(128)[:, None]).astype(np.int32)
        maps.append({
            "x": x[b], "xres": np.ascontiguousarray(x[b, 2048 * j:2048 * (j + 1)]),
            "w_in": np.ascontiguousarray(win[:, cols]), "g1": g1, "w2aug": f(w2aug), "gng": gng,
            "w_out": wo, "g2r": g2r, "gfr": gfr, "wr": wr, "brr": brr, "w1": w1, "w3": w3, "w2": w2,
            "cf": cf, "cb": cb, "rowidx": np.ascontiguousarray(rowidx),
        })
    return maps


_NC_CACHE = {}


def kernel(**inputs):
    if "nc" not in _NC_CACHE:
        _NC_CACHE["nc"] = build()
    nc = _NC_CACHE["nc"]
    maps = make_in_maps(**inputs)
    res = run_bass_kernel_spmd(nc, maps, core_ids=list(range(8)))
    outs = [np.asarray(res.results[c]["out"], dtype=np.float32) for c in range(8)]
    y = np.stack([np.concatenate(outs[0:4], axis=0), np.concatenate(outs[4:8], axis=0)], axis=0)
    return y
```

```python
import numpy as np
import ml_dtypes
from contextlib import ExitStack
import concourse.bass as bass
import concourse.mybir as mybir
from concourse.bass_utils import run_bass_kernel_spmd

F32 = mybir.dt.float32
BF16 = mybir.dt.bfloat16
I32 = mybir.dt.int32
AF = mybir.ActivationFunctionType
ALU = mybir.AluOpType
AX = mybir.AxisListType

T = 8192
D = 1024
NSB = 16
NSUP = 4
CAP = 256
NE = 32
EPS = 1e-6
BIG = 1.0e30
RING = 6144
SEM_LIMIT = 12000
import os
ATT_MODE = int(os.environ.get('ATT_MODE', '3'))
MASK_ENG = os.environ.get('MASK_ENG', 'V')
ATT_NOTR = int(os.environ.get('ATT_NOTR', '0'))
ATT_DS = tuple(int(v) for v in os.environ.get('ATT_DS', '1,4,16').split(','))


class Buf:
    __slots__ = ("name", "w", "r", "psum")

    def __init__(self, name, psum=False):
        self.name = name
        self.w = {}
        self.r = {}
        self.psum = psum


class Tile:
    def __init__(self, t, name, buf=None):
        self.t = t
        self.b = buf if buf is not None else Buf(name)


def _split_psum(R, W):
    R2, W2 = [], list(W)
    for x in R:
        b = x.b if isinstance(x, Tile) else x
        if b.psum:
            W2.append(x)
        else:
            R2.append(x)
    return R2, W2


class SemSlot:
    def __init__(self, sem):
        self.s = sem
        self.v = 0


class Eng:
    def __init__(self, prog, name, is_pe=False):
        self.prog = prog
        self.name = name
        self.is_pe = is_pe
        self.sem = prog.new_sem()
        self.cnt = 0
        self.known = {}
        self.thunks = []

    def _wait(self, sem, val):
        if self.known.get(id(sem), 0) >= val:
            return
        self.known[id(sem)] = val
        self.thunks.append(lambda e, s=sem, v=val: e.wait_ge(s, v))

    def _deps(self, R, W):
        for x in R:
            b = x.b if isinstance(x, Tile) else x
            for (sem, val) in b.w.values():
                if sem is self.sem and self.is_pe:
                    continue
                self._wait(sem, val)
        for x in W:
            b = x.b if isinstance(x, Tile) else x
            for (sem, val) in list(b.w.values()) + list(b.r.values()):
                if sem is self.sem and self.is_pe:
                    continue
                self._wait(sem, val)

    def _record(self, R, W, sem, val):
        for x in R:
            b = x.b if isinstance(x, Tile) else x
            b.r[id(sem)] = (sem, val)
        for x in W:
            b = x.b if isinstance(x, Tile) else x
            b.w = {id(sem): (sem, val)}
            b.r = {}

    def op(self, fn, R=(), W=()):
        R, W = _split_psum(R, W)
        self._deps(R, W)
        if self.cnt >= SEM_LIMIT:
            self.sem = self.prog.new_sem()
            self.cnt = 0
        self.cnt += 1
        sem, val = self.sem, self.cnt
        self.thunks.append(lambda e, f=fn, s=sem: f(e).then_inc(s, 1))
        self._record(R, W, sem, val)

    def dma(self, fn, slot, R=(), W=()):
        self._deps(R, W)
        slot.v += 16
        sem, val = slot.s, slot.v
        self.thunks.append(lambda e, f=fn, s=sem: f(e).then_inc(s, 16))
        self._record(R, W, sem, val)

    def coll(self, fn, R=(), W=()):
        self._deps(R, W)
        sem = self.prog.new_sem()
        self.thunks.append(lambda e, f=fn, s=sem: f(e).then_inc(s))
        self._record(R, W, sem, 1)

    def wait_all(self, bufs):
        self._deps(bufs, ())


class Prog:
    def __init__(self, nc, es):
        self.nc = nc
        self.es = es
        self.es_sem = es
        self.nsem = 0
        self.V = Eng(self, "vector")
        self.A = Eng(self, "scalar")
        self.G = Eng(self, "gpsimd")
        self.P = Eng(self, "tensor", is_pe=True)
        self.S = Eng(self, "sync")

    def new_sem(self):
        self.nsem += 1
        return self.es_sem.enter_context(self.nc.semaphore("s%d" % self.nsem))

    def slot(self):
        return SemSlot(self.new_sem())

    def sb(self, name, shape, dt):
        return Tile(self.es.enter_context(self.nc.sbuf_tensor(name, shape, dt)), name)

    def ps(self, name, shape, dt):
        return Tile(self.es.enter_context(self.nc.psum_tensor(name, shape, dt)), name)

    def emit(self):
        with self.nc.Block() as block:
            @block.vector
            def _(e):
                for th in self.V.thunks:
                    th(e)

            @block.scalar
            def _(e):
                for th in self.A.thunks:
                    th(e)

            @block.gpsimd
            def _(e):
                for th in self.G.thunks:
                    th(e)

            @block.tensor
            def _(e):
                for th in self.P.thunks:
                    th(e)

            @block.sync
            def _(e):
                for th in self.S.thunks:
                    th(e)


def build(debug=False, phase2=True, do_coll=True, nsb_run=NSB, do_gla=True, do_att=True):
    nc = bass.Bass("TRN2", target_bir_lowering=False)
    dram = lambda n, s, dt, k="ExternalInput": nc.dram_tensor(n, s, dt, kind=k).ap()
    x = dram("x", [T, D], F32)
    xres = dram("xres", [2048, D], F32)
    w_in = dram("w_in", [D, 848], F32)
    g1 = dram("g1", [128, 8], F32)
    w2aug = dram("w2aug", [17, 64], F32)
    gng = dram("gng", [128, 512], F32)
    w_out = dram("w_out", [D, D], F32)
    g2r = dram("g2r", [128, D], F32)
    gfr = dram("gfr", [128, D], F32)
    wr = dram("wr", [D, 36], F32)
    brr = dram("brr", [128, 36], F32)
    NEd = NE if phase2 else 1
    w1 = dram("w1", [NEd, D, 512], F32)
    w3 = dram("w3", [NEd, D, 512], F32)
    w2 = dram("w2", [NEd, 512, D], F32)
    cf = dram("cf", [128, 544], F32)
    cb = dram("cb", [128, 640], BF16)
    out = dram("out", [2048, D], F32, "ExternalOutput")
    a_int = [dram("a_int%d" % s, [256, 2048], BF16, "Internal") for s in range(NSUP)]
    b_all = dram("b_all", [NSUP * 1024, 2048], BF16, "Internal")
    rowidx = dram("rowidx", [128, 8], I32)
    w1b = dram("w1b", [NEd, D, 512], BF16, "Internal")
    w3b = dram("w3b", [NEd, D, 512], BF16, "Internal")
    w2b = dram("w2b", [NEd, 512, D], BF16, "Internal")
    wb_buf = [Buf("wb%d" % e) for e in range(NE)]
    Xs = dram("xs_scr", [NE * CAP, D], BF16, "Internal")
    Ys = dram("ys_scr", [NE * CAP, D], F32, "Internal")
    if debug:
        dbg_o = dram("dbg_o", [NSUP * 1024, 2048], BF16, "ExternalOutput")
        dbg_h = dram("dbg_h", [2048, D], F32, "ExternalOutput")
    a_buf = [Buf("a%d" % s) for s in range(NSUP)]
    b_buf = [Buf("b%d" % s) for s in range(NSUP)]
    Xs_buf = Buf("Xs")
    Ys_buf = [Buf("Ys%d" % e) for e in range(NE)]
    out_buf = Buf("out")

    with ExitStack() as es0:
        pg = Prog(nc, es0)
        V, A, G, P, S = pg.V, pg.A, pg.G, pg.P, pg.S
        cslot = pg.slot()
        cslot_g = pg.slot()
        banks = [es0.enter_context(nc.psum_tensor("bank%d" % i, [128, 512], F32)) for i in range(8)]
        bank_buf = [Buf("bank%d" % i, psum=True) for i in range(8)]

        def carve(name, bank, c0, ncol, dt=F32, pat=None, parts=128, **kw):
            ap = banks[bank][0:parts, c0:c0 + ncol]
            if dt is not F32:
                ap = ap.bitcast(dt)
            if pat is not None:
                ap = ap.rearrange(pat, **kw)
            return Tile(ap, name, bank_buf[bank])

        cfT = pg.sb("cfT", [128, 544], F32)
        cbT = pg.sb("cbT", [128, 640], BF16)
        S.dma(lambda e: e.dma_start(out=cfT.t[:], in_=cf), cslot, W=[cfT])
        S.dma(lambda e: e.dma_start(out=cbT.t[:], in_=cb), cslot, W=[cbT])
        epsT = pg.sb("epsT", [128, 2], F32)
        V.op(lambda e: e.memset(epsT.t[:, 0:1], EPS), W=[epsT])
        V.op(lambda e: e.memset(epsT.t[:, 1:2], 64 * EPS), W=[epsT])
        identF = cfT.t[:, 0:128]
        triU = cfT.t[:, 128:256]
        strictT = cfT.t[:, 256:384]
        onesF = cfT.t[:, 384:512]
        ecol = cfT.t[:, 512:544]
        identB = cbT.t[:, 0:128]
        mask4 = cbT.t[:, 128:640]

        with ExitStack() as es1:
            pg.es = es1
            NX = 6
            Wt = pg.sb("Wt", [128, 8, 848], BF16)
            g1T = pg.sb("g1T", [128, 8], F32)
            w2a = pg.sb("w2a", [17, 64], F32)
            gngT = pg.sb("gngT", [128, 4, 128], F32)
            xt = [pg.sb("xt%d" % i, [128, D], F32) for i in range(NX)]
            xsl = [pg.slot() for _ in range(NX)]
            junk = pg.sb("junk", [128, D], BF16)
            ss1 = pg.sb("ss1", [128, 64], F32)
            rs1 = pg.sb("rs1", [128, 64], F32)
            xs = [pg.sb("xs%d" % i, [128, D], BF16) for i in range(4)]
            nT = [pg.sb("nT%d" % i, [128, 8, 512], BF16) for i in range(2)]
            qT = [pg.sb("qT%d" % i, [64, 512], BF16) for i in range(2)]
            kT = [pg.sb("kT%d" % i, [64, 512], BF16) for i in range(2)]
            QT = [[pg.sb("QT%d_%d" % (h, i), [64, 2048], BF16) for i in range(2)] for h in range(2)]
            KT = [[pg.sb("KT%d_%d" % (h, i), [64, 2048], BF16) for i in range(3)] for h in range(2)]
            VT = [[pg.sb("VT%d_%d" % (h, i), [64, 2048], BF16) for i in range(3)] for h in range(2)]
            tm = [pg.sb("tm%d" % i, [128, 4, 320], BF16) for i in range(2)]
            glrT = [pg.sb("glrT%d" % i, [32, 512], F32) for i in range(2)]
            e1 = pg.sb("e1", [128, 256], F32)
            sp = pg.sb("sp", [128, 256], F32)
            Ek = pg.sb("Ek", [128, 256], F32)
            ktl = pg.sb("ktl", [128, 4, 64], BF16)
            EqT = pg.sb("EqT", [64, 512], F32)
            EkT = pg.sb("EkT", [64, 512], F32)
            qtl = pg.sb("qtl", [64, 512], BF16)
            ktlT = pg.sb("ktlT", [64, 512], BF16)
            gg = pg.sb("gg", [128, 4, 128], F32)
            AmT = [pg.sb("AmT%d" % i, [128, 128], BF16) for i in range(4)]
            og = [pg.sb("og%d" % i, [128, 128], BF16) for i in range(4)]
            Sst = pg.sb("Sst", [64, 128], F32)
            Sbf = pg.sb("Sbf", [64, 128], BF16)
            Stmp = pg.sb("Stmp", [64, 128], F32)
            ssg = pg.sb("ssg", [128, 64], F32)
            rsg = pg.sb("rsg", [128, 64], F32)
            junk2 = pg.sb("junk2", [128, 128], BF16)
            ogT = [pg.sb("ogT%d" % i, [128, 2048], BF16) for i in range(2)]
            pTa = [pg.sb("pTa%d" % i, [128, 512], BF16) for i in range(3)]
            vp = [pg.sb("vp%d" % i, [128, 4, 128], BF16) for i in range(3)]
            Oacc = pg.sb("Oacc", [128, 2, 2048], F32)
            rden = pg.sb("rden", [64, 1024], F32)
            oTa = [pg.sb("oTa%d" % i, [128, 2048], BF16) for i in range(2)]
            stsl = [pg.slot() for _ in range(2)]
            ps_pT_l = [carve("ps_pT", 0, 0, 512, BF16, "p (a b) -> p a b", a=8), carve("ps_pT2", 4, 0, 512, BF16, "p (a b) -> p a b", a=8)]
            ps_pr = [carve("ps_pr%d" % i, 1 + i, 0, 512) for i in range(2)]
            ps_GT = carve("ps_GT", 3, 0, 512, parts=64)
            ps_s = carve("ps_s", 4, 0, 512)
            ps_z = carve("ps_z", 5, 0, 256)
            ps_G = ps_z
            ps_U = Tile(banks[5][0:64, 256:384], "ps_U", bank_buf[5])
            ps_ogT_l = [Tile(banks[5][:, 384:448].bitcast(BF16), "ps_ogT0", bank_buf[5]),
                        Tile(banks[5][:, 448:512].bitcast(BF16), "ps_ogT1", bank_buf[5])]
            ps_A4 = carve("ps_A4", 6, 0, 512, F32, "p (a b) -> p a b", a=4)
            ps_og4 = carve("ps_og4", 7, 0, 512, F32, "p (a b) -> p a b", a=4)
            ps_o = Tile(banks[6][:, 0:256].rearrange("p (a b) -> p a b", a=2), "ps_o", bank_buf[6])
            ps_v = Tile(banks[7][:, 128:256].bitcast(BF16).rearrange("p (a b) -> p a b", a=4), "ps_v", bank_buf[7])
            ps_s_l = [ps_s, Tile(banks[5][:, :], "ps_s2", bank_buf[5]), Tile(banks[3][:, :], "ps_s3", bank_buf[3])]
            ps_o_l = [ps_o, Tile(banks[0][:, 0:256].rearrange("p (a b) -> p a b", a=2), "ps_o2", bank_buf[0])]
            ps_v_l = [ps_v, Tile(banks[1][:, 128:256].bitcast(BF16).rearrange("p (a b) -> p a b", a=4), "ps_v2", bank_buf[1])]

            for kc in range(8):
                G.dma(lambda e, kc=kc: e.dma_start(out=Wt.t[:, kc, :], in_=w_in[kc * 128:(kc + 1) * 128, :]),
                      cslot_g, W=[Wt])
            S.dma(lambda e: e.dma_start(out=g1T.t[:], in_=g1), cslot, W=[g1T])
            S.dma(lambda e: e.dma_start(out=w2a.t[:], in_=w2aug), cslot, W=[w2a])
            S.dma(lambda e: e.dma_start(out=gngT.t[:].rearrange("p a b -> p (a b)"), in_=gng), cslot, W=[gngT])
            for kc in range(8):
                V.op(lambda e, kc=kc: e.tensor_scalar(out=Wt.t[:, kc, :], in0=Wt.t[:, kc, :], scalar1=g1T.t[:, kc:kc + 1],
                                                      scalar2=None, op0=ALU.mult), R=[g1T, Wt], W=[Wt])
            V.op(lambda e: e.memset(ss1.t[:], 0.0), W=[ss1])
            V.op(lambda e: e.memset(ssg.t[:], 0.0), W=[ssg])
            V.op(lambda e: e.memset(Sst.t[:], 0.0), W=[Sst])
            V.op(lambda e: e.memset(Sbf.t[:], 0.0), W=[Sbf])
            for i in range(2):
                G.op(lambda e, i=i: e.memset(glrT[i].t[:], 1.0), W=[glrT[i]])
            for i in range(3):
                G.op(lambda e, i=i: e.memset(vp[i].t[:], 1.0), W=[vp[i]])

            pr_i = [0]

            def next_pr():
                pr_i[0] ^= 1
                return ps_pr[pr_i[0]]

            def stage1(sb):
                par = sb % 2
                sup = sb // 4
                for i in range(4):
                    ti = sb * 4 + i
                    sl = ti % NX
                    S.dma(lambda e, ti=ti, sl=sl: e.dma_start(out=xt[sl].t[:], in_=x[ti * 128:(ti + 1) * 128, :]),
                          xsl[sl], W=[xt[sl]])
                    A.op(lambda e, ti=ti, sl=sl: e.activation(out=junk.t[:], in_=xt[sl].t[:], func=AF.Square,
                                                              accum_out=ss1.t[:, ti:ti + 1]),
                         R=[xt[sl]], W=[junk, ss1])
                t0i = sb * 4
                A.op(lambda e: e.activation(out=rs1.t[:, t0i:t0i + 4], in_=ss1.t[:, t0i:t0i + 4], func=AF.Sqrt, scale=1.0 / D, bias=epsT.t[:, 0:1]),
                     R=[ss1, epsT], W=[rs1])
                V.op(lambda e: e.reciprocal(out=rs1.t[:, t0i:t0i + 4], in_=rs1.t[:, t0i:t0i + 4]), R=[rs1], W=[rs1])
                for i in range(4):
                    ti = sb * 4 + i
                    sl = ti % NX
                    xp = ti % 4
                    V.op(lambda e, ti=ti, sl=sl, xp=xp: e.tensor_scalar(out=xs[xp].t[:], in0=xt[sl].t[:],
                                                                        scalar1=rs1.t[:, ti:ti + 1], scalar2=None, op0=ALU.mult),
                         R=[xt[sl], rs1], W=[xs[xp]])
                yield "front_a"
                for i in range(4):
                    ti = sb * 4 + i
                    xp = ti % 4
                    ps_pT = ps_pT_l[ti % 2]
                    for kc in range(8):
                        P.op(lambda e, kc=kc, xp=xp, ps_pT=ps_pT: e.transpose(out=ps_pT.t[:, kc, :], in_=xs[xp].t[:, kc * 128:(kc + 1) * 128],
                                                                 identity=identB), R=[xs[xp], cbT], W=[ps_pT])
                    A.op(lambda e, i=i, par=par, ps_pT=ps_pT: e.activation(out=nT[par].t[:, :, i * 128:(i + 1) * 128], in_=ps_pT.t[:, :, :],
                                                              func=AF.Copy), R=[ps_pT], W=[nT[par]])
                yield "front_b"
                col = (sb % 4) * 512
                kslot = sup % 3
                fm = [
                    (0, 128, [(qT[par], qT[par].t[0:64, :]), (kT[par], kT[par].t[0:64, :])]),
                    (128, 128, [(QT[0][sup % 2], QT[0][sup % 2].t[:, col:col + 512]), (QT[1][sup % 2], QT[1][sup % 2].t[:, col:col + 512])]),
                    (256, 128, [(KT[0][kslot], KT[0][kslot].t[:, col:col + 512]), (KT[1][kslot], KT[1][kslot].t[:, col:col + 512])]),
                    (384, 128, [(VT[0][kslot], VT[0][kslot].t[:, col:col + 512]), (VT[1][kslot], VT[1][kslot].t[:, col:col + 512])]),
                    (512, 16, [(glrT[par], glrT[par].t[0:16, :])]),
                ]
                for gi, (c0, m, dsts) in enumerate(fm):
                    pp = next_pr()
                    for kc in range(8):
                        P.op(lambda e, kc=kc, c0=c0, m=m, pp=pp, par=par: e.matmul(
                            out=pp.t[0:m, :], lhsT=Wt.t[:, kc, c0:c0 + m], rhs=nT[par].t[:, kc, :],
                            start=(kc == 0), stop=(kc == 7)), R=[Wt, nT[par]], W=[pp])
                    if len(dsts) == 2:
                        (t0_, d0_), (t1_, d1_) = dsts
                        A.op(lambda e, pp=pp, d0_=d0_: e.activation(out=d0_, in_=pp.t[0:64, :], func=AF.Copy), R=[pp], W=[t0_])
                        V.op(lambda e, pp=pp, d1_=d1_: e.tensor_copy(out=d1_, in_=pp.t[64:128, :]), R=[pp], W=[t1_])
                    else:
                        (t0_, d0_), = dsts
                        V.op(lambda e, pp=pp, m=m, d0_=d0_: e.tensor_copy(out=d0_, in_=pp.t[0:m, :]), R=[pp], W=[t0_])
                    if gi == 1 or gi == 4:
                        yield "fm"
                for i in range(4):
                    pp = next_pr()
                    for kc in range(8):
                        P.op(lambda e, kc=kc, i=i, pp=pp, par=par: e.matmul(
                            out=pp.t[:, 0:320], lhsT=nT[par].t[:, kc, i * 128:(i + 1) * 128], rhs=Wt.t[:, kc, 528:848],
                            start=(kc == 0), stop=(kc == 7)), R=[Wt, nT[par]], W=[pp])
                    V.op(lambda e, pp=pp, i=i, par=par: e.tensor_copy(out=tm[par].t[:, i, 0:192], in_=pp.t[:, 0:192]),
                         R=[pp], W=[tm[par]])
                    A.op(lambda e, pp=pp, i=i, par=par: e.activation(out=tm[par].t[:, i, 192:320], in_=pp.t[:, 192:320],
                                                                     func=AF.Silu), R=[pp], W=[tm[par]])
                    if i == 1 or i == 3:
                        yield "tm"

            def gla(sb):
                par = sb % 2
                sup = sb // 4
                for i in range(4):
                    P.op(lambda e, i=i, par=par: e.matmul(out=ps_z.t[:, i * 64:(i + 1) * 64], lhsT=glrT[par].t[0:17, i * 128:(i + 1) * 128],
                                                          rhs=w2a.t[0:17, :], start=True, stop=True),
                         R=[glrT[par], w2a], W=[ps_z])
                A.op(lambda e: e.activation(out=e1.t[:], in_=ps_z.t[:], func=AF.Exp, scale=-1.0), R=[ps_z], W=[e1])
                A.op(lambda e: e.activation(out=sp.t[:], in_=e1.t[:], func=AF.Ln, bias=1.0), R=[e1], W=[sp])
                yield "p1"
                P.op(lambda e: e.matmul(out=ps_G.t[:], lhsT=triU, rhs=sp.t[:], start=True, stop=True), R=[sp, cfT], W=[ps_G])
                for i in range(4):
                    P.op(lambda e, i=i: e.matmul(out=ps_GT.t[:, i * 128:(i + 1) * 128], lhsT=sp.t[:, i * 64:(i + 1) * 64], rhs=triU,
                                                 start=True, stop=True), R=[sp, cfT], W=[ps_GT])
                A.op(lambda e: e.activation(out=Ek.t[:], in_=ps_G.t[:], func=AF.Exp, scale=1.0 / 16), R=[ps_G], W=[Ek])
                A.op(lambda e: e.activation(out=EqT.t[:], in_=ps_GT.t[:], func=AF.Exp, scale=-1.0 / 16), R=[ps_GT], W=[EqT])
                A.op(lambda e: e.activation(out=EkT.t[:], in_=ps_GT.t[:], func=AF.Exp, scale=1.0 / 16), R=[ps_GT], W=[EkT])
                V.op(lambda e, par=par: e.tensor_tensor(out=ktl.t[:, :, :], in0=tm[par].t[:, :, 0:64],
                                                        in1=Ek.t[:].rearrange("p (a b) -> p a b", a=4), op=ALU.mult),
                     R=[tm[par], Ek], W=[ktl])
                V.op(lambda e, par=par: e.tensor_tensor(out=qtl.t[:], in0=qT[par].t[:], in1=EqT.t[:], op=ALU.mult),
                     R=[qT[par], EqT], W=[qtl])
                V.op(lambda e, par=par: e.tensor_tensor(out=ktlT.t[:], in0=kT[par].t[:], in1=EkT.t[:], op=ALU.mult),
                     R=[kT[par], EkT], W=[ktlT])
                V.op(lambda e, par=par: e.tensor_tensor(out=gg.t[:], in0=tm[par].t[:, :, 192:320], in1=gngT.t[:], op=ALU.mult),
                     R=[tm[par], gngT], W=[gg])
                yield "p2"
                ch0 = sb * 4
                css = [slice(i * 128, (i + 1) * 128) for i in range(4)]
                vaps = [tm[par].t[:, i, 64:192] for i in range(4)]
                for i in range(4):
                    P.op(lambda e, i=i: e.matmul(out=ps_A4.t[:, i, :], lhsT=ktlT.t[:, css[i]], rhs=qtl.t[:, css[i]], start=True, stop=True),
                         R=[ktlT, qtl], W=[ps_A4])
                for i in range(4):
                    V.op(lambda e, i=i: e.tensor_tensor(out=AmT[i].t[:], in0=ps_A4.t[:, i, :], in1=triU, op=ALU.mult),
                         R=[ps_A4, cfT], W=[AmT[i]])
                yield "a"
                for i in range(4):
                    P.op(lambda e, i=i: e.matmul(out=ps_U.t[:], lhsT=ktl.t[:, i, :], rhs=vaps[i], start=True, stop=True),
                         R=[ktl, tm[par]], W=[ps_U])
                    P.op(lambda e, i=i: e.matmul(out=ps_og4.t[:, i, :], lhsT=AmT[i].t[:], rhs=vaps[i], start=True, stop=False),
                         R=[AmT[i], tm[par]], W=[ps_og4])
                    P.op(lambda e, i=i: e.matmul(out=ps_og4.t[:, i, :], lhsT=qtl.t[:, css[i]], rhs=Sbf.t[:], start=False, stop=True),
                         R=[qtl, Sbf], W=[ps_og4])
                    acol = EqT.t[:, i * 128 + 127:i * 128 + 128]
                    V.op(lambda e: e.tensor_tensor(out=Stmp.t[:], in0=ps_U.t[:], in1=Sst.t[:], op=ALU.add),
                         R=[ps_U, Sst], W=[Stmp])
                    A.op(lambda e, acol=acol: e.activation(out=Sbf.t[:], in_=Stmp.t[:], func=AF.Copy, scale=acol),
                         R=[Stmp, EqT], W=[Sbf])
                    V.op(lambda e, acol=acol: e.tensor_scalar(out=Sst.t[:], in0=Stmp.t[:], scalar1=acol, scalar2=None, op0=ALU.mult),
                         R=[Stmp, EqT], W=[Sst])
                yield "b"
                for i in range(4):
                    A.op(lambda e, i=i: e.activation(out=junk2.t[:], in_=ps_og4.t[:, i, :], func=AF.Square, accum_out=ssg.t[:, ch0 + i:ch0 + i + 1]),
                         R=[ps_og4], W=[junk2, ssg])
                A.op(lambda e: e.activation(out=rsg.t[:, ch0:ch0 + 4], in_=ssg.t[:, ch0:ch0 + 4], func=AF.Sqrt, scale=1.0 / 128, bias=epsT.t[:, 1:2]),
                     R=[ssg, epsT], W=[rsg])
                V.op(lambda e: e.reciprocal(out=rsg.t[:, ch0:ch0 + 4], in_=rsg.t[:, ch0:ch0 + 4]), R=[rsg], W=[rsg])
                for i in range(4):
                    V.op(lambda e, i=i: e.scalar_tensor_tensor(out=og[i].t[:], in0=ps_og4.t[:, i, :], scalar=rsg.t[:, ch0 + i:ch0 + i + 1],
                                                               in1=gg.t[:, i, :], op0=ALU.mult, op1=ALU.mult),
                         R=[ps_og4, rsg, gg], W=[og[i]])
                yield "c"
                for i in range(4):
                    pt = ps_ogT_l[i % 2]
                    P.op(lambda e, i=i, pt=pt: e.transpose(out=pt.t[:], in_=og[i].t[:], identity=identB), R=[og[i], cbT], W=[pt])
                    cc = ((ch0 + i) % 16) * 128
                    A.op(lambda e, cc=cc, sup=sup, pt=pt: e.activation(out=ogT[sup % 2].t[:, cc:cc + 128], in_=pt.t[:], func=AF.Copy),
                         R=[pt], W=[ogT[sup % 2]])

            def attention(sup):
                qp = sup % 2
                units = []
                for d in ATT_DS:
                    for r in range(d):
                        for n in range(16 // d):
                            units.append((d, r, n))
                ui = [0]

                def kcols(t0, d):
                    slot = (t0 // 2048) % 3
                    c0 = t0 % 2048
                    return slot, slice(c0, c0 + 127 * d + 1, d)

                def scores(u):
                    d, r, n = units[u]
                    sl3 = u % 3
                    qb = r + d * 128 * n
                    qsl = slice(qb, qb + 127 * d + 1, d)
                    t0 = sup * 2048 + qb
                    has_prev = (t0 - 128 * d) >= 0
                    cur = kcols(t0, d)
                    prev = kcols(t0 - 128 * d, d) if has_prev else cur
                    pss = ps_s_l[u % 3]
                    psv = ps_v_l[u % 2]
                    for h in range(2):
                        for blk, (ks, kc) in enumerate((prev, cur)):
                            P.op(lambda e, h=h, blk=blk, ks=ks, kc=kc, qsl=qsl, pss=pss: e.matmul(
                                out=pss.t[:, (h * 2 + blk) * 128:(h * 2 + blk + 1) * 128], lhsT=KT[h][ks].t[:, kc],
                                rhs=QT[h][qp].t[:, qsl], start=True, stop=True), R=[KT[h][ks], QT[h][qp]], W=[pss])
                    for h in range(2):
                        for blk, (ks, kc) in enumerate((prev, cur)):
                            P.op(lambda e, h=h, blk=blk, ks=ks, kc=kc, psv=psv: e.transpose(
                                out=psv.t[:, h * 2 + blk, :], in_=VT[h][ks].t[:, kc], identity=cbT.t[0:64, 0:64]),
                                R=[VT[h][ks], cbT], W=[psv])
                    A.op(lambda e, sl3=sl3, pss=pss: e.activation(out=pTa[sl3].t[:], in_=pss.t[:], func=AF.Exp, scale=0.125),
                         R=[pss], W=[pTa[sl3]])
                    V.op(lambda e, sl3=sl3, psv=psv: e.tensor_copy(out=vp[sl3].t[:, :, 0:64], in_=psv.t[:, :, :]), R=[psv], W=[vp[sl3]])
                    V.op(lambda e, sl3=sl3: e.tensor_tensor(out=pTa[sl3].t[:], in0=pTa[sl3].t[:], in1=mask4, op=ALU.mult),
                         R=[pTa[sl3], cbT], W=[pTa[sl3]])
                    return has_prev

                def pv(u, has_prev):
                    d, r, n = units[u]
                    sl3 = u % 3
                    qb = r + d * 128 * n
                    qsl = slice(qb, qb + 127 * d + 1, d)
                    blks = (0, 1) if has_prev else (1,)
                    ps_o = ps_o_l[u % 2]
                    for h in range(2):
                        for bi, blk in enumerate(blks):
                            P.op(lambda e, h=h, blk=blk, bi=bi, sl3=sl3, nb=len(blks), ps_o=ps_o: e.matmul(
                                out=ps_o.t[:, h, :], lhsT=vp[sl3].t[:, h * 2 + blk, :],
                                rhs=pTa[sl3].t[:, (h * 2 + blk) * 128:(h * 2 + blk + 1) * 128],
                                start=(bi == 0), stop=(bi == nb - 1)), R=[vp[sl3], pTa[sl3]], W=[ps_o])
                    if d == 1:
                        V.op(lambda e, qsl=qsl, ps_o=ps_o: e.tensor_copy(out=Oacc.t[:, :, qsl], in_=ps_o.t[:, :, :]), R=[ps_o], W=[Oacc])
                    else:
                        V.op(lambda e, qsl=qsl, ps_o=ps_o: e.tensor_tensor(out=Oacc.t[:, :, qsl], in0=ps_o.t[:, :, :], in1=Oacc.t[:, :, qsl],
                                                                op=ALU.add), R=[ps_o, Oacc], W=[Oacc])

                hp = {}
                for u in range(len(units) + 1):
                    if u < len(units):
                        hp[u] = scores(u)
                    if u >= 1 and ATT_MODE >= 2:
                        pv(u - 1, hp[u - 1])
                for h in range(2 if ATT_MODE >= 3 else 0):
                    for hf in range(2):
                        cs_ = slice(hf * 1024, (hf + 1) * 1024)
                        V.op(lambda e, h=h, cs_=cs_: e.reciprocal(out=rden.t[:], in_=Oacc.t[64:128, h, cs_]), R=[Oacc], W=[rden])
                        V.op(lambda e, h=h, cs_=cs_: e.tensor_tensor(out=oTa[qp].t[h * 64:(h + 1) * 64, cs_], in0=Oacc.t[0:64, h, cs_], in1=rden.t[:],
                                                                     op=ALU.mult), R=[Oacc, rden], W=[oTa[qp]])

            def exchange(sup):
                qp = sup % 2
                S.dma(lambda e: e.dma_start(out=a_int[sup][0:128, :], in_=ogT[qp].t[:]), stsl[qp], R=[ogT[qp]], W=[a_buf[sup]])
                S.dma(lambda e: e.dma_start(out=a_int[sup][128:256, :], in_=oTa[qp].t[:]), stsl[qp], R=[oTa[qp]], W=[a_buf[sup]])
                if do_coll:
                  G.coll(lambda e: e.collective_compute("AllGather", ALU.bypass, replica_groups=[[0, 1, 2, 3], [4, 5, 6, 7]],
                                                      ins=[a_int[sup]], outs=[b_all[sup * 1024:(sup + 1) * 1024, :]]), R=[a_buf[sup]], W=[b_buf[sup]])

            wcs = [pg.slot() for _ in range(4)]

            def precast(ex):
                for (src, dst) in ((w1, w1b), (w3, w3b), (w2, w2b)):
                    G.dma(lambda e, src=src, dst=dst, ex=ex: e.dma_start(
                        out=dst[ex].rearrange("(p r) f -> p (r f)", p=128), in_=src[ex].rearrange("(p r) f -> p (r f)", p=128)),
                        wcs[ex % 4], W=[wb_buf[ex]])

            gen1 = {}
            gen2 = {}

            def adv(gd, k):
                if k in gd:
                    if next(gd[k], None) is None:
                        del gd[k]

            for it in range(nsb_run + 2):
                if phase2 and it < NSB:
                    precast(it)
                if it < nsb_run:
                    gen1[it] = stage1(it)
                if it >= 2 and do_gla:
                    gen2[it - 2] = gla(it - 2)
                adv(gen1, it)
                adv(gen2, it - 2)
                adv(gen1, it - 1)
                adv(gen2, it - 2)
                adv(gen1, it - 1)
                adv(gen2, it - 2)
                adv(gen1, it - 1)
                adv(gen2, it - 2)
                adv(gen1, it - 1)
                adv(gen2, it - 2)
                adv(gen1, it)
                adv(gen2, it - 2)
                adv(gen1, it - 1)
                adv(gen2, it - 2)
                sbd = it - 2
                if sbd >= 0 and sbd % 4 == 3:
                    if do_att:
                        attention(sbd // 4)
                    if do_gla and do_att:
                        exchange(sbd // 4)
            assert not gen1 and not gen2, (list(gen1), list(gen2))
            if debug and not (do_gla and do_att):
                dbg1 = Buf("dbg1")
                S.dma(lambda e: e.dma_start(out=dbg_o[0:64, :], in_=QT[0][0].t[:]), stsl[0], R=[QT[0][0]], W=[dbg1])
                S.dma(lambda e: e.dma_start(out=dbg_o[128:192, :], in_=KT[0][0].t[:]), stsl[0], R=[KT[0][0]], W=[dbg1])
                S.dma(lambda e: e.dma_start(out=dbg_o[256:384, :], in_=ogT[0].t[:]), stsl[0], R=[ogT[0]], W=[dbg1])
                S.dma(lambda e: e.dma_start(out=dbg_o[384:512, :], in_=oTa[0].t[:]), stsl[0], R=[oTa[0]], W=[dbg1])
                S.wait_all([dbg1])
            pg.es = es0
        dslot = pg.slot()
        if debug and do_coll:
            S.dma(lambda e: e.dma_start(out=dbg_o, in_=b_all), dslot, R=b_buf, W=[out_buf])
        if debug and not do_coll and do_gla and do_att:
            for sup in range(nsb_run // 4):
                S.dma(lambda e, sup=sup: e.dma_start(out=dbg_o[sup * 256:(sup + 1) * 256, :], in_=a_int[sup]), dslot, R=[a_buf[sup]], W=[out_buf])
        if phase2:
          with ExitStack() as es2:
            pg.es = es2
            hh = pg.sb("hh", [128, 16, D], F32)
            gfT = pg.sb("gfT", [128, D], F32)
            ss3 = pg.sb("ss3", [128, 16], F32)
            rs3 = pg.sb("rs3", [128, 16], F32)
            gates = pg.sb("gates", [128, 16, 2], F32)
            dstf = pg.sb("dstf", [128, 16, 2], F32)
            dsti = pg.sb("dsti", [128, 16, 2], I32)

            es2a = ExitStack()
            pg.es = es2a
            ridx = pg.sb("ridx", [128, 8], I32)
            oT = pg.sb("oT", [128, 8, 2048], BF16)
            Wo = pg.sb("Wo", [128, 8, D], BF16)
            g2T = pg.sb("g2T", [128, D], F32)
            wrT = pg.sb("wrT", [128, 8, 36], F32)
            brT = pg.sb("brT", [128, 36], F32)
            xr = [pg.sb("xr%d" % i, [128, D], F32) for i in range(2)]
            xrs = [pg.slot() for _ in range(2)]
            junk3 = pg.sb("junk3", [128, D], BF16)
            ss2 = pg.sb("ss2", [128, 16], F32)
            rs2 = pg.sb("rs2", [128, 16], F32)
            n2f = [pg.sb("n2f%d" % i, [128, D], F32) for i in range(2)]
            n2b = [pg.sb("n2b%d" % i, [128, D], BF16) for i in range(2)]
            n2s = [pg.slot() for _ in range(2)]
            n2Ts = [pg.sb("n2T_%d" % i, [128, 8, 128], F32) for i in range(2)]
            lgs = [pg.sb("lg_%d" % i, [128, 36], F32) for i in range(2)]
            sms = [pg.sb("sm_%d" % i, [128, 16], F32) for i in range(2)]
            gmasks = [pg.sb("gmask_%d" % i, [128, 4], F32) for i in range(2)]
            pens = [pg.sb("pen_%d" % i, [128, 4], F32) for i in range(2)]
            gexs = [pg.sb("gex_%d" % i, [128, 4], F32) for i in range(2)]
            elms = [pg.sb("elm_%d" % i, [128, 32], F32) for i in range(2)]
            elm2s = [pg.sb("elm2_%d" % i, [128, 32], F32) for i in range(2)]
            mk1s = [pg.sb("mk1_%d" % i, [128, 32], F32) for i in range(2)]
            mk2s = [pg.sb("mk2_%d" % i, [128, 32], F32) for i in range(2)]
            cnts = [pg.sb("cnt_%d" % i, [128, 32], F32) for i in range(2)]
            tot = pg.sb("tot", [128, 32], F32)
            poss = [pg.sb("pos_%d" % i, [128, 32], F32) for i in range(2)]
            tmp32s = [pg.sb("tmp32_%d" % i, [128, 32], F32) for i in range(2)]
            ps_h = [carve("ps_h%d" % i, i, 0, 512) for i in range(2)]
            ps_t = [carve("ps_t%d" % i, 2 + i, 0, 512, F32, "p (a b) -> p a b", a=4) for i in range(2)]
            ps_y = [carve("ps_y%d" % i, 4 + i, 0, 512) for i in range(2)]
            ps_a2 = [Tile(banks[0][:, 0:256], "ps_a0", bank_buf[0]), Tile(banks[2][:, 0:256], "ps_a1", bank_buf[2])]
            ps_b2 = [Tile(banks[1][:, 0:256], "ps_b0", bank_buf[1]), Tile(banks[3][:, 0:256], "ps_b1", bank_buf[3])]
            ps_x = Tile(banks[7][:, 0:256].bitcast(BF16).rearrange("p (a b) -> p a b", a=2), "ps_x", bank_buf[7])
            ps_l = Tile(banks[7][:, 256:292], "ps_l", bank_buf[7])
            ps_r = Tile(banks[7][:, 320:352], "ps_r", bank_buf[7])
            ps_c = Tile(banks[7][:, 352:384], "ps_c", bank_buf[7])

            S.dma(lambda e: e.dma_start(out=ridx.t[:], in_=rowidx), cslot, W=[ridx])
            S.dma(lambda e: e.dma_start(out=g2T.t[:], in_=g2r), cslot, W=[g2T])
            S.dma(lambda e: e.dma_start(out=gfT.t[:], in_=gfr), cslot, W=[gfT])
            S.dma(lambda e: e.dma_start(out=brT.t[:], in_=brr), cslot, W=[brT])
            S.dma(lambda e: e.dma_start(out=wrT.t[:], in_=wr.rearrange("(c p) n -> p c n", p=128)), cslot, W=[wrT])
            for kc in range(8):
                G.dma(lambda e, kc=kc: e.dma_start(out=Wo.t[:, kc, :], in_=w_out[kc * 128:(kc + 1) * 128, :]), cslot_g, W=[Wo])
            V.op(lambda e: e.memset(ss2.t[:], 0.0), W=[ss2])
            V.op(lambda e: e.memset(ss3.t[:], 0.0), W=[ss3])
            V.op(lambda e: e.memset(tot.t[:], 0.0), W=[tot])
            for sm_ in sms:
                V.op(lambda e, sm_=sm_: e.memset(sm_.t[:], 0.0), W=[sm_])
            oslot = pg.slot()
            for c in range(8):
                G.dma(lambda e, c=c: e.indirect_dma_start(out=oT.t[:, c, :], out_offset=None, in_=b_all,
                                                          in_offset=bass.IndirectOffsetOnAxis(ap=ridx.t[:, c:c + 1], axis=0)),
                      oslot, R=b_buf + [ridx], W=[oT])
            wch = [(c // 2) + 4 * (c % 2) for c in range(8)]

            hhb = [Buf("hh%d" % i) for i in range(16)]
            gtb = [Buf("gt%d" % i) for i in range(16)]
            dfb = [Buf("df%d" % i) for i in range(16)]
            dib = [Buf("di%d" % i) for i in range(16)]

            def rstd_ops(ssT, rsT, ti, n, eps):
                A.op(lambda e: e.activation(out=rsT.t[:, ti:ti + 1], in_=ssT.t[:, ti:ti + 1], func=AF.Sqrt, scale=1.0 / n, bias=epsT.t[:, 0:1]),
                     R=[ssT, epsT], W=[rsT])
                V.op(lambda e: e.reciprocal(out=rsT.t[:, ti:ti + 1], in_=rsT.t[:, ti:ti + 1]), R=[rsT], W=[rsT])

            def route(ti):
                p2 = ti % 2
                n2T = n2Ts[p2]
                lg = lgs[p2]
                sm = sms[p2]
                gmask = gmasks[p2]
                pen = pens[p2]
                gex = gexs[p2]
                elm = elms[p2]
                elm2 = elm2s[p2]
                mk1 = mk1s[p2]
                mk2 = mk2s[p2]
                cnt = cnts[p2]
                pos = poss[p2]
                tmp32 = tmp32s[p2]
                tsl = slice(ti * 128, (ti + 1) * 128)
                precast(NSB + ti)
                S.dma(lambda e, tsl=tsl, p2=p2: e.dma_start(out=xr[p2].t[:], in_=xres[tsl, :]), xrs[p2], W=[xr[p2]])
                for half in range(2):
                    for c in range(8):
                        P.op(lambda e, c=c, half=half, tsl=tsl: e.matmul(
                            out=ps_h[half].t[:], lhsT=oT.t[:, c, tsl], rhs=Wo.t[:, wch[c], half * 512:(half + 1) * 512],
                            start=(c == 0), stop=(c == 7)), R=[oT, Wo], W=[ps_h[half]])
                    V.op(lambda e, half=half, ti=ti, p2=p2: e.tensor_tensor(
                        out=hh.t[:, ti, half * 512:(half + 1) * 512], in0=ps_h[half].t[:], in1=xr[p2].t[:, half * 512:(half + 1) * 512],
                        op=ALU.add), R=[ps_h[half], xr[p2]], W=[hhb[ti]])
                    yield
                A.op(lambda e, ti=ti: e.activation(out=junk3.t[:], in_=hh.t[:, ti, :], func=AF.Square, accum_out=ss2.t[:, ti:ti + 1]),
                     R=[hhb[ti]], W=[junk3, ss2])
                yield
                rstd_ops(ss2, rs2, ti, D, EPS)
                V.op(lambda e, ti=ti, p2=p2: e.scalar_tensor_tensor(out=n2f[p2].t[:], in0=hh.t[:, ti, :], scalar=rs2.t[:, ti:ti + 1],
                                                                    in1=g2T.t[:], op0=ALU.mult, op1=ALU.mult),
                     R=[hhb[ti], rs2, g2T], W=[n2f[p2]])
                yield
                A.op(lambda e, p2=p2: e.activation(out=n2b[p2].t[:], in_=n2f[p2].t[:], func=AF.Copy), R=[n2f[p2]], W=[n2b[p2]])
                yield
                for q in range(2):
                    for c4 in range(4):
                        c = q * 4 + c4
                        P.op(lambda e, c=c, c4=c4, q=q, p2=p2: e.transpose(out=ps_t[q].t[:, c4, :], in_=n2f[p2].t[:, c * 128:(c + 1) * 128],
                                                                           identity=identF), R=[n2f[p2], cfT], W=[ps_t[q]])
                    if q == 0:
                        A.op(lambda e: e.activation(out=n2T.t[:, 0:4, :], in_=ps_t[0].t[:], func=AF.Copy), R=[ps_t[0]], W=[n2T])
                        yield
                    else:
                        V.op(lambda e: e.tensor_copy(out=n2T.t[:, 4:8, :], in_=ps_t[1].t[:]), R=[ps_t[1]], W=[n2T])
                        yield
                for c in range(8):
                    P.op(lambda e, c=c: e.matmul(out=ps_l.t[:], lhsT=n2T.t[:, c, :], rhs=wrT.t[:, c, :], start=(c == 0), stop=(c == 7)),
                         R=[n2T, wrT], W=[ps_l])
                V.op(lambda e: e.tensor_tensor(out=lg.t[:], in0=ps_l.t[:], in1=brT.t[:], op=ALU.add), R=[ps_l, brT], W=[lg])
                yield
                gl = lg.t[:, 0:4]
                el = lg.t[:, 4:36]
                c_ = lambda k: sm.t[:, k:k + 1]
                V.op(lambda e: e.reduce_max(out=c_(0), in_=gl, axis=AX.X), R=[lg], W=[sm])
                yield
                V.op(lambda e: e.tensor_scalar(out=gmask.t[:], in0=gl, scalar1=c_(0), scalar2=None, op0=ALU.is_equal), R=[lg, sm], W=[gmask])
                yield
                V.op(lambda e: e.tensor_single_scalar(out=c_(1), in_=c_(0), scalar=-1.0, op=ALU.mult), R=[sm], W=[sm])
                yield
                V.op(lambda e: e.memset(c_(2), 0.0), W=[sm])
                yield
                A.op(lambda e: e.activation(out=gex.t[:], in_=gl, func=AF.Exp, bias=c_(1), accum_out=c_(2)), R=[lg, sm], W=[gex, sm])
                yield
                V.op(lambda e: e.reciprocal(out=c_(3), in_=c_(2)), R=[sm], W=[sm])
                yield
                V.op(lambda e: e.tensor_scalar(out=pen.t[:], in0=gmask.t[:], scalar1=-1.0, scalar2=BIG, op0=ALU.add, op1=ALU.mult),
                     R=[gmask], W=[pen])
                yield
                for g in range(4):
                    V.op(lambda e, g=g: e.tensor_scalar(out=elm.t[:, g * 8:(g + 1) * 8], in0=lg.t[:, 4 + g * 8:12 + g * 8],
                                                        scalar1=pen.t[:, g:g + 1], scalar2=None, op0=ALU.add), R=[lg, pen], W=[elm])
                    yield
                V.op(lambda e: e.reduce_max(out=c_(4), in_=elm.t[:], axis=AX.X), R=[elm], W=[sm])
                yield
                V.op(lambda e: e.tensor_scalar(out=mk1.t[:], in0=elm.t[:], scalar1=c_(4), scalar2=None, op0=ALU.is_equal), R=[elm, sm], W=[mk1])
                yield
                V.op(lambda e: e.scalar_tensor_tensor(out=elm2.t[:], in0=mk1.t[:], scalar=-BIG, in1=elm.t[:], op0=ALU.mult, op1=ALU.add),
                     R=[mk1, elm], W=[elm2])
                yield
                V.op(lambda e: e.reduce_max(out=c_(5), in_=elm2.t[:], axis=AX.X), R=[elm2], W=[sm])
                yield
                V.op(lambda e: e.tensor_scalar(out=mk2.t[:], in0=elm2.t[:], scalar1=c_(5), scalar2=None, op0=ALU.is_equal), R=[elm2, sm], W=[mk2])
                yield
                V.op(lambda e: e.tensor_tensor(out=c_(6), in0=c_(5), in1=c_(4), op=ALU.subtract), R=[sm], W=[sm])
                yield
                A.op(lambda e: e.activation(out=c_(7), in_=c_(6), func=AF.Exp), R=[sm], W=[sm])
                yield
                V.op(lambda e: e.tensor_single_scalar(out=c_(8), in_=c_(7), scalar=1.0, op=ALU.add), R=[sm], W=[sm])
                yield
                V.op(lambda e: e.reciprocal(out=c_(9), in_=c_(8)), R=[sm], W=[sm])
                yield
                V.op(lambda e, ti=ti: e.tensor_tensor(out=gates.t[:, ti, 0:1], in0=c_(9), in1=c_(3), op=ALU.mult), R=[sm], W=[gtb[ti]])
                yield
                V.op(lambda e, ti=ti: e.tensor_tensor(out=gates.t[:, ti, 1:2], in0=gates.t[:, ti, 0:1], in1=c_(7), op=ALU.mult),
                     R=[sm, gtb[ti]], W=[gtb[ti]])
                yield
                V.op(lambda e: e.tensor_tensor(out=cnt.t[:], in0=mk1.t[:], in1=mk2.t[:], op=ALU.add), R=[mk1, mk2], W=[cnt])
                yield
                P.op(lambda e: e.matmul(out=ps_r.t[:], lhsT=strictT, rhs=cnt.t[:], start=True, stop=True), R=[cnt, cfT], W=[ps_r])
                P.op(lambda e: e.matmul(out=ps_c.t[:], lhsT=onesF, rhs=cnt.t[:], start=True, stop=True), R=[cnt, cfT], W=[ps_c])
                V.op(lambda e: e.tensor_tensor(out=pos.t[:], in0=ps_r.t[:], in1=tot.t[:], op=ALU.add), R=[ps_r, tot], W=[pos])
                V.op(lambda e: e.tensor_tensor(out=tot.t[:], in0=ps_c.t[:], in1=tot.t[:], op=ALU.add), R=[ps_c, tot], W=[tot])
                yield
                V.op(lambda e: e.tensor_single_scalar(out=pos.t[:], in_=pos.t[:], scalar=float(CAP - 1), op=ALU.min), R=[pos], W=[pos])
                yield
                V.op(lambda e: e.tensor_tensor(out=pos.t[:], in0=pos.t[:], in1=ecol, op=ALU.add), R=[pos, cfT], W=[pos])
                yield
                for k, mk in enumerate((mk1, mk2)):
                    V.op(lambda e, mk=mk: e.tensor_tensor(out=tmp32.t[:], in0=pos.t[:], in1=mk.t[:], op=ALU.mult), R=[pos, mk], W=[tmp32])
                    yield
                    V.op(lambda e, k=k, ti=ti: e.reduce_sum(out=dstf.t[:, ti, k:k + 1], in_=tmp32.t[:], axis=AX.X), R=[tmp32], W=[dfb[ti]])
                    yield
                V.op(lambda e, ti=ti: e.tensor_copy(out=dsti.t[:, ti, :], in_=dstf.t[:, ti, :]), R=[dfb[ti]], W=[dib[ti]])
                yield
                for k in range(2):
                    G.dma(lambda e, k=k, ti=ti, p2=p2: e.indirect_dma_start(
                        out=Xs, out_offset=bass.IndirectOffsetOnAxis(ap=dsti.t[:, ti, k:k + 1], axis=0), in_=n2b[p2].t[:, :], in_offset=None),
                        n2s[p2], R=[n2b[p2], dib[ti]], W=[Xs_buf])
            DONE2 = object()
            for pr_ in range(8):
                gens = [route(2 * pr_), route(2 * pr_ + 1)]
                while gens:
                    for g_ in list(gens):
                        if next(g_, DONE2) is DONE2:
                            gens.remove(g_)
            if debug:
                S.dma(lambda e: e.dma_start(out=dbg_h.rearrange("(t p) d -> p t d", p=128), in_=hh.t[:]), dslot, R=hhb, W=[out_buf])

            es2a.close()
            es2b = ExitStack()
            pg.es = es2b
            junk4 = pg.sb("junk3b", [128, D], BF16)
            xg = [pg.sb("xg%d" % i, [128, 2, D], BF16) for i in range(2)]
            xgs = [pg.slot() for _ in range(2)]
            xT = [pg.sb("xT%d" % i, [128, 8, 256], BF16) for i in range(2)]
            w1s = [pg.sb("w1s%d" % i, [128, 8, 512], BF16) for i in range(2)]
            w3s = [pg.sb("w3s%d" % i, [128, 8, 512], BF16) for i in range(2)]
            w2s = [pg.sb("w2s%d" % i, [128, 4, D], BF16) for i in range(2)]
            wsl = [pg.slot() for _ in range(2)]
            sa = [pg.sb("sa%d" % i, [128, 256], F32) for i in range(2)]
            hT = [pg.sb("hT%d" % i, [128, 4, 256], BF16) for i in range(2)]
            ysb = [pg.sb("ysb%d" % i, [128, 2, D], F32) for i in range(1)]
            yss = [pg.slot() for _ in range(2)]
            ya = [pg.sb("ya%d" % i, [128, D], F32) for i in range(2)]
            yb = [pg.sb("yb%d" % i, [128, D], F32) for i in range(2)]
            ygs = [pg.slot() for _ in range(2)]
            ot = [pg.sb("ot%d" % i, [128, D], F32) for i in range(2)]
            ots = [pg.slot() for _ in range(2)]
            Xs_all = Buf("Xs_all")
            Xs_all.w = dict(Xs_buf.w)
            for p2 in range(2):
                Xs_all.w[id(n2s[p2].s)] = (n2s[p2].s, n2s[p2].v)
            def eloads(ex):
                p2 = ex % 2
                S.dma(lambda e, ex=ex, p2=p2: e.dma_start(out=xg[p2].t[:], in_=Xs[ex * CAP:(ex + 1) * CAP, :].rearrange("(s p) f -> p s f", p=128)),
                      xgs[p2], R=[Xs_all], W=[xg[p2]])
                S.dma(lambda e, ex=ex, p2=p2: e.dma_start(out=w1s[p2].t[:], in_=w1b[ex].rearrange("(c p) f -> p c f", p=128)), wsl[p2], R=[wb_buf[ex]], W=[w1s[p2]])
                S.dma(lambda e, ex=ex, p2=p2: e.dma_start(out=w3s[p2].t[:], in_=w3b[ex].rearrange("(c p) f -> p c f", p=128)), wsl[p2], R=[wb_buf[ex]], W=[w3s[p2]])
                S.dma(lambda e, ex=ex, p2=p2: e.dma_start(out=w2s[p2].t[:], in_=w2b[ex].rearrange("(c p) f -> p c f", p=128)), wsl[p2], R=[wb_buf[ex]], W=[w2s[p2]])

            eloads(0)
            for ex in range(NE):
                p2 = ex % 2
                if ex + 1 < NE:
                    eloads(ex + 1)
                for cq in range(4):
                    for cc in range(2):
                        c = cq * 2 + cc
                        for s in range(2):
                            P.op(lambda e, c=c, cc=cc, s=s, p2=p2: e.transpose(out=ps_x.t[:, cc, s * 128:(s + 1) * 128],
                                                                               in_=xg[p2].t[:, s, c * 128:(c + 1) * 128], identity=identB),
                                 R=[xg[p2], cbT], W=[ps_x])
                    if cq % 2 == 0:
                        V.op(lambda e, cq=cq, p2=p2: e.tensor_copy(out=xT[p2].t[:, cq * 2:cq * 2 + 2, :], in_=ps_x.t[:]), R=[ps_x], W=[xT[p2]])
                    else:
                        A.op(lambda e, cq=cq, p2=p2: e.activation(out=xT[p2].t[:, cq * 2:cq * 2 + 2, :], in_=ps_x.t[:], func=AF.Copy),
                             R=[ps_x], W=[xT[p2]])
                for fc in range(4):
                    f2 = fc % 2
                    ps_a = ps_a2[f2]
                    ps_b = ps_b2[f2]
                    for c in range(8):
                        P.op(lambda e, c=c, fc=fc, p2=p2, ps_a=ps_a: e.matmul(out=ps_a.t[:], lhsT=w1s[p2].t[:, c, fc * 128:(fc + 1) * 128], rhs=xT[p2].t[:, c, :],
                                                                   start=(c == 0), stop=(c == 7)), R=[w1s[p2], xT[p2]], W=[ps_a])
                    for c in range(8):
                        P.op(lambda e, c=c, fc=fc, p2=p2, ps_b=ps_b: e.matmul(out=ps_b.t[:], lhsT=w3s[p2].t[:, c, fc * 128:(fc + 1) * 128], rhs=xT[p2].t[:, c, :],
                                                                   start=(c == 0), stop=(c == 7)), R=[w3s[p2], xT[p2]], W=[ps_b])
                    A.op(lambda e, f2=f2, ps_a=ps_a: e.activation(out=sa[f2].t[:], in_=ps_a.t[:], func=AF.Silu), R=[ps_a], W=[sa[f2]])
                    V.op(lambda e, f2=f2, fc=fc, p2=p2, ps_b=ps_b: e.tensor_tensor(out=hT[p2].t[:, fc, :], in0=ps_b.t[:], in1=sa[f2].t[:], op=ALU.mult),
                         R=[ps_b, sa[f2]], W=[hT[p2]])
                for s in range(2):
                    for half in range(2):
                        yy = ps_y[half]
                        for fc in range(4):
                            P.op(lambda e, fc=fc, s=s, half=half, p2=p2, yy=yy: e.matmul(
                                out=yy.t[:], lhsT=hT[p2].t[:, fc, s * 128:(s + 1) * 128], rhs=w2s[p2].t[:, fc, half * 512:(half + 1) * 512],
                                start=(fc == 0), stop=(fc == 3)), R=[hT[p2], w2s[p2]], W=[yy])
                        if half == 0:
                            V.op(lambda e, s=s, p2=p2, yy=yy: e.tensor_copy(out=ysb[0].t[:, s, 0:512], in_=yy.t[:]), R=[yy], W=[ysb[0]])
                        else:
                            A.op(lambda e, s=s, p2=p2, yy=yy: e.activation(out=ysb[0].t[:, s, 512:1024], in_=yy.t[:], func=AF.Copy), R=[yy], W=[ysb[0]])
                G.dma(lambda e, ex=ex, p2=p2: e.dma_start(out=Ys[ex * CAP:(ex + 1) * CAP, :].rearrange("(s p) f -> p s f", p=128), in_=ysb[0].t[:]),
                      yss[0], R=[ysb[0]], W=[Ys_buf[ex]])

            def gathers(ti):
                p2 = ti % 2
                G.dma(lambda e, ti=ti, p2=p2: e.indirect_dma_start(out=ya[p2].t[:, :], out_offset=None, in_=Ys,
                                                                   in_offset=bass.IndirectOffsetOnAxis(ap=dsti.t[:, ti, 0:1], axis=0)),
                      ygs[p2], R=Ys_buf + [dib[ti]], W=[ya[p2]])
                G.dma(lambda e, ti=ti, p2=p2: e.indirect_dma_start(out=yb[p2].t[:, :], out_offset=None, in_=Ys,
                                                                   in_offset=bass.IndirectOffsetOnAxis(ap=dsti.t[:, ti, 1:2], axis=0)),
                      ygs[p2], R=Ys_buf + [dib[ti]], W=[yb[p2]])

            gathers(0)
            for ti in range(16):
                p2 = ti % 2
                if ti + 1 < 16:
                    gathers(ti + 1)
                V.op(lambda e, ti=ti, p2=p2: e.scalar_tensor_tensor(out=hh.t[:, ti, :], in0=ya[p2].t[:], scalar=gates.t[:, ti, 0:1], in1=hh.t[:, ti, :],
                                                                    op0=ALU.mult, op1=ALU.add), R=[ya[p2], gtb[ti], hhb[ti]], W=[hhb[ti]])
                V.op(lambda e, ti=ti, p2=p2: e.scalar_tensor_tensor(out=hh.t[:, ti, :], in0=yb[p2].t[:], scalar=gates.t[:, ti, 1:2], in1=hh.t[:, ti, :],
                                                                    op0=ALU.mult, op1=ALU.add), R=[yb[p2], gtb[ti], hhb[ti]], W=[hhb[ti]])
                A.op(lambda e, ti=ti: e.activation(out=junk4.t[:], in_=hh.t[:, ti, :], func=AF.Square, accum_out=ss3.t[:, ti:ti + 1]),
                     R=[hhb[ti]], W=[junk4, ss3])
                rstd_ops(ss3, rs3, ti, D, EPS)
                V.op(lambda e, ti=ti, p2=p2: e.scalar_tensor_tensor(out=ot[p2].t[:], in0=hh.t[:, ti, :], scalar=rs3.t[:, ti:ti + 1], in1=gfT.t[:],
                                                                    op0=ALU.mult, op1=ALU.mult), R=[hhb[ti], rs3, gfT], W=[ot[p2]])
                S.dma(lambda e, ti=ti, p2=p2: e.dma_start(out=out[ti * 128:(ti + 1) * 128, :], in_=ot[p2].t[:]), ots[p2], R=[ot[p2]], W=[out_buf])
            fin = Buf("fin")
            for sl in ots + [dslot]:
                if sl.v:
                    fin.w[id(sl.s)] = (sl.s, sl.v)
            S.wait_all([fin])
            es2b.close()
            pg.es = es0
        else:
            fin = Buf("fin")
            if dslot.v:
                fin.w[id(dslot.s)] = (dslot.s, dslot.v)
            S.wait_all([fin])
        pg.emit()
    return nc


def _consts():
    a = np.arange(128)
    ident = np.eye(128, dtype=np.float32)
    triu = (a[:, None] <= a[None, :]).astype(np.float32)
    strict = (a[:, None] < a[None, :]).astype(np.float32)
    ones = np.ones((128, 128), np.float32)
    ecol = np.tile((np.arange(NE) * CAP).astype(np.float32)[None, :], (128, 1))
    cf = np.concatenate([ident, triu, strict, ones, ecol], axis=1)
    L = (a[:, None] >= a[None, :]).astype(np.float32)
    U = triu
    cb = np.concatenate([ident, L, U, L, U], axis=1).astype(ml_dtypes.bfloat16)
    return np.ascontiguousarray(cf), np.ascontiguousarray(cb)


def make_in_maps(x, norm1_g, w_in, gla_gate_w2, gla_gate_b, gla_norm_g, w_out, norm2_g,
                 router_group_w, router_group_b, router_expert_w, router_expert_b,
                 expert_w1, expert_w3, expert_w2, final_norm_g):
    f = lambda a: np.ascontiguousarray(np.asarray(a, dtype=np.float32))
    x = f(x)
    win = f(w_in)[0]
    cf, cb = _consts()
    gq0, gk0, gv0, gr0, glr0, aq0, ak0, av0 = 0, 256, 512, 1024, 1536, 1552, 2064, 2576
    w1 = f(expert_w1)[0]
    w3 = f(expert_w3)[0]
    w2 = f(expert_w2)[0]
    wo = f(w_out)[0]
    g2r = f(np.tile(np.asarray(norm2_g)[0][None, :], (128, 1)))
    gfr = f(np.tile(np.asarray(final_norm_g)[None, :], (128, 1)))
    wr = f(np.concatenate([np.asarray(router_group_w)[0], np.asarray(router_expert_w)[0]], axis=1))
    br = np.concatenate([np.asarray(router_group_b)[0], np.asarray(router_expert_b)[0]])
    brr = f(np.tile(br[None, :], (128, 1)))
    g1 = f(np.asarray(norm1_g)[0].reshape(8, 128).T)
    gng = f(np.tile(np.asarray(gla_norm_g)[0][None, :], (128, 4)))
    maps = []
    for c in range(8):
        b, j = c // 4, c % 4
        cols = np.concatenate([
            np.arange(gq0 + 64 * j, gq0 + 64 * j + 64), np.arange(gk0 + 64 * j, gk0 + 64 * j + 64),
            np.arange(aq0 + 128 * j, aq0 + 128 * j + 128), np.arange(ak0 + 128 * j, ak0 + 128 * j + 128),
            np.arange(av0 + 128 * j, av0 + 128 * j + 128), np.arange(glr0, glr0 + 16),
            np.arange(gk0 + 64 * j, gk0 + 64 * j + 64), np.arange(gv0 + 128 * j, gv0 + 128 * j + 128),
            np.arange(gr0 + 128 * j, gr0 + 128 * j + 128)])
        w2aug = np.concatenate([np.asarray(gla_gate_w2)[0][:, 64 * j:64 * j + 64],
                                np.asarray(gla_gate_b)[0][None, 64 * j:64 * j + 64]], axis=0)
        rowidx = (j * 1024 + np.arange(8)[None, :] * 128 + np.arange(128)[:, None]).astype(np.int32)
        maps.append({
            "x": x[b], "xres": np.ascontiguousarray(x[b, 2048 * j:2048 * (j + 1)]),
            "w_in": np.ascontiguousarray(win[:, cols]), "g1": g1, "w2aug": f(w2aug), "gng": gng,
            "w_out": wo, "g2r": g2r, "gfr": gfr, "wr": wr, "brr": brr, "w1": w1, "w3": w3, "w2": w2,
            "cf": cf, "cb": cb, "rowidx": np.ascontiguousarray(rowidx),
        })
    return maps


_NC_CACHE = {}


def kernel(**inputs):
    if "nc" not in _NC_CACHE:
        _NC_CACHE["nc"] = build()
    nc = _NC_CACHE["nc"]
    maps = make_in_maps(**inputs)
    res = run_bass_kernel_spmd(nc, maps, core_ids=list(range(8)))
    outs = [np.asarray(res.results[c]["out"], dtype=np.float32) for c in range(8)]
    y = np.stack([np.concatenate(outs[0:4], axis=0), np.concatenate(outs[4:8], axis=0)], axis=0)
    return y
```

```python
import numpy as np
import ml_dtypes
from contextlib import ExitStack
import concourse.bass as bass
import concourse.mybir as mybir
from concourse.bass_utils import run_bass_kernel_spmd

F32 = mybir.dt.float32
BF16 = mybir.dt.bfloat16
I32 = mybir.dt.int32
AF = mybir.ActivationFunctionType
ALU = mybir.AluOpType
AX = mybir.AxisListType

T = 8192
D = 1024
NSB = 16
NSUP = 4
CAP = 256
NE = 32
EPS = 1e-6
BIG = 1.0e30
RING = 6144
SEM_LIMIT = 12000
import os
ATT_MODE = int(os.environ.get('ATT_MODE', '3'))
MASK_ENG = os.environ.get('MASK_ENG', 'V')
ATT_NOTR = int(os.environ.get('ATT_NOTR', '0'))
ATT_DS = tuple(int(v) for v in os.environ.get('ATT_DS', '1,4,16').split(','))


class Buf:
    __slots__ = ("name", "w", "r", "psum")

    def __init__(self, name, psum=False):
        self.name = name
        self.w = {}
        self.r = {}
        self.psum = psum


class Tile:
    def __init__(self, t, name, buf=None):
        self.t = t
        self.b = buf if buf is not None else Buf(name)


def _split_psum(R, W):
    R2, W2 = [], list(W)
    for x in R:
        b = x.b if isinstance(x, Tile) else x
        if b.psum:
            W2.append(x)
        else:
            R2.append(x)
    return R2, W2


class SemSlot:
    def __init__(self, sem):
        self.s = sem
        self.v = 0


class Eng:
    def __init__(self, prog, name, is_pe=False):
        self.prog = prog
        self.name = name
        self.is_pe = is_pe
        self.sem = prog.new_sem()
        self.cnt = 0
        self.known = {}
        self.thunks = []

    def _wait(self, sem, val):
        if self.known.get(id(sem), 0) >= val:
            return
        self.known[id(sem)] = val
        self.thunks.append(lambda e, s=sem, v=val: e.wait_ge(s, v))

    def _deps(self, R, W):
        for x in R:
            b = x.b if isinstance(x, Tile) else x
            for (sem, val) in b.w.values():
                if sem is self.sem and self.is_pe:
                    continue
                self._wait(sem, val)
        for x in W:
            b = x.b if isinstance(x, Tile) else x
            for (sem, val) in list(b.w.values()) + list(b.r.values()):
                if sem is self.sem and self.is_pe:
                    continue
                self._wait(sem, val)

    def _record(self, R, W, sem, val):
        for x in R:
            b = x.b if isinstance(x, Tile) else x
            b.r[id(sem)] = (sem, val)
        for x in W:
            b = x.b if isinstance(x, Tile) else x
            b.w = {id(sem): (sem, val)}
            b.r = {}

    def op(self, fn, R=(), W=()):
        R, W = _split_psum(R, W)
        self._deps(R, W)
        if self.cnt >= SEM_LIMIT:
            self.sem = self.prog.new_sem()
            self.cnt = 0
        self.cnt += 1
        sem, val = self.sem, self.cnt
        self.thunks.append(lambda e, f=fn, s=sem: f(e).then_inc(s, 1))
        self._record(R, W, sem, val)

    def dma(self, fn, slot, R=(), W=()):
        self._deps(R, W)
        slot.v += 16
        sem, val = slot.s, slot.v
        self.thunks.append(lambda e, f=fn, s=sem: f(e).then_inc(s, 16))
        self._record(R, W, sem, val)

    def coll(self, fn, R=(), W=()):
        self._deps(R, W)
        sem = self.prog.new_sem()
        self.thunks.append(lambda e, f=fn, s=sem: f(e).then_inc(s))
        self._record(R, W, sem, 1)

    def wait_all(self, bufs):
        self._deps(bufs, ())


class Prog:
    def __init__(self, nc, es):
        self.nc = nc
        self.es = es
        self.es_sem = es
        self.nsem = 0
        self.V = Eng(self, "vector")
        self.A = Eng(self, "scalar")
        self.G = Eng(self, "gpsimd")
        self.P = Eng(self, "tensor", is_pe=True)
        self.S = Eng(self, "sync")

    def new_sem(self):
        self.nsem += 1
        return self.es_sem.enter_context(self.nc.semaphore("s%d" % self.nsem))

    def slot(self):
        return SemSlot(self.new_sem())

    def sb(self, name, shape, dt):
        return Tile(self.es.enter_context(self.nc.sbuf_tensor(name, shape, dt)), name)

    def ps(self, name, shape, dt):
        return Tile(self.es.enter_context(self.nc.psum_tensor(name, shape, dt)), name)

    def emit(self):
        with self.nc.Block() as block:
            @block.vector
            def _(e):
                for th in self.V.thunks:
                    th(e)

            @block.scalar
            def _(e):
                for th in self.A.thunks:
                    th(e)

            @block.gpsimd
            def _(e):
                for th in self.G.thunks:
                    th(e)

            @block.tensor
            def _(e):
                for th in self.P.thunks:
                    th(e)

            @block.sync
            def _(e):
                for th in self.S.thunks:
                    th(e)


def build(debug=False, phase2=True, do_coll=True, nsb_run=NSB, do_gla=True, do_att=True):
    nc = bass.Bass("TRN2", target_bir_lowering=False)
    dram = lambda n, s, dt, k="ExternalInput": nc.dram_tensor(n, s, dt, kind=k).ap()
    x = dram("x", [T, D], F32)
    xres = dram("xres", [2048, D], F32)
    w_in = dram("w_in", [D, 848], F32)
    g1 = dram("g1", [128, 8], F32)
    w2aug = dram("w2aug", [17, 64], F32)
    gng = dram("gng", [128, 512], F32)
    w_out = dram("w_out", [D, D], F32)
    g2r = dram("g2r", [128, D], F32)
    gfr = dram("gfr", [128, D], F32)
    wr = dram("wr", [D, 36], F32)
    brr = dram("brr", [128, 36], F32)
    NEd = NE if phase2 else 1
    w1 = dram("w1", [NEd, D, 512], F32)
    w3 = dram("w3", [NEd, D, 512], F32)
    w2 = dram("w2", [NEd, 512, D], F32)
    cf = dram("cf", [128, 544], F32)
    cb = dram("cb", [128, 640], BF16)
    out = dram("out", [2048, D], F32, "ExternalOutput")
    a_int = [dram("a_int%d" % s, [256, 2048], BF16, "Internal") for s in range(NSUP)]
    b_all = dram("b_all", [NSUP * 1024, 2048], BF16, "Internal")
    rowidx = dram("rowidx", [128, 8], I32)
    w1b = dram("w1b", [NEd, D, 512], BF16, "Internal")
    w3b = dram("w3b", [NEd, D, 512], BF16, "Internal")
    w2b = dram("w2b", [NEd, 512, D], BF16, "Internal")
    wb_buf = [Buf("wb%d" % e) for e in range(NE)]
    Xs = dram("xs_scr", [NE * CAP, D], BF16, "Internal")
    Ys = dram("ys_scr", [NE * CAP, D], F32, "Internal")
    if debug:
        dbg_o = dram("dbg_o", [NSUP * 1024, 2048], BF16, "ExternalOutput")
        dbg_h = dram("dbg_h", [2048, D], F32, "ExternalOutput")
    a_buf = [Buf("a%d" % s) for s in range(NSUP)]
    b_buf = [Buf("b%d" % s) for s in range(NSUP)]
    Xs_buf = Buf("Xs")
    Ys_buf = [Buf("Ys%d" % e) for e in range(NE)]
    out_buf = Buf("out")

    with ExitStack() as es0:
        pg = Prog(nc, es0)
        V, A, G, P, S = pg.V, pg.A, pg.G, pg.P, pg.S
        cslot = pg.slot()
        cslot_g = pg.slot()
        banks = [es0.enter_context(nc.psum_tensor("bank%d" % i, [128, 512], F32)) for i in range(8)]
        bank_buf = [Buf("bank%d" % i, psum=True) for i in range(8)]

        def carve(name, bank, c0, ncol, dt=F32, pat=None, parts=128, **kw):
            ap = banks[bank][0:parts, c0:c0 + ncol]
            if dt is not F32:
                ap = ap.bitcast(dt)
            if pat is not None:
                ap = ap.rearrange(pat, **kw)
            return Tile(ap, name, bank_buf[bank])

        cfT = pg.sb("cfT", [128, 544], F32)
        cbT = pg.sb("cbT", [128, 640], BF16)
        S.dma(lambda e: e.dma_start(out=cfT.t[:], in_=cf), cslot, W=[cfT])
        S.dma(lambda e: e.dma_start(out=cbT.t[:], in_=cb), cslot, W=[cbT])
        epsT = pg.sb("epsT", [128, 2], F32)
        V.op(lambda e: e.memset(epsT.t[:, 0:1], EPS), W=[epsT])
        V.op(lambda e: e.memset(epsT.t[:, 1:2], 64 * EPS), W=[epsT])
        identF = cfT.t[:, 0:128]
        triU = cfT.t[:, 128:256]
        strictT = cfT.t[:, 256:384]
        onesF = cfT.t[:, 384:512]
        ecol = cfT.t[:, 512:544]
        identB = cbT.t[:, 0:128]
        mask4 = cbT.t[:, 128:640]

        with ExitStack() as es1:
            pg.es = es1
            NX = 6
            Wt = pg.sb("Wt", [128, 8, 848], BF16)
            g1T = pg.sb("g1T", [128, 8], F32)
            w2a = pg.sb("w2a", [17, 64], F32)
            gngT = pg.sb("gngT", [128, 4, 128], F32)
            xt = [pg.sb("xt%d" % i, [128, D], F32) for i in range(NX)]
            xsl = [pg.slot() for _ in range(NX)]
            junk = pg.sb("junk", [128, D], BF16)
            ss1 = pg.sb("ss1", [128, 64], F32)
            rs1 = pg.sb("rs1", [128, 64], F32)
            xs = [pg.sb("xs%d" % i, [128, D], BF16) for i in range(4)]
            nT = [pg.sb("nT%d" % i, [128, 8, 512], BF16) for i in range(2)]
            qT = [pg.sb("qT%d" % i, [64, 512], BF16) for i in range(2)]
            kT = [pg.sb("kT%d" % i, [64, 512], BF16) for i in range(2)]
            QT = [[pg.sb("QT%d_%d" % (h, i), [64, 2048], BF16) for i in range(2)] for h in range(2)]
            KT = [[pg.sb("KT%d_%d" % (h, i), [64, 2048], BF16) for i in range(3)] for h in range(2)]
            VT = [[pg.sb("VT%d_%d" % (h, i), [64, 2048], BF16) for i in range(3)] for h in range(2)]
            tm = [pg.sb("tm%d" % i, [128, 4, 320], BF16) for i in range(2)]
            glrT = [pg.sb("glrT%d" % i, [32, 512], F32) for i in range(2)]
            e1 = pg.sb("e1", [128, 256], F32)
            sp = pg.sb("sp", [128, 256], F32)
            Ek = pg.sb("Ek", [128, 256], F32)
            ktl = pg.sb("ktl", [128, 4, 64], BF16)
            EqT = pg.sb("EqT", [64, 512], F32)
            EkT = pg.sb("EkT", [64, 512], F32)
            qtl = pg.sb("qtl", [64, 512], BF16)
            ktlT = pg.sb("ktlT", [64, 512], BF16)
            gg = pg.sb("gg", [128, 4, 128], F32)
            AmT = [pg.sb("AmT%d" % i, [128, 128], BF16) for i in range(4)]
            og = [pg.sb("og%d" % i, [128, 128], BF16) for i in range(4)]
            Sst = pg.sb("Sst", [64, 128], F32)
            Sbf = pg.sb("Sbf", [64, 128], BF16)
            Stmp = pg.sb("Stmp", [64, 128], F32)
            ssg = pg.sb("ssg", [128, 64], F32)
            rsg = pg.sb("rsg", [128, 64], F32)
            junk2 = pg.sb("junk2", [128, 128], BF16)
            ogT = [pg.sb("ogT%d" % i, [128, 2048], BF16) for i in range(2)]
            pTa = [pg.sb("pTa%d" % i, [128, 512], BF16) for i in range(3)]
            vp = [pg.sb("vp%d" % i, [128, 4, 128], BF16) for i in range(3)]
            Oacc = pg.sb("Oacc", [128, 2, 2048], F32)
            rden = pg.sb("rden", [64, 1024], F32)
            oTa = [pg.sb("oTa%d" % i, [128, 2048], BF16) for i in range(2)]
            stsl = [pg.slot() for _ in range(2)]
            ps_pT_l = [carve("ps_pT", 0, 0, 512, BF16, "p (a b) -> p a b", a=8), carve("ps_pT2", 4, 0, 512, BF16, "p (a b) -> p a b", a=8)]
            ps_pr = [carve("ps_pr%d" % i, 1 + i, 0, 512) for i in range(2)]
            ps_GT = carve("ps_GT", 3, 0, 512, parts=64)
            ps_s = carve("ps_s", 4, 0, 512)
            ps_z = carve("ps_z", 5, 0, 256)
            ps_G = ps_z
            ps_U = Tile(banks[5][0:64, 256:384], "ps_U", bank_buf[5])
            ps_ogT_l = [Tile(banks[5][:, 384:448].bitcast(BF16), "ps_ogT0", bank_buf[5]),
                        Tile(banks[5][:, 448:512].bitcast(BF16), "ps_ogT1", bank_buf[5])]
            ps_A4 = carve("ps_A4", 6, 0, 512, F32, "p (a b) -> p a b", a=4)
            ps_og4 = carve("ps_og4", 7, 0, 512, F32, "p (a b) -> p a b", a=4)
            ps_o = Tile(banks[6][:, 0:256].rearrange("p (a b) -> p a b", a=2), "ps_o", bank_buf[6])
            ps_v = Tile(banks[7][:, 128:256].bitcast(BF16).rearrange("p (a b) -> p a b", a=4), "ps_v", bank_buf[7])
            ps_s_l = [ps_s, Tile(banks[5][:, :], "ps_s2", bank_buf[5]), Tile(banks[3][:, :], "ps_s3", bank_buf[3])]
            ps_o_l = [ps_o, Tile(banks[0][:, 0:256].rearrange("p (a b) -> p a b", a=2), "ps_o2", bank_buf[0])]
            ps_v_l = [ps_v, Tile(banks[1][:, 128:256].bitcast(BF16).rearrange("p (a b) -> p a b", a=4), "ps_v2", bank_buf[1])]

            for kc in range(8):
                G.dma(lambda e, kc=kc: e.dma_start(out=Wt.t[:, kc, :], in_=w_in[kc * 128:(kc + 1) * 128, :]),
                      cslot_g, W=[Wt])
            S.dma(lambda e: e.dma_start(out=g1T.t[:], in_=g1), cslot, W=[g1T])
            S.dma(lambda e: e.dma_start(out=w2a.t[:], in_=w2aug), cslot, W=[w2a])
            S.dma(lambda e: e.dma_start(out=gngT.t[:].rearrange("p a b -> p (a b)"), in_=gng), cslot, W=[gngT])
            for kc in range(8):
                V.op(lambda e, kc=kc: e.tensor_scalar(out=Wt.t[:, kc, :], in0=Wt.t[:, kc, :], scalar1=g1T.t[:, kc:kc + 1],
                                                      scalar2=None, op0=ALU.mult), R=[g1T, Wt], W=[Wt])
            V.op(lambda e: e.memset(ss1.t[:], 0.0), W=[ss1])
            V.op(lambda e: e.memset(ssg.t[:], 0.0), W=[ssg])
            V.op(lambda e: e.memset(Sst.t[:], 0.0), W=[Sst])
            V.op(lambda e: e.memset(Sbf.t[:], 0.0), W=[Sbf])
            for i in range(2):
                G.op(lambda e, i=i: e.memset(glrT[i].t[:], 1.0), W=[glrT[i]])
            for i in range(3):
                G.op(lambda e, i=i: e.memset(vp[i].t[:], 1.0), W=[vp[i]])

            pr_i = [0]

            def next_pr():
                pr_i[0] ^= 1
                return ps_pr[pr_i[0]]

            def stage1(sb):
                par = sb % 2
                sup = sb // 4
                for i in range(4):
                    ti = sb * 4 + i
                    sl = ti % NX
                    S.dma(lambda e, ti=ti, sl=sl: e.dma_start(out=xt[sl].t[:], in_=x[ti * 128:(ti + 1) * 128, :]),
                          xsl[sl], W=[xt[sl]])
                    A.op(lambda e, ti=ti, sl=sl: e.activation(out=junk.t[:], in_=xt[sl].t[:], func=AF.Square,
                                                              accum_out=ss1.t[:, ti:ti + 1]),
                         R=[xt[sl]], W=[junk, ss1])
                t0i = sb * 4
                A.op(lambda e: e.activation(out=rs1.t[:, t0i:t0i + 4], in_=ss1.t[:, t0i:t0i + 4], func=AF.Sqrt, scale=1.0 / D, bias=epsT.t[:, 0:1]),
                     R=[ss1, epsT], W=[rs1])
                V.op(lambda e: e.reciprocal(out=rs1.t[:, t0i:t0i + 4], in_=rs1.t[:, t0i:t0i + 4]), R=[rs1], W=[rs1])
                for i in range(4):
                    ti = sb * 4 + i
                    sl = ti % NX
                    xp = ti % 4
                    V.op(lambda e, ti=ti, sl=sl, xp=xp: e.tensor_scalar(out=xs[xp].t[:], in0=xt[sl].t[:],
                                                                        scalar1=rs1.t[:, ti:ti + 1], scalar2=None, op0=ALU.mult),
                         R=[xt[sl], rs1], W=[xs[xp]])
                yield "front_a"
                for i in range(4):
                    ti = sb * 4 + i
                    xp = ti % 4
                    ps_pT = ps_pT_l[ti % 2]
                    for kc in range(8):
                        P.op(lambda e, kc=kc, xp=xp, ps_pT=ps_pT: e.transpose(out=ps_pT.t[:, kc, :], in_=xs[xp].t[:, kc * 128:(kc + 1) * 128],
                                                                 identity=identB), R=[xs[xp], cbT], W=[ps_pT])
                    A.op(lambda e, i=i, par=par, ps_pT=ps_pT: e.activation(out=nT[par].t[:, :, i * 128:(i + 1) * 128], in_=ps_pT.t[:, :, :],
                                                              func=AF.Copy), R=[ps_pT], W=[nT[par]])
                yield "front_b"
                col = (sb % 4) * 512
                kslot = sup % 3
                fm = [
                    (0, 128, [(qT[par], qT[par].t[0:64, :]), (kT[par], kT[par].t[0:64, :])]),
                    (128, 128, [(QT[0][sup % 2], QT[0][sup % 2].t[:, col:col + 512]), (QT[1][sup % 2], QT[1][sup % 2].t[:, col:col + 512])]),
                    (256, 128, [(KT[0][kslot], KT[0][kslot].t[:, col:col + 512]), (KT[1][kslot], KT[1][kslot].t[:, col:col + 512])]),
                    (384, 128, [(VT[0][kslot], VT[0][kslot].t[:, col:col + 512]), (VT[1][kslot], VT[1][kslot].t[:, col:col + 512])]),
                    (512, 16, [(glrT[par], glrT[par].t[0:16, :])]),
                ]
                for gi, (c0, m, dsts) in enumerate(fm):
                    pp = next_pr()
                    for kc in range(8):
                        P.op(lambda e, kc=kc, c0=c0, m=m, pp=pp, par=par: e.matmul(
                            out=pp.t[0:m, :], lhsT=Wt.t[:, kc, c0:c0 + m], rhs=nT[par].t[:, kc, :],
                            start=(kc == 0), stop=(kc == 7)), R=[Wt, nT[par]], W=[pp])
                    if len(dsts) == 2:
                        (t0_, d0_), (t1_, d1_) = dsts
                        A.op(lambda e, pp=pp, d0_=d0_: e.activation(out=d0_, in_=pp.t[0:64, :], func=AF.Copy), R=[pp], W=[t0_])
                        V.op(lambda e, pp=pp, d1_=d1_: e.tensor_copy(out=d1_, in_=pp.t[64:128, :]), R=[pp], W=[t1_])
                    else:
                        (t0_, d0_), = dsts
                        V.op(lambda e, pp=pp, m=m, d0_=d0_: e.tensor_copy(out=d0_, in_=pp.t[0:m, :]), R=[pp], W=[t0_])
                    if gi == 1 or gi == 4:
                        yield "fm"
                for i in range(4):
                    pp = next_pr()
                    for kc in range(8):
                        P.op(lambda e, kc=kc, i=i, pp=pp, par=par: e.matmul(
                            out=pp.t[:, 0:320], lhsT=nT[par].t[:, kc, i * 128:(i + 1) * 128], rhs=Wt.t[:, kc, 528:848],
                            start=(kc == 0), stop=(kc == 7)), R=[Wt, nT[par]], W=[pp])
                    V.op(lambda e, pp=pp, i=i, par=par: e.tensor_copy(out=tm[par].t[:, i, 0:192], in_=pp.t[:, 0:192]),
                         R=[pp], W=[tm[par]])
                    A.op(lambda e, pp=pp, i=i, par=par: e.activation(out=tm[par].t[:, i, 192:320], in_=pp.t[:, 192:320],
                                                                     func=AF.Silu), R=[pp], W=[tm[par]])
                    if i == 1 or i == 3:
                        yield "tm"

            def gla(sb):
                par = sb % 2
                sup = sb // 4
                for i in range(4):
                    P.op(lambda e, i=i, par=par: e.matmul(out=ps_z.t[:, i * 64:(i + 1) * 64], lhsT=glrT[par].t[0:17, i * 128:(i + 1) * 128],
                                                          rhs=w2a.t[0:17, :], start=True, stop=True),
                         R=[glrT[par], w2a], W=[ps_z])
                A.op(lambda e: e.activation(out=e1.t[:], in_=ps_z.t[:], func=AF.Exp, scale=-1.0), R=[ps_z], W=[e1])
                A.op(lambda e: e.activation(out=sp.t[:], in_=e1.t[:], func=AF.Ln, bias=1.0), R=[e1], W=[sp])
                yield "p1"
                P.op(lambda e: e.matmul(out=ps_G.t[:], lhsT=triU, rhs=sp.t[:], start=True, stop=True), R=[sp, cfT], W=[ps_G])
                for i in range(4):
                    P.op(lambda e, i=i: e.matmul(out=ps_GT.t[:, i * 128:(i + 1) * 128], lhsT=sp.t[:, i * 64:(i + 1) * 64], rhs=triU,
                                                 start=True, stop=True), R=[sp, cfT], W=[ps_GT])
                A.op(lambda e: e.activation(out=Ek.t[:], in_=ps_G.t[:], func=AF.Exp, scale=1.0 / 16), R=[ps_G], W=[Ek])
                A.op(lambda e: e.activation(out=EqT.t[:], in_=ps_GT.t[:], func=AF.Exp, scale=-1.0 / 16), R=[ps_GT], W=[EqT])
                A.op(lambda e: e.activation(out=EkT.t[:], in_=ps_GT.t[:], func=AF.Exp, scale=1.0 / 16), R=[ps_GT], W=[EkT])
                V.op(lambda e, par=par: e.tensor_tensor(out=ktl.t[:, :, :], in0=tm[par].t[:, :, 0:64],
                                                        in1=Ek.t[:].rearrange("p (a b) -> p a b", a=4), op=ALU.mult),
                     R=[tm[par], Ek], W=[ktl])
                V.op(lambda e, par=par: e.tensor_tensor(out=qtl.t[:], in0=qT[par].t[:], in1=EqT.t[:], op=ALU.mult),
                     R=[qT[par], EqT], W=[qtl])
                V.op(lambda e, par=par: e.tensor_tensor(out=ktlT.t[:], in0=kT[par].t[:], in1=EkT.t[:], op=ALU.mult),
                     R=[kT[par], EkT], W=[ktlT])
                V.op(lambda e, par=par: e.tensor_tensor(out=gg.t[:], in0=tm[par].t[:, :, 192:320], in1=gngT.t[:], op=ALU.mult),
                     R=[tm[par], gngT], W=[gg])
                yield "p2"
                ch0 = sb * 4
                css = [slice(i * 128, (i + 1) * 128) for i in range(4)]
                vaps = [tm[par].t[:, i, 64:192] for i in range(4)]
                for i in range(4):
                    P.op(lambda e, i=i: e.matmul(out=ps_A4.t[:, i, :], lhsT=ktlT.t[:, css[i]], rhs=qtl.t[:, css[i]], start=True, stop=True),
                         R=[ktlT, qtl], W=[ps_A4])
                for i in range(4):
                    V.op(lambda e, i=i: e.tensor_tensor(out=AmT[i].t[:], in0=ps_A4.t[:, i, :], in1=triU, op=ALU.mult),
                         R=[ps_A4, cfT], W=[AmT[i]])
                yield "a"
                for i in range(4):
                    P.op(lambda e, i=i: e.matmul(out=ps_U.t[:], lhsT=ktl.t[:, i, :], rhs=vaps[i], start=True, stop=True),
                         R=[ktl, tm[par]], W=[ps_U])
                    P.op(lambda e, i=i: e.matmul(out=ps_og4.t[:, i, :], lhsT=AmT[i].t[:], rhs=vaps[i], start=True, stop=False),
                         R=[AmT[i], tm[par]], W=[ps_og4])
                    P.op(lambda e, i=i: e.matmul(out=ps_og4.t[:, i, :], lhsT=qtl.t[:, css[i]], rhs=Sbf.t[:], start=False, stop=True),
                         R=[qtl, Sbf], W=[ps_og4])
                    acol = EqT.t[:, i * 128 + 127:i * 128 + 128]
                    V.op(lambda e: e.tensor_tensor(out=Stmp.t[:], in0=ps_U.t[:], in1=Sst.t[:], op=ALU.add),
                         R=[ps_U, Sst], W=[Stmp])
                    A.op(lambda e, acol=acol: e.activation(out=Sbf.t[:], in_=Stmp.t[:], func=AF.Copy, scale=acol),
                         R=[Stmp, EqT], W=[Sbf])
                    V.op(lambda e, acol=acol: e.tensor_scalar(out=Sst.t[:], in0=Stmp.t[:], scalar1=acol, scalar2=None, op0=ALU.mult),
                         R=[Stmp, EqT], W=[Sst])
                yield "b"
                for i in range(4):
                    A.op(lambda e, i=i: e.activation(out=junk2.t[:], in_=ps_og4.t[:, i, :], func=AF.Square, accum_out=ssg.t[:, ch0 + i:ch0 + i + 1]),
                         R=[ps_og4], W=[junk2, ssg])
                A.op(lambda e: e.activation(out=rsg.t[:, ch0:ch0 + 4], in_=ssg.t[:, ch0:ch0 + 4], func=AF.Sqrt, scale=1.0 / 128, bias=epsT.t[:, 1:2]),
                     R=[ssg, epsT], W=[rsg])
                V.op(lambda e: e.reciprocal(out=rsg.t[:, ch0:ch0 + 4], in_=rsg.t[:, ch0:ch0 + 4]), R=[rsg], W=[rsg])
                for i in range(4):
                    V.op(lambda e, i=i: e.scalar_tensor_tensor(out=og[i].t[:], in0=ps_og4.t[:, i, :], scalar=rsg.t[:, ch0 + i:ch0 + i + 1],
                                                               in1=gg.t[:, i, :], op0=ALU.mult, op1=ALU.mult),
                         R=[ps_og4, rsg, gg], W=[og[i]])
                yield "c"
                for i in range(4):
                    pt = ps_ogT_l[i % 2]
                    P.op(lambda e, i=i, pt=pt: e.transpose(out=pt.t[:], in_=og[i].t[:], identity=identB), R=[og[i], cbT], W=[pt])
                    cc = ((ch0 + i) % 16) * 128
                    A.op(lambda e, cc=cc, sup=sup, pt=pt: e.activation(out=ogT[sup % 2].t[:, cc:cc + 128], in_=pt.t[:], func=AF.Copy),
                         R=[pt], W=[ogT[sup % 2]])

            def attention(sup):
                qp = sup % 2
                units = []
                for d in ATT_DS:
                    for r in range(d):
                        for n in range(16 // d):
                            units.append((d, r, n))
                ui = [0]

                def kcols(t0, d):
                    slot = (t0 // 2048) % 3
                    c0 = t0 % 2048
                    return slot, slice(c0, c0 + 127 * d + 1, d)

                def scores(u):
                    d, r, n = units[u]
                    sl3 = u % 3
                    qb = r + d * 128 * n
                    qsl = slice(qb, qb + 127 * d + 1, d)
                    t0 = sup * 2048 + qb
                    has_prev = (t0 - 128 * d) >= 0
                    cur = kcols(t0, d)
                    prev = kcols(t0 - 128 * d, d) if has_prev else cur
                    pss = ps_s_l[u % 3]
                    psv = ps_v_l[u % 2]
                    for h in range(2):
                        for blk, (ks, kc) in enumerate((prev, cur)):
                            P.op(lambda e, h=h, blk=blk, ks=ks, kc=kc, qsl=qsl, pss=pss: e.matmul(
                                out=pss.t[:, (h * 2 + blk) * 128:(h * 2 + blk + 1) * 128], lhsT=KT[h][ks].t[:, kc],
                                rhs=QT[h][qp].t[:, qsl], start=True, stop=True), R=[KT[h][ks], QT[h][qp]], W=[pss])
                    for h in range(2):
                        for blk, (ks, kc) in enumerate((prev, cur)):
                            P.op(lambda e, h=h, blk=blk, ks=ks, kc=kc, psv=psv: e.transpose(
                                out=psv.t[:, h * 2 + blk, :], in_=VT[h][ks].t[:, kc], identity=cbT.t[0:64, 0:64]),
                                R=[VT[h][ks], cbT], W=[psv])
                    A.op(lambda e, sl3=sl3, pss=pss: e.activation(out=pTa[sl3].t[:], in_=pss.t[:], func=AF.Exp, scale=0.125),
                         R=[pss], W=[pTa[sl3]])
                    V.op(lambda e, sl3=sl3, psv=psv: e.tensor_copy(out=vp[sl3].t[:, :, 0:64], in_=psv.t[:, :, :]), R=[psv], W=[vp[sl3]])
                    V.op(lambda e, sl3=sl3: e.tensor_tensor(out=pTa[sl3].t[:], in0=pTa[sl3].t[:], in1=mask4, op=ALU.mult),
                         R=[pTa[sl3], cbT], W=[pTa[sl3]])
                    return has_prev

                def pv(u, has_prev):
                    d, r, n = units[u]
                    sl3 = u % 3
                    qb = r + d * 128 * n
                    qsl = slice(qb, qb + 127 * d + 1, d)
                    blks = (0, 1) if has_prev else (1,)
                    ps_o = ps_o_l[u % 2]
                    for h in range(2):
                        for bi, blk in enumerate(blks):
                            P.op(lambda e, h=h, blk=blk, bi=bi, sl3=sl3, nb=len(blks), ps_o=ps_o: e.matmul(
                                out=ps_o.t[:, h, :], lhsT=vp[sl3].t[:, h * 2 + blk, :],
                                rhs=pTa[sl3].t[:, (h * 2 + blk) * 128:(h * 2 + blk + 1) * 128],
                                start=(bi == 0), stop=(bi == nb - 1)), R=[vp[sl3], pTa[sl3]], W=[ps_o])
                    if d == 1:
                        V.op(lambda e, qsl=qsl, ps_o=ps_o: e.tensor_copy(out=Oacc.t[:, :, qsl], in_=ps_o.t[:, :, :]), R=[ps_o], W=[Oacc])
                    else:
                        V.op(lambda e, qsl=qsl, ps_o=ps_o: e.tensor_tensor(out=Oacc.t[:, :, qsl], in0=ps_o.t[:, :, :], in1=Oacc.t[:, :, qsl],
                                                                op=ALU.add), R=[ps_o, Oacc], W=[Oacc])

                hp = {}
                for u in range(len(units) + 1):
                    if u < len(units):
                        hp[u] = scores(u)
                    if u >= 1 and ATT_MODE >= 2:
                        pv(u - 1, hp[u - 1])
                for h in range(2 if ATT_MODE >= 3 else 0):
                    for hf in range(2):
                        cs_ = slice(hf * 1024, (hf + 1) * 1024)
                        V.op(lambda e, h=h, cs_=cs_: e.reciprocal(out=rden.t[:], in_=Oacc.t[64:128, h, cs_]), R=[Oacc], W=[rden])
                        V.op(lambda e, h=h, cs_=cs_: e.tensor_tensor(out=oTa[qp].t[h * 64:(h + 1) * 64, cs_], in0=Oacc.t[0:64, h, cs_], in1=rden.t[:],
                                                                     op=ALU.mult), R=[Oacc, rden], W=[oTa[qp]])

            def exchange(sup):
                qp = sup % 2
                S.dma(lambda e: e.dma_start(out=a_int[sup][0:128, :], in_=ogT[qp].t[:]), stsl[qp], R=[ogT[qp]], W=[a_buf[sup]])
                S.dma(lambda e: e.dma_start(out=a_int[sup][128:256, :], in_=oTa[qp].t[:]), stsl[qp], R=[oTa[qp]], W=[a_buf[sup]])
                if do_coll:
                  G.coll(lambda e: e.collective_compute("AllGather", ALU.bypass, replica_groups=[[0, 1, 2, 3], [4, 5, 6, 7]],
                                                      ins=[a_int[sup]], outs=[b_all[sup * 1024:(sup + 1) * 1024, :]]), R=[a_buf[sup]], W=[b_buf[sup]])

            wcs = [pg.slot() for _ in range(4)]

            def precast(ex):
                for (src, dst) in ((w1, w1b), (w3, w3b), (w2, w2b)):
                    G.dma(lambda e, src=src, dst=dst, ex=ex: e.dma_start(
                        out=dst[ex].rearrange("(p r) f -> p (r f)", p=128), in_=src[ex].rearrange("(p r) f -> p (r f)", p=128)),
                        wcs[ex % 4], W=[wb_buf[ex]])

            gen1 = {}
            gen2 = {}

            def adv(gd, k):
                if k in gd:
                    if next(gd[k], None) is None:
                        del gd[k]

            for it in range(nsb_run + 2):
                if phase2 and it < NSB:
                    precast(it)
                if it < nsb_run:
                    gen1[it] = stage1(it)
                if it >= 2 and do_gla:
                    gen2[it - 2] = gla(it - 2)
                adv(gen1, it)
                adv(gen2, it - 2)
                adv(gen1, it - 1)
                adv(gen2, it - 2)
                adv(gen1, it - 1)
                adv(gen2, it - 2)
                adv(gen1, it - 1)
                adv(gen2, it - 2)
                adv(gen1, it - 1)
                adv(gen2, it - 2)
                adv(gen1, it)
                adv(gen2, it - 2)
                adv(gen1, it - 1)
                adv(gen2, it - 2)
                sbd = it - 2
                if sbd >= 0 and sbd % 4 == 3:
                    if phase2:
                        precast(NSB + sbd // 4)
                    if do_att:
                        attention(sbd // 4)
                    if do_gla and do_att:
                        exchange(sbd // 4)
            assert not gen1 and not gen2, (list(gen1), list(gen2))
            if debug and not (do_gla and do_att):
                dbg1 = Buf("dbg1")
                S.dma(lambda e: e.dma_start(out=dbg_o[0:64, :], in_=QT[0][0].t[:]), stsl[0], R=[QT[0][0]], W=[dbg1])
                S.dma(lambda e: e.dma_start(out=dbg_o[128:192, :], in_=KT[0][0].t[:]), stsl[0], R=[KT[0][0]], W=[dbg1])
                S.dma(lambda e: e.dma_start(out=dbg_o[256:384, :], in_=ogT[0].t[:]), stsl[0], R=[ogT[0]], W=[dbg1])
                S.dma(lambda e: e.dma_start(out=dbg_o[384:512, :], in_=oTa[0].t[:]), stsl[0], R=[oTa[0]], W=[dbg1])
                S.wait_all([dbg1])
            pg.es = es0
        dslot = pg.slot()
        if debug and do_coll:
            S.dma(lambda e: e.dma_start(out=dbg_o, in_=b_all), dslot, R=b_buf, W=[out_buf])
        if debug and not do_coll and do_gla and do_att:
            for sup in range(nsb_run // 4):
                S.dma(lambda e, sup=sup: e.dma_start(out=dbg_o[sup * 256:(sup + 1) * 256, :], in_=a_int[sup]), dslot, R=[a_buf[sup]], W=[out_buf])
        if phase2:
          with ExitStack() as es2:
            pg.es = es2
            hh = pg.sb("hh", [128, 16, D], F32)
            gfT = pg.sb("gfT", [128, D], F32)
            ss3 = pg.sb("ss3", [128, 16], F32)
            rs3 = pg.sb("rs3", [128, 16], F32)
            gates = pg.sb("gates", [128, 16, 2], F32)
            dstf = pg.sb("dstf", [128, 16, 2], F32)
            dsti = pg.sb("dsti", [128, 16, 2], I32)

            es2a = ExitStack()
            pg.es = es2a
            ridx = pg.sb("ridx", [128, 8], I32)
            oT = pg.sb("oT", [128, 8, 2048], BF16)
            Wo = pg.sb("Wo", [128, 8, D], BF16)
            g2T = pg.sb("g2T", [128, D], F32)
            wrT = pg.sb("wrT", [128, 8, 36], F32)
            brT = pg.sb("brT", [128, 36], F32)
            xr = [pg.sb("xr%d" % i, [128, D], F32) for i in range(2)]
            xrs = [pg.slot() for _ in range(2)]
            junk3 = pg.sb("junk3", [128, D], BF16)
            ss2 = pg.sb("ss2", [128, 16], F32)
            rs2 = pg.sb("rs2", [128, 16], F32)
            n2f = [pg.sb("n2f%d" % i, [128, D], F32) for i in range(2)]
            n2b = [pg.sb("n2b%d" % i, [128, D], BF16) for i in range(2)]
            n2s = [pg.slot() for _ in range(2)]
            n2Ts = [pg.sb("n2T_%d" % i, [128, 8, 128], F32) for i in range(2)]
            lgs = [pg.sb("lg_%d" % i, [128, 36], F32) for i in range(2)]
            sms = [pg.sb("sm_%d" % i, [128, 16], F32) for i in range(2)]
            gmasks = [pg.sb("gmask_%d" % i, [128, 4], F32) for i in range(2)]
            pens = [pg.sb("pen_%d" % i, [128, 4], F32) for i in range(2)]
            gexs = [pg.sb("gex_%d" % i, [128, 4], F32) for i in range(2)]
            elms = [pg.sb("elm_%d" % i, [128, 32], F32) for i in range(2)]
            elm2s = [pg.sb("elm2_%d" % i, [128, 32], F32) for i in range(2)]
            mk1s = [pg.sb("mk1_%d" % i, [128, 32], F32) for i in range(2)]
            mk2s = [pg.sb("mk2_%d" % i, [128, 32], F32) for i in range(2)]
            cnts = [pg.sb("cnt_%d" % i, [128, 32], F32) for i in range(2)]
            tot = pg.sb("tot", [128, 32], F32)
            poss = [pg.sb("pos_%d" % i, [128, 32], F32) for i in range(2)]
            tmp32s = [pg.sb("tmp32_%d" % i, [128, 32], F32) for i in range(2)]
            ps_h = [carve("ps_h%d" % i, i, 0, 512) for i in range(2)]
            ps_t = [carve("ps_t%d" % i, 2 + i, 0, 512, F32, "p (a b) -> p a b", a=4) for i in range(2)]
            ps_y = [carve("ps_y%d" % i, 4 + i, 0, 512) for i in range(2)]
            ps_a2 = [Tile(banks[0][:, 0:256], "ps_a0", bank_buf[0]), Tile(banks[2][:, 0:256], "ps_a1", bank_buf[2])]
            ps_b2 = [Tile(banks[1][:, 0:256], "ps_b0", bank_buf[1]), Tile(banks[3][:, 0:256], "ps_b1", bank_buf[3])]
            ps_x = Tile(banks[7][:, 0:256].bitcast(BF16).rearrange("p (a b) -> p a b", a=2), "ps_x", bank_buf[7])
            ps_l = Tile(banks[7][:, 256:292], "ps_l", bank_buf[7])
            ps_r = Tile(banks[7][:, 320:352], "ps_r", bank_buf[7])
            ps_c = Tile(banks[7][:, 352:384], "ps_c", bank_buf[7])

            S.dma(lambda e: e.dma_start(out=ridx.t[:], in_=rowidx), cslot, W=[ridx])
            S.dma(lambda e: e.dma_start(out=g2T.t[:], in_=g2r), cslot, W=[g2T])
            S.dma(lambda e: e.dma_start(out=gfT.t[:], in_=gfr), cslot, W=[gfT])
            S.dma(lambda e: e.dma_start(out=brT.t[:], in_=brr), cslot, W=[brT])
            S.dma(lambda e: e.dma_start(out=wrT.t[:], in_=wr.rearrange("(c p) n -> p c n", p=128)), cslot, W=[wrT])
            for kc in range(8):
                G.dma(lambda e, kc=kc: e.dma_start(out=Wo.t[:, kc, :], in_=w_out[kc * 128:(kc + 1) * 128, :]), cslot_g, W=[Wo])
            V.op(lambda e: e.memset(ss2.t[:], 0.0), W=[ss2])
            V.op(lambda e: e.memset(ss3.t[:], 0.0), W=[ss3])
            V.op(lambda e: e.memset(tot.t[:], 0.0), W=[tot])
            for sm_ in sms:
                V.op(lambda e, sm_=sm_: e.memset(sm_.t[:], 0.0), W=[sm_])
            oslot = pg.slot()
            for c in range(8):
                G.dma(lambda e, c=c: e.indirect_dma_start(out=oT.t[:, c, :], out_offset=None, in_=b_all,
                                                          in_offset=bass.IndirectOffsetOnAxis(ap=ridx.t[:, c:c + 1], axis=0)),
                      oslot, R=b_buf + [ridx], W=[oT])
            wch = [(c // 2) + 4 * (c % 2) for c in range(8)]

            hhb = [Buf("hh%d" % i) for i in range(16)]
            gtb = [Buf("gt%d" % i) for i in range(16)]
            dfb = [Buf("df%d" % i) for i in range(16)]
            dib = [Buf("di%d" % i) for i in range(16)]

            def rstd_ops(ssT, rsT, ti, n, eps):
                A.op(lambda e: e.activation(out=rsT.t[:, ti:ti + 1], in_=ssT.t[:, ti:ti + 1], func=AF.Sqrt, scale=1.0 / n, bias=epsT.t[:, 0:1]),
                     R=[ssT, epsT], W=[rsT])
                V.op(lambda e: e.reciprocal(out=rsT.t[:, ti:ti + 1], in_=rsT.t[:, ti:ti + 1]), R=[rsT], W=[rsT])

            def route(ti):
                p2 = ti % 2
                n2T = n2Ts[p2]
                lg = lgs[p2]
                sm = sms[p2]
                gmask = gmasks[p2]
                pen = pens[p2]
                gex = gexs[p2]
                elm = elms[p2]
                elm2 = elm2s[p2]
                mk1 = mk1s[p2]
                mk2 = mk2s[p2]
                cnt = cnts[p2]
                pos = poss[p2]
                tmp32 = tmp32s[p2]
                tsl = slice(ti * 128, (ti + 1) * 128)
                if NSB + 4 + ti < NE:
                    precast(NSB + 4 + ti)
                S.dma(lambda e, tsl=tsl, p2=p2: e.dma_start(out=xr[p2].t[:], in_=xres[tsl, :]), xrs[p2], W=[xr[p2]])
                for half in range(2):
                    for c in range(8):
                        P.op(lambda e, c=c, half=half, tsl=tsl: e.matmul(
                            out=ps_h[half].t[:], lhsT=oT.t[:, c, tsl], rhs=Wo.t[:, wch[c], half * 512:(half + 1) * 512],
                            start=(c == 0), stop=(c == 7)), R=[oT, Wo], W=[ps_h[half]])
                    V.op(lambda e, half=half, ti=ti, p2=p2: e.tensor_tensor(
                        out=hh.t[:, ti, half * 512:(half + 1) * 512], in0=ps_h[half].t[:], in1=xr[p2].t[:, half * 512:(half + 1) * 512],
                        op=ALU.add), R=[ps_h[half], xr[p2]], W=[hhb[ti]])
                    yield
                A.op(lambda e, ti=ti: e.activation(out=junk3.t[:], in_=hh.t[:, ti, :], func=AF.Square, accum_out=ss2.t[:, ti:ti + 1]),
                     R=[hhb[ti]], W=[junk3, ss2])
                yield
                rstd_ops(ss2, rs2, ti, D, EPS)
                V.op(lambda e, ti=ti, p2=p2: e.scalar_tensor_tensor(out=n2f[p2].t[:], in0=hh.t[:, ti, :], scalar=rs2.t[:, ti:ti + 1],
                                                                    in1=g2T.t[:], op0=ALU.mult, op1=ALU.mult),
                     R=[hhb[ti], rs2, g2T], W=[n2f[p2]])
                yield
                A.op(lambda e, p2=p2: e.activation(out=n2b[p2].t[:], in_=n2f[p2].t[:], func=AF.Copy), R=[n2f[p2]], W=[n2b[p2]])
                yield
                for q in range(2):
                    for c4 in range(4):
                        c = q * 4 + c4
                        P.op(lambda e, c=c, c4=c4, q=q, p2=p2: e.transpose(out=ps_t[q].t[:, c4, :], in_=n2f[p2].t[:, c * 128:(c + 1) * 128],
                                                                           identity=identF), R=[n2f[p2], cfT], W=[ps_t[q]])
                    if q == 0:
                        A.op(lambda e: e.activation(out=n2T.t[:, 0:4, :], in_=ps_t[0].t[:], func=AF.Copy), R=[ps_t[0]], W=[n2T])
                        yield
                    else:
                        V.op(lambda e: e.tensor_copy(out=n2T.t[:, 4:8, :], in_=ps_t[1].t[:]), R=[ps_t[1]], W=[n2T])
                        yield
                for c in range(8):
                    P.op(lambda e, c=c: e.matmul(out=ps_l.t[:], lhsT=n2T.t[:, c, :], rhs=wrT.t[:, c, :], start=(c == 0), stop=(c == 7)),
                         R=[n2T, wrT], W=[ps_l])
                V.op(lambda e: e.tensor_tensor(out=lg.t[:], in0=ps_l.t[:], in1=brT.t[:], op=ALU.add), R=[ps_l, brT], W=[lg])
                yield
                gl = lg.t[:, 0:4]
                el = lg.t[:, 4:36]
                c_ = lambda k: sm.t[:, k:k + 1]
                V.op(lambda e: e.reduce_max(out=c_(0), in_=gl, axis=AX.X), R=[lg], W=[sm])
                yield
                V.op(lambda e: e.tensor_scalar(out=gmask.t[:], in0=gl, scalar1=c_(0), scalar2=None, op0=ALU.is_equal), R=[lg, sm], W=[gmask])
                yield
                V.op(lambda e: e.tensor_single_scalar(out=c_(1), in_=c_(0), scalar=-1.0, op=ALU.mult), R=[sm], W=[sm])
                yield
                V.op(lambda e: e.memset(c_(2), 0.0), W=[sm])
                yield
                A.op(lambda e: e.activation(out=gex.t[:], in_=gl, func=AF.Exp, bias=c_(1), accum_out=c_(2)), R=[lg, sm], W=[gex, sm])
                yield
                V.op(lambda e: e.reciprocal(out=c_(3), in_=c_(2)), R=[sm], W=[sm])
                yield
                V.op(lambda e: e.tensor_scalar(out=pen.t[:], in0=gmask.t[:], scalar1=-1.0, scalar2=BIG, op0=ALU.add, op1=ALU.mult),
                     R=[gmask], W=[pen])
                yield
                for g in range(4):
                    V.op(lambda e, g=g: e.tensor_scalar(out=elm.t[:, g * 8:(g + 1) * 8], in0=lg.t[:, 4 + g * 8:12 + g * 8],
                                                        scalar1=pen.t[:, g:g + 1], scalar2=None, op0=ALU.add), R=[lg, pen], W=[elm])
                    yield
                V.op(lambda e: e.reduce_max(out=c_(4), in_=elm.t[:], axis=AX.X), R=[elm], W=[sm])
                yield
                V.op(lambda e: e.tensor_scalar(out=mk1.t[:], in0=elm.t[:], scalar1=c_(4), scalar2=None, op0=ALU.is_equal), R=[elm, sm], W=[mk1])
                yield
                V.op(lambda e: e.scalar_tensor_tensor(out=elm2.t[:], in0=mk1.t[:], scalar=-BIG, in1=elm.t[:], op0=ALU.mult, op1=ALU.add),
                     R=[mk1, elm], W=[elm2])
                yield
                V.op(lambda e: e.reduce_max(out=c_(5), in_=elm2.t[:], axis=AX.X), R=[elm2], W=[sm])
                yield
                V.op(lambda e: e.tensor_scalar(out=mk2.t[:], in0=elm2.t[:], scalar1=c_(5), scalar2=None, op0=ALU.is_equal), R=[elm2, sm], W=[mk2])
                yield
                V.op(lambda e: e.tensor_tensor(out=c_(6), in0=c_(5), in1=c_(4), op=ALU.subtract), R=[sm], W=[sm])
                yield
                A.op(lambda e: e.activation(out=c_(7), in_=c_(6), func=AF.Exp), R=[sm], W=[sm])
                yield
                V.op(lambda e: e.tensor_single_scalar(out=c_(8), in_=c_(7), scalar=1.0, op=ALU.add), R=[sm], W=[sm])
                yield
                V.op(lambda e: e.reciprocal(out=c_(9), in_=c_(8)), R=[sm], W=[sm])
                yield
                V.op(lambda e, ti=ti: e.tensor_tensor(out=gates.t[:, ti, 0:1], in0=c_(9), in1=c_(3), op=ALU.mult), R=[sm], W=[gtb[ti]])
                yield
                V.op(lambda e, ti=ti: e.tensor_tensor(out=gates.t[:, ti, 1:2], in0=gates.t[:, ti, 0:1], in1=c_(7), op=ALU.mult),
                     R=[sm, gtb[ti]], W=[gtb[ti]])
                yield
                V.op(lambda e: e.tensor_tensor(out=cnt.t[:], in0=mk1.t[:], in1=mk2.t[:], op=ALU.add), R=[mk1, mk2], W=[cnt])
                yield
                P.op(lambda e: e.matmul(out=ps_r.t[:], lhsT=strictT, rhs=cnt.t[:], start=True, stop=True), R=[cnt, cfT], W=[ps_r])
                P.op(lambda e: e.matmul(out=ps_c.t[:], lhsT=onesF, rhs=cnt.t[:], start=True, stop=True), R=[cnt, cfT], W=[ps_c])
                V.op(lambda e: e.tensor_tensor(out=pos.t[:], in0=ps_r.t[:], in1=tot.t[:], op=ALU.add), R=[ps_r, tot], W=[pos])
                V.op(lambda e: e.tensor_tensor(out=tot.t[:], in0=ps_c.t[:], in1=tot.t[:], op=ALU.add), R=[ps_c, tot], W=[tot])
                yield
                V.op(lambda e: e.tensor_single_scalar(out=pos.t[:], in_=pos.t[:], scalar=float(CAP - 1), op=ALU.min), R=[pos], W=[pos])
                yield
                V.op(lambda e: e.tensor_tensor(out=pos.t[:], in0=pos.t[:], in1=ecol, op=ALU.add), R=[pos, cfT], W=[pos])
                yield
                for k, mk in enumerate((mk1, mk2)):
                    V.op(lambda e, mk=mk: e.tensor_tensor(out=tmp32.t[:], in0=pos.t[:], in1=mk.t[:], op=ALU.mult), R=[pos, mk], W=[tmp32])
                    yield
                    V.op(lambda e, k=k, ti=ti: e.reduce_sum(out=dstf.t[:, ti, k:k + 1], in_=tmp32.t[:], axis=AX.X), R=[tmp32], W=[dfb[ti]])
                    yield
                V.op(lambda e, ti=ti: e.tensor_copy(out=dsti.t[:, ti, :], in_=dstf.t[:, ti, :]), R=[dfb[ti]], W=[dib[ti]])
                yield
                for k in range(2):
                    G.dma(lambda e, k=k, ti=ti, p2=p2: e.indirect_dma_start(
                        out=Xs, out_offset=bass.IndirectOffsetOnAxis(ap=dsti.t[:, ti, k:k + 1], axis=0), in_=n2b[p2].t[:, :], in_offset=None),
                        n2s[p2], R=[n2b[p2], dib[ti]], W=[Xs_buf])
            DONE2 = object()
            for pr_ in range(8):
                gens = [route(2 * pr_), route(2 * pr_ + 1)]
                while gens:
                    for g_ in list(gens):
                        if next(g_, DONE2) is DONE2:
                            gens.remove(g_)
            if debug:
                S.dma(lambda e: e.dma_start(out=dbg_h.rearrange("(t p) d -> p t d", p=128), in_=hh.t[:]), dslot, R=hhb, W=[out_buf])

            es2a.close()
            es2b = ExitStack()
            pg.es = es2b
            junk4 = pg.sb("junk3b", [128, D], BF16)
            xg = [pg.sb("xg%d" % i, [128, 2, D], BF16) for i in range(2)]
            xgs = [pg.slot() for _ in range(2)]
            xT = [pg.sb("xT%d" % i, [128, 8, 256], BF16) for i in range(2)]
            w1s = [pg.sb("w1s%d" % i, [128, 8, 512], BF16) for i in range(2)]
            w3s = [pg.sb("w3s%d" % i, [128, 8, 512], BF16) for i in range(2)]
            w2s = [pg.sb("w2s%d" % i, [128, 4, D], BF16) for i in range(2)]
            wsl = [pg.slot() for _ in range(2)]
            sa = [pg.sb("sa%d" % i, [128, 256], F32) for i in range(2)]
            hT = [pg.sb("hT%d" % i, [128, 4, 256], BF16) for i in range(2)]
            ysb = [pg.sb("ysb%d" % i, [128, 2, D], F32) for i in range(1)]
            yss = [pg.slot() for _ in range(2)]
            ya = [pg.sb("ya%d" % i, [128, D], F32) for i in range(2)]
            yb = [pg.sb("yb%d" % i, [128, D], F32) for i in range(2)]
            ygs = [pg.slot() for _ in range(2)]
            ot = [pg.sb("ot%d" % i, [128, D], F32) for i in range(2)]
            ots = [pg.slot() for _ in range(2)]
            Xs_all = Buf("Xs_all")
            Xs_all.w = dict(Xs_buf.w)
            for p2 in range(2):
                Xs_all.w[id(n2s[p2].s)] = (n2s[p2].s, n2s[p2].v)
            def eloads(ex):
                p2 = ex % 2
                S.dma(lambda e, ex=ex, p2=p2: e.dma_start(out=xg[p2].t[:], in_=Xs[ex * CAP:(ex + 1) * CAP, :].rearrange("(s p) f -> p s f", p=128)),
                      xgs[p2], R=[Xs_all], W=[xg[p2]])
                S.dma(lambda e, ex=ex, p2=p2: e.dma_start(out=w1s[p2].t[:], in_=w1b[ex].rearrange("(c p) f -> p c f", p=128)), wsl[p2], R=[wb_buf[ex]], W=[w1s[p2]])
                S.dma(lambda e, ex=ex, p2=p2: e.dma_start(out=w3s[p2].t[:], in_=w3b[ex].rearrange("(c p) f -> p c f", p=128)), wsl[p2], R=[wb_buf[ex]], W=[w3s[p2]])
                S.dma(lambda e, ex=ex, p2=p2: e.dma_start(out=w2s[p2].t[:], in_=w2b[ex].rearrange("(c p) f -> p c f", p=128)), wsl[p2], R=[wb_buf[ex]], W=[w2s[p2]])

            eloads(0)
            for ex in range(NE):
                p2 = ex % 2
                if ex + 1 < NE:
                    eloads(ex + 1)
                for cq in range(4):
                    for cc in range(2):
                        c = cq * 2 + cc
                        for s in range(2):
                            P.op(lambda e, c=c, cc=cc, s=s, p2=p2: e.transpose(out=ps_x.t[:, cc, s * 128:(s + 1) * 128],
                                                                               in_=xg[p2].t[:, s, c * 128:(c + 1) * 128], identity=identB),
                                 R=[xg[p2], cbT], W=[ps_x])
                    if cq % 2 == 0:
                        V.op(lambda e, cq=cq, p2=p2: e.tensor_copy(out=xT[p2].t[:, cq * 2:cq * 2 + 2, :], in_=ps_x.t[:]), R=[ps_x], W=[xT[p2]])
                    else:
                        A.op(lambda e, cq=cq, p2=p2: e.activation(out=xT[p2].t[:, cq * 2:cq * 2 + 2, :], in_=ps_x.t[:], func=AF.Copy),
                             R=[ps_x], W=[xT[p2]])
                for fc in range(4):
                    f2 = fc % 2
                    ps_a = ps_a2[f2]
                    ps_b = ps_b2[f2]
                    for c in range(8):
                        P.op(lambda e, c=c, fc=fc, p2=p2, ps_a=ps_a: e.matmul(out=ps_a.t[:], lhsT=w1s[p2].t[:, c, fc * 128:(fc + 1) * 128], rhs=xT[p2].t[:, c, :],
                                                                   start=(c == 0), stop=(c == 7)), R=[w1s[p2], xT[p2]], W=[ps_a])
                    for c in range(8):
                        P.op(lambda e, c=c, fc=fc, p2=p2, ps_b=ps_b: e.matmul(out=ps_b.t[:], lhsT=w3s[p2].t[:, c, fc * 128:(fc + 1) * 128], rhs=xT[p2].t[:, c, :],
                                                                   start=(c == 0), stop=(c == 7)), R=[w3s[p2], xT[p2]], W=[ps_b])
                    A.op(lambda e, f2=f2, ps_a=ps_a: e.activation(out=sa[f2].t[:], in_=ps_a.t[:], func=AF.Silu), R=[ps_a], W=[sa[f2]])
                    V.op(lambda e, f2=f2, fc=fc, p2=p2, ps_b=ps_b: e.tensor_tensor(out=hT[p2].t[:, fc, :], in0=ps_b.t[:], in1=sa[f2].t[:], op=ALU.mult),
                         R=[ps_b, sa[f2]], W=[hT[p2]])
                for s in range(2):
                    for half in range(2):
                        yy = ps_y[half]
                        for fc in range(4):
                            P.op(lambda e, fc=fc, s=s, half=half, p2=p2, yy=yy: e.matmul(
                                out=yy.t[:], lhsT=hT[p2].t[:, fc, s * 128:(s + 1) * 128], rhs=w2s[p2].t[:, fc, half * 512:(half + 1) * 512],
                                start=(fc == 0), stop=(fc == 3)), R=[hT[p2], w2s[p2]], W=[yy])
                        if half == 0:
                            V.op(lambda e, s=s, p2=p2, yy=yy: e.tensor_copy(out=ysb[0].t[:, s, 0:512], in_=yy.t[:]), R=[yy], W=[ysb[0]])
                        else:
                            A.op(lambda e, s=s, p2=p2, yy=yy: e.activation(out=ysb[0].t[:, s, 512:1024], in_=yy.t[:], func=AF.Copy), R=[yy], W=[ysb[0]])
                G.dma(lambda e, ex=ex, p2=p2: e.dma_start(out=Ys[ex * CAP:(ex + 1) * CAP, :].rearrange("(s p) f -> p s f", p=128), in_=ysb[0].t[:]),
                      yss[0], R=[ysb[0]], W=[Ys_buf[ex]])

            def gathers(ti):
                p2 = ti % 2
                G.dma(lambda e, ti=ti, p2=p2: e.indirect_dma_start(out=ya[p2].t[:, :], out_offset=None, in_=Ys,
                                                                   in_offset=bass.IndirectOffsetOnAxis(ap=dsti.t[:, ti, 0:1], axis=0)),
                      ygs[p2], R=Ys_buf + [dib[ti]], W=[ya[p2]])
                G.dma(lambda e, ti=ti, p2=p2: e.indirect_dma_start(out=yb[p2].t[:, :], out_offset=None, in_=Ys,
                                                                   in_offset=bass.IndirectOffsetOnAxis(ap=dsti.t[:, ti, 1:2], axis=0)),
                      ygs[p2], R=Ys_buf + [dib[ti]], W=[yb[p2]])

            gathers(0)
            for ti in range(16):
                p2 = ti % 2
                if ti + 1 < 16:
                    gathers(ti + 1)
                V.op(lambda e, ti=ti, p2=p2: e.scalar_tensor_tensor(out=hh.t[:, ti, :], in0=ya[p2].t[:], scalar=gates.t[:, ti, 0:1], in1=hh.t[:, ti, :],
                                                                    op0=ALU.mult, op1=ALU.add), R=[ya[p2], gtb[ti], hhb[ti]], W=[hhb[ti]])
                V.op(lambda e, ti=ti, p2=p2: e.scalar_tensor_tensor(out=hh.t[:, ti, :], in0=yb[p2].t[:], scalar=gates.t[:, ti, 1:2], in1=hh.t[:, ti, :],
                                                                    op0=ALU.mult, op1=ALU.add), R=[yb[p2], gtb[ti], hhb[ti]], W=[hhb[ti]])
                A.op(lambda e, ti=ti: e.activation(out=junk4.t[:], in_=hh.t[:, ti, :], func=AF.Square, accum_out=ss3.t[:, ti:ti + 1]),
                     R=[hhb[ti]], W=[junk4, ss3])
                rstd_ops(ss3, rs3, ti, D, EPS)
                V.op(lambda e, ti=ti, p2=p2: e.scalar_tensor_tensor(out=ot[p2].t[:], in0=hh.t[:, ti, :], scalar=rs3.t[:, ti:ti + 1], in1=gfT.t[:],
                                                                    op0=ALU.mult, op1=ALU.mult), R=[hhb[ti], rs3, gfT], W=[ot[p2]])
                S.dma(lambda e, ti=ti, p2=p2: e.dma_start(out=out[ti * 128:(ti + 1) * 128, :], in_=ot[p2].t[:]), ots[p2], R=[ot[p2]], W=[out_buf])
            fin = Buf("fin")
            for sl in ots + [dslot]:
                if sl.v:
                    fin.w[id(sl.s)] = (sl.s, sl.v)
            S.wait_all([fin])
            es2b.close()
            pg.es = es0
        else:
            fin = Buf("fin")
            if dslot.v:
                fin.w[id(dslot.s)] = (dslot.s, dslot.v)
            S.wait_all([fin])
        pg.emit()
    return nc


def _consts():
    a = np.arange(128)
    ident = np.eye(128, dtype=np.float32)
    triu = (a[:, None] <= a[None, :]).astype(np.float32)
    strict = (a[:, None] < a[None, :]).astype(np.float32)
    ones = np.ones((128, 128), np.float32)
    ecol = np.tile((np.arange(NE) * CAP).astype(np.float32)[None, :], (128, 1))
    cf = np.concatenate([ident, triu, strict, ones, ecol], axis=1)
    L = (a[:, None] >= a[None, :]).astype(np.float32)
    U = triu
    cb = np.concatenate([ident, L, U, L, U], axis=1).astype(ml_dtypes.bfloat16)
    return np.ascontiguousarray(cf), np.ascontiguousarray(cb)


def make_in_maps(x, norm1_g, w_in, gla_gate_w2, gla_gate_b, gla_norm_g, w_out, norm2_g,
                 router_group_w, router_group_b, router_expert_w, router_expert_b,
                 expert_w1, expert_w3, expert_w2, final_norm_g):
    f = lambda a: np.ascontiguousarray(np.asarray(a, dtype=np.float32))
    x = f(x)
    win = f(w_in)[0]
    cf, cb = _consts()
    gq0, gk0, gv0, gr0, glr0, aq0, ak0, av0 = 0, 256, 512, 1024, 1536, 1552, 2064, 2576
    w1 = f(expert_w1)[0]
    w3 = f(expert_w3)[0]
    w2 = f(expert_w2)[0]
    wo = f(w_out)[0]
    g2r = f(np.tile(np.asarray(norm2_g)[0][None, :], (128, 1)))
    gfr = f(np.tile(np.asarray(final_norm_g)[None, :], (128, 1)))
    wr = f(np.concatenate([np.asarray(router_group_w)[0], np.asarray(router_expert_w)[0]], axis=1))
    br = np.concatenate([np.asarray(router_group_b)[0], np.asarray(router_expert_b)[0]])
    brr = f(np.tile(br[None, :], (128, 1)))
    g1 = f(np.asarray(norm1_g)[0].reshape(8, 128).T)
    gng = f(np.tile(np.asarray(gla_norm_g)[0][None, :], (128, 4)))
    maps = []
    for c in range(8):
        b, j = c // 4, c % 4
        cols = np.concatenate([
            np.arange(gq0 + 64 * j, gq0 + 64 * j + 64), np.arange(gk0 + 64 * j, gk0 + 64 * j + 64),
            np.arange(aq0 + 128 * j, aq0 + 128 * j + 128), np.arange(ak0 + 128 * j, ak0 + 128 * j + 128),
            np.arange(av0 + 128 * j, av0 + 128 * j + 128), np.arange(glr0, glr0 + 16),
            np.arange(gk0 + 64 * j, gk0 + 64 * j + 64), np.arange(gv0 + 128 * j, gv0 + 128 * j + 128),
            np.arange(gr0 + 128 * j, gr0 + 128 * j + 128)])
        w2aug = np.concatenate([np.asarray(gla_gate_w2)[0][:, 64 * j:64 * j + 64],
                                np.asarray(gla_gate_b)[0][None, 64 * j:64 * j + 64]], axis=0)
        rowidx = (j * 1024 + np.arange(8)[None, :] * 128 + np.arange(128)[:, None]).astype(np.int32)
        maps.append({
            "x": x[b], "xres": np.ascontiguousarray(x[b, 2048 * j:2048 * (j + 1)]),
            "w_in": np.ascontiguousarray(win[:, cols]), "g1": g1, "w2aug": f(w2aug), "gng": gng,
            "w_out": wo, "g2r": g2r, "gfr": gfr, "wr": wr, "brr": brr, "w1": w1, "w3": w3, "w2": w2,
            "cf": cf, "cb": cb, "rowidx": np.ascontiguousarray(rowidx),
        })
    return maps


_NC_CACHE = {}


def kernel(**inputs):
    if "nc" not in _NC_CACHE:
        _NC_CACHE["nc"] = build()
    nc = _NC_CACHE["nc"]
    maps = make_in_maps(**inputs)
    res = run_bass_kernel_spmd(nc, maps, core_ids=list(range(8)))
    outs = [np.asarray(res.results[c]["out"], dtype=np.float32) for c in range(8)]
    y = np.stack([np.concatenate(outs[0:4], axis=0), np.concatenate(outs[4:8], axis=0)], axis=0)
    return y
```
